# Optimizing a Trainium2 kernel written in Bass

```python
import jax, jax.numpy as jnp
from jax import lax
import numpy as np

D_MODEL = 1024
BATCH = 16
SEQ = 2048
DEPTH = 2

GRID_W = 64
CTX_LEN = 256
N_EVEN = (DEPTH + 1) // 2
N_ODD = DEPTH // 2
EPS = 1e-6
N_MOD = 6

A_HEADS = 8
A_KV_HEADS = 2
A_GROUP = A_HEADS // A_KV_HEADS
A_HEAD_DIM = 64
ROPE_THETA = 10000.0
Q_BLOCK = 128

B_HEADS = 4
B_DK = 64
B_DV = 128
B_GATE_RANK = 16
B_GATE_TAU = 16.0
B_CHUNK = 64

AB_SIZES = (A_HEADS * A_HEAD_DIM, A_KV_HEADS * A_HEAD_DIM, A_KV_HEADS * A_HEAD_DIM,
            B_HEADS * B_DK, B_HEADS * B_DK, B_HEADS * B_DV, B_HEADS * B_DV,
            B_GATE_RANK, B_GATE_RANK)
AB_WIDTH = sum(AB_SIZES)
AB_OUT = A_HEADS * A_HEAD_DIM + B_HEADS * B_DV

C_HEADS = 16
C_HEAD_DIM = 64
C_WIDTH = C_HEADS * C_HEAD_DIM
NA_ROWS = 8
NA_COLS = 16

D_FF = 3584
N_EXPERTS = 8
TOP_K = 2

kernel_name = 'hybrid_dit_gqa_gla_natten_moe'


def _rmsnorm(x, g):
    x32 = x.astype(jnp.float32)
    y = x32 * lax.rsqrt(jnp.mean(x32 * x32, axis=-1, keepdims=True) + EPS)
    return (y * g.astype(jnp.float32)).astype(x.dtype)


def _modulate(h, shift, scale):
    return h * (1 + scale) + shift


def _adaln(cond, w_mod, b_mod):
    m = jax.nn.silu(cond) @ w_mod + b_mod
    return jnp.split(m[..., None, :], N_MOD, axis=-1)


def _swiglu(h, wg, wu, wd):
    return (jax.nn.silu(h @ wg) * (h @ wu)) @ wd


def _moe(h, w_router, wg, wu, wd):
    logits = (h @ w_router).astype(jnp.float32)
    top_v, top_i = lax.top_k(logits, TOP_K)
    gates = jax.nn.softmax(top_v, axis=-1)
    comb = jnp.sum(gates[..., None] * jax.nn.one_hot(top_i, N_EXPERTS, dtype=jnp.float32), axis=-2).astype(h.dtype)
    out = jnp.zeros_like(h)
    for e in range(N_EXPERTS):
        out = out + comb[..., e:e + 1] * _swiglu(h, wg[e], wu[e], wd[e])
    return out


def _rope_2d(seq_len):
    t = jnp.arange(seq_len, dtype=jnp.int32)
    row = (t // GRID_W).astype(jnp.float32)
    col = (t % GRID_W).astype(jnp.float32)
    n_axis = A_HEAD_DIM // 4
    inv = jnp.power(ROPE_THETA, -jnp.arange(n_axis, dtype=jnp.float32) / n_axis)
    ang = jnp.concatenate([row[:, None] * inv, col[:, None] * inv], axis=-1)
    return jnp.cos(ang), jnp.sin(ang)


def _apply_rope(x, cos, sin):
    shp = (1, x.shape[1]) + (1,) * (x.ndim - 3) + (cos.shape[-1],)
    cos = cos.reshape(shp).astype(x.dtype)
    sin = sin.reshape(shp).astype(x.dtype)
    xp = x.reshape(x.shape[:-1] + (-1, 2))
    x0, x1 = xp[..., 0], xp[..., 1]
    return jnp.stack([x0 * cos - x1 * sin, x0 * sin + x1 * cos], axis=-1).reshape(x.shape)


def _sdpa(q, k, v):
    s = jnp.einsum('bqkgd,bskd->bkgqs', q, k).astype(jnp.float32)
    p = jax.nn.softmax(s, axis=-1).astype(v.dtype)
    return jnp.einsum('bkgqs,bskd->bqkgd', p, v)


def _blocked_sdpa(q, k, v):
    bsz, seq_len = q.shape[:2]
    nb = seq_len // Q_BLOCK
    qb = jnp.moveaxis(q.reshape((bsz, nb, Q_BLOCK) + q.shape[2:]), 1, 0)
    ob = lax.map(lambda qi: _sdpa(qi, k, v), qb)
    return jnp.moveaxis(ob, 0, 1).reshape(q.shape)


def _log_decay(z, w2, b):
    bsz, tot, _ = z.shape
    la = jax.nn.log_sigmoid((z @ w2 + b).astype(jnp.float32)) / B_GATE_TAU
    return la.reshape(bsz, tot, B_HEADS, B_DK)


def _gla_chunked(q, k, v, log_a):
    bsz, tot, nh, dk = q.shape
    dv = v.shape[-1]
    n = tot // B_CHUNK

    def chunks(a):
        return a.astype(jnp.float32).reshape(bsz, n, B_CHUNK, nh, a.shape[-1]).transpose(0, 3, 1, 2, 4)

    qc, kc, vc = chunks(q), chunks(k), chunks(v)
    bcum = jnp.cumsum(chunks(log_a), axis=3)
    b_last = bcum[:, :, :, -1:, :]
    q_dec = qc * jnp.exp(bcum)
    k_intra = kc * jnp.exp(-bcum)
    k_state = kc * jnp.exp(b_last - bcum)
    lower = jnp.tril(jnp.ones((B_CHUNK, B_CHUNK), dtype=bool))
    att = jnp.where(lower, jnp.einsum('bhncd,bhnsd->bhncs', q_dec, k_intra), 0.0)
    o_intra = jnp.einsum('bhncs,bhnsv->bhncv', att, vc)
    d_state = jnp.einsum('bhncd,bhncv->bhndv', k_state, vc)
    chunk_decay = jnp.exp(b_last[:, :, :, 0, :])

    def step(state, inp):
        dec, ds = inp
        return dec[..., None] * state + ds, state

    s0 = jnp.zeros((bsz, nh, dk, dv), jnp.float32)
    _, s_prev = lax.scan(step, s0, (jnp.moveaxis(chunk_decay, 2, 0), jnp.moveaxis(d_state, 2, 0)))
    o_inter = jnp.einsum('bhncd,nbhdv->bhncv', q_dec, s_prev)
    o = (o_intra + o_inter).transpose(0, 2, 3, 1, 4).reshape(bsz, tot, nh, dv)
    return o.astype(v.dtype)


def _mixer_ab(h_lat, h_ctx, w_in, g_q, g_k, w_a2_f, b_a_f, w_a2_b, b_a_b, g_gla, w_out, need_ctx):
    bsz, seq_len, _ = h_lat.shape
    n_ctx = h_ctx.shape[1]
    tot = n_ctx + seq_len
    proj = jnp.concatenate([h_ctx, h_lat], axis=1) @ w_in
    q_a, k_a, v_a, q_b, k_b, v_b, r_b, z_f, z_b = jnp.split(proj, np.cumsum(AB_SIZES)[:-1].tolist(), axis=-1)

    q_a = _rmsnorm(q_a.reshape(bsz, tot, A_KV_HEADS, A_GROUP, A_HEAD_DIM), g_q) * (A_HEAD_DIM ** -0.5)
    k_a = _rmsnorm(k_a.reshape(bsz, tot, A_KV_HEADS, A_HEAD_DIM), g_k)
    v_a = v_a.reshape(bsz, tot, A_KV_HEADS, A_HEAD_DIM)
    cos, sin = _rope_2d(seq_len)
    q_lat = _apply_rope(q_a[:, n_ctx:], cos, sin)
    k_all = jnp.concatenate([k_a[:, :n_ctx], _apply_rope(k_a[:, n_ctx:], cos, sin)], axis=1)
    o_a_lat = _blocked_sdpa(q_lat, k_all, v_a).reshape(bsz, seq_len, A_HEADS * A_HEAD_DIM)

    q_b = q_b.reshape(bsz, tot, B_HEADS, B_DK) * (B_DK ** -0.5)
    k_b = k_b.reshape(bsz, tot, B_HEADS, B_DK)
    v_b = v_b.reshape(bsz, tot, B_HEADS, B_DV)
    la_f = _log_decay(z_f, w_a2_f, b_a_f)
    la_b = _log_decay(z_b, w_a2_b, b_a_b)

    def rev(a):
        return jnp.concatenate([a[:, :n_ctx][:, ::-1], a[:, n_ctx:][:, ::-1]], axis=1)

    o_f = _gla_chunked(q_b, k_b, v_b, la_f)
    o_r = rev(_gla_chunked(rev(q_b), rev(k_b), rev(v_b), rev(la_b)))
    o_b = _rmsnorm(o_f + o_r, g_gla).reshape(bsz, tot, B_HEADS * B_DV) * jax.nn.silu(r_b)

    y_lat = jnp.concatenate([o_a_lat, o_b[:, n_ctx:]], axis=-1) @ w_out
    if not need_ctx:
        return y_lat, None
    o_a_ctx = _sdpa(q_a[:, :n_ctx], k_a[:, :n_ctx], v_a[:, :n_ctx]).reshape(bsz, n_ctx, A_HEADS * A_HEAD_DIM)
    y_ctx = jnp.concatenate([o_a_ctx, o_b[:, :n_ctx]], axis=-1) @ w_out
    return y_lat, y_ctx


def _mixer_c(h_lat, h_ctx, w_in, rpb, w_out, need_ctx):
    bsz, seq_len, _ = h_lat.shape
    n_ctx = h_ctx.shape[1]
    rows = seq_len // GRID_W
    kr = min(NA_ROWS, rows)
    kc = NA_COLS
    scale = C_HEAD_DIM ** -0.5
    q, k, v = jnp.split(h_lat @ w_in, 3, axis=-1)
    grid = (bsz, rows, GRID_W, C_HEADS, C_HEAD_DIM)
    q = q.reshape(grid) * scale
    k = k.reshape(grid)
    v = v.reshape(grid)
    k_ctx, v_ctx = jnp.split(h_ctx @ w_in[:, C_WIDTH:], 2, axis=-1)
    k_ctx = k_ctx.reshape(bsz, n_ctx, C_HEADS, C_HEAD_DIM)
    v_ctx = v_ctx.reshape(bsz, n_ctx, C_HEADS, C_HEAD_DIM)

    cols = jnp.arange(GRID_W, dtype=jnp.int32)
    col_start = jnp.clip(cols - kc // 2, 0, GRID_W - kc)
    in_win = (cols[None, :] >= col_start[:, None]) & (cols[None, :] < col_start[:, None] + kc)
    col_idx = jnp.clip(cols[None, :] - cols[:, None] + NA_COLS - 1, 0, 2 * NA_COLS - 2)
    rpb_cols = rpb.astype(jnp.float32)[:, :, col_idx]

    def row_block(r):
        rs = jnp.clip(r - kr // 2, 0, rows - kr)
        dr_idx = rs + jnp.arange(kr, dtype=jnp.int32) - r + NA_ROWS - 1
        bias = jnp.where(in_win[None, None], rpb_cols[:, dr_idx], -jnp.inf)
        bias = bias.transpose(0, 2, 1, 3)
        q_r = lax.dynamic_index_in_dim(q, r, axis=1, keepdims=False)
        k_blk = lax.dynamic_slice_in_dim(k, rs, kr, axis=1)
        v_blk = lax.dynamic_slice_in_dim(v, rs, kr, axis=1)
        s_loc = jnp.einsum('bqhd,bikhd->bhqik', q_r, k_blk).astype(jnp.float32) + bias
        s_ctx = jnp.einsum('bqhd,bshd->bhqs', q_r, k_ctx).astype(jnp.float32)
        s = jnp.concatenate([s_loc.reshape(bsz, C_HEADS, GRID_W, kr * GRID_W), s_ctx], axis=-1)
        p = jax.nn.softmax(s, axis=-1).astype(v.dtype)
        p_loc = p[..., :kr * GRID_W].reshape(bsz, C_HEADS, GRID_W, kr, GRID_W)
        p_ctx = p[..., kr * GRID_W:]
        return jnp.einsum('bhqik,bikhd->bqhd', p_loc, v_blk) + jnp.einsum('bhqs,bshd->bqhd', p_ctx, v_ctx)

    o = lax.map(row_block, jnp.arange(rows, dtype=jnp.int32))
    y_lat = jnp.moveaxis(o, 0, 1).reshape(bsz, seq_len, C_WIDTH) @ w_out
    if not need_ctx:
        return y_lat, None
    q_ctx = (h_ctx @ w_in[:, :C_WIDTH]).reshape(bsz, n_ctx, C_HEADS, 1, C_HEAD_DIM) * scale
    o_ctx = _sdpa(q_ctx, k_ctx, v_ctx).reshape(bsz, n_ctx, C_WIDTH)
    return y_lat, o_ctx @ w_out


def setup_inputs(seed: int = 0) -> dict:
    key = jax.random.key(seed)
    keys = iter(jax.random.split(key, 40))

    def nrm(shape, s):
        return jax.random.normal(next(keys), shape, jnp.float32) * s

    def gain(shape):
        return 1.0 + nrm(shape, 0.02)

    d = D_MODEL
    return {
        'x': nrm((BATCH, SEQ, d), 1.0),
        'c': nrm((BATCH, d), 1.0),
        'ctx': nrm((BATCH, CTX_LEN, d), 1.0),
        'c_ctx': nrm((d,), 1.0),
        'w_mod': nrm((DEPTH, d, N_MOD * d), 0.5 * d ** -0.5),
        'b_mod': nrm((DEPTH, N_MOD * d), 0.01),
        'g_norm1': gain((DEPTH, d)),
        'g_norm2': gain((DEPTH, d)),
        'w_in_ab': nrm((N_EVEN, d, AB_WIDTH), d ** -0.5),
        'g_q': gain((N_EVEN, A_HEAD_DIM)),
        'g_k': gain((N_EVEN, A_HEAD_DIM)),
        'w_a2_f': nrm((N_EVEN, B_GATE_RANK, B_HEADS * B_DK), B_GATE_RANK ** -0.5),
        'b_a_f': nrm((N_EVEN, B_HEADS * B_DK), 0.1),
        'w_a2_b': nrm((N_EVEN, B_GATE_RANK, B_HEADS * B_DK), B_GATE_RANK ** -0.5),
        'b_a_b': nrm((N_EVEN, B_HEADS * B_DK), 0.1),
        'g_gla': gain((N_EVEN, B_DV)),
        'w_out_ab': nrm((N_EVEN, AB_OUT, d), AB_OUT ** -0.5),
        'w_ff_gate': nrm((N_EVEN, d, D_FF), d ** -0.5),
        'w_ff_up': nrm((N_EVEN, d, D_FF), d ** -0.5),
        'w_ff_down': nrm((N_EVEN, D_FF, d), D_FF ** -0.5),
        'w_in_c': nrm((N_ODD, d, 3 * C_WIDTH), d ** -0.5),
        'rpb_c': nrm((N_ODD, C_HEADS, 2 * NA_ROWS - 1, 2 * NA_COLS - 1), 0.02),
        'w_out_c': nrm((N_ODD, C_WIDTH, d), C_WIDTH ** -0.5),
        'w_router': nrm((N_ODD, d, N_EXPERTS), d ** -0.5),
        'w_moe_gate': nrm((N_ODD, N_EXPERTS, d, D_FF), d ** -0.5),
        'w_moe_up': nrm((N_ODD, N_EXPERTS, d, D_FF), d ** -0.5),
        'w_moe_down': nrm((N_ODD, N_EXPERTS, D_FF, d), D_FF ** -0.5),
        'g_final': gain((d,)),
    }


def reference(x, c, ctx, c_ctx, w_mod, b_mod, g_norm1, g_norm2, w_in_ab, g_q, g_k, w_a2_f, b_a_f,
              w_a2_b, b_a_b, g_gla, w_out_ab, w_ff_gate, w_ff_up, w_ff_down, w_in_c, rpb_c, w_out_c,
              w_router, w_moe_gate, w_moe_up, w_moe_down, g_final):
    for i in range(DEPTH):
        j = i // 2
        need_ctx = i < DEPTH - 1
        sh1, sc1, gt1, sh2, sc2, gt2 = _adaln(c, w_mod[i], b_mod[i])
        cmod = _adaln(c_ctx, w_mod[i], b_mod[i])
        h_lat = _modulate(_rmsnorm(x, g_norm1[i]), sh1, sc1)
        h_ctx = _modulate(_rmsnorm(ctx, g_norm1[i]), cmod[0], cmod[1])
        if i % 2 == 0:
            y_lat, y_ctx = _mixer_ab(h_lat, h_ctx, w_in_ab[j], g_q[j], g_k[j], w_a2_f[j], b_a_f[j],
                                     w_a2_b[j], b_a_b[j], g_gla[j], w_out_ab[j], need_ctx)

            def ffn(h):
                return _swiglu(h, w_ff_gate[j], w_ff_up[j], w_ff_down[j])
        else:
            y_lat, y_ctx = _mixer_c(h_lat, h_ctx, w_in_c[j], rpb_c[j], w_out_c[j], need_ctx)

            def ffn(h):
                return _moe(h, w_router[j], w_moe_gate[j], w_moe_up[j], w_moe_down[j])
        x = x + gt1 * y_lat
        x = x + gt2 * ffn(_modulate(_rmsnorm(x, g_norm2[i]), sh2, sc2))
        if need_ctx:
            ctx = ctx + cmod[2] * y_ctx
            ctx = ctx + cmod[5] * ffn(_modulate(_rmsnorm(ctx, g_norm2[i]), cmod[3], cmod[4]))
    return _rmsnorm(x, g_final)
```

```python
import numpy as np
from contextlib import ExitStack
import concourse.bass as bass
import concourse.mybir as mybir
from concourse.bass_utils import run_bass_kernel_spmd

F32 = mybir.dt.float32
BF16 = mybir.dt.bfloat16
AF = mybir.ActivationFunctionType
ALU = mybir.AluOpType
AX = mybir.AxisListType

CENG = ('pe', 'act', 'dve', 'pool')
ENG = ('pe', 'act', 'dve', 'pool', 'sp')
NDSEM = 8
EPS = 1e-6
NCORES = 8
D = 1024
SEQ = 2048
NCTX = 256
TOT = SEQ + NCTX
NT = TOT // 128
DFF = 3584
NFT = DFF // 128
ABW = 2336
NEXP = 8
SLOT_T = 512
NKT = 24
NSLOT = NKT * SLOT_T
BIGIDX = 1.0e6
I32 = mybir.dt.int32


class Prog:
    def __init__(self, nc):
        self.nc = nc
        self.stack = ExitStack()
        self.ops = {e: [] for e in ENG}
        self.cnt = {e: 0 for e in CENG}
        self.pending_inc = {e: False for e in CENG}
        self.clock = {e: {} for e in ENG}
        self.lastw = {}
        self.readers = {}
        self.dma_rr = {q: 0 for q in ('sp', 'act', 'pool')}
        self.dma_cnt = {}
        self.dma_last = {}
        self.sems = {}
        for e in CENG:
            self.sems['c_' + e] = self.stack.enter_context(nc.semaphore('c_' + e))
        for q in ('sp', 'act', 'pool'):
            for i in range(NDSEM):
                n = 'd_%s_%d' % (q, i)
                self.sems[n] = self.stack.enter_context(nc.semaphore(n))
                self.dma_cnt[n] = 0
        self._cur = None
        self.psum_keys = set()

    def _uname(self, name):
        self._uid = getattr(self, '_uid', 0) + 1
        return 's%d_%s' % (self._uid, name)

    def sb(self, name, shape, dtype, stack=None):
        return (stack or self.stack).enter_context(self.nc.sbuf_tensor(self._uname(name), list(shape), dtype))

    def ps(self, name, shape, dtype, stack=None):
        self.psum_keys.add(name)
        return (stack or self.stack).enter_context(self.nc.psum_tensor(self._uname(name), list(shape), dtype))

    def bank(self, name, dtype, stack=None):
        return self.ps(name, [128, 512] if dtype == F32 else [128, 1024], dtype, stack)

    def _need(self, eng, ev):
        s, v, origin, clk = ev
        c = self.clock[eng]
        if c.get(s, 0) >= v:
            return
        self.ops[eng].append(('wait', s, v))
        c[s] = v
        for s2, v2 in clk.items():
            if c.get(s2, 0) < v2:
                c[s2] = v2

    def _deps(self, eng, reads, writes, is_dma=False):
        for r in reads:
            ev = self.lastw.get(r)
            if ev is not None:
                if (not is_dma) and eng == 'pe' and ev[2] == 'pe':
                    continue
                self._need(eng, ev)
            if r in self.psum_keys:
                rd = self.readers.get(r)
                if rd:
                    for ev2 in list(rd.values()):
                        if ev2[2] != eng:
                            self._need(eng, ev2)
        for w in writes:
            ev = self.lastw.get(w)
            if ev is not None and (is_dma or ev[2] != eng or eng != 'pe'):
                self._need(eng, ev)
            rd = self.readers.get(w)
            if rd:
                for ev in rd.values():
                    if is_dma or ev[2] != eng or eng != 'pe':
                        self._need(eng, ev)

    def _record(self, ev, reads, writes):
        for w in writes:
            self.lastw[w] = ev
            self.readers[w] = {}
        for r in reads:
            d = self.readers.setdefault(r, {})
            old = d.get(ev[0])
            if old is None or old[1] < ev[1]:
                d[ev[0]] = ev

    def op(self, eng, fn, reads=(), writes=(), inc=True):
        self._deps(eng, reads, writes)
        v = self.cnt[eng] + 1
        if inc:
            self.cnt[eng] = v
            self.pending_inc[eng] = False
        else:
            self.pending_inc[eng] = True
        ev = ('c_' + eng, v, eng, dict(self.clock[eng]))
        self._record(ev, reads, writes)
        self.ops[eng].append(('op', fn, inc))
        return ev

    def dma(self, q, out, in_, reads=(), writes=(), **kw):
        self._deps(q, reads, writes, is_dma=True)
        i = self.dma_rr[q]
        self.dma_rr[q] = (i + 1) % NDSEM
        n = 'd_%s_%d' % (q, i)
        last = self.dma_last.get(n)
        if last is not None:
            self._need(q, last)
        v = self.dma_cnt[n] + 16
        self.dma_cnt[n] = v
        ev = (n, v, 'dma', dict(self.clock[q]))
        self.dma_last[n] = ev
        self._record(ev, reads, writes)
        self.ops[q].append(('dma', out, in_, n, kw))
        return ev

    def dma_raw(self, q, fn, reads=(), writes=()):
        self._deps(q, reads, writes, is_dma=True)
        i = self.dma_rr[q]
        self.dma_rr[q] = (i + 1) % NDSEM
        n = 'd_%s_%d' % (q, i)
        last = self.dma_last.get(n)
        if last is not None:
            self._need(q, last)
        v = self.dma_cnt[n] + 16
        self.dma_cnt[n] = v
        ev = (n, v, 'dma', dict(self.clock[q]))
        self.dma_last[n] = ev
        self._record(ev, reads, writes)
        self.ops[q].append(('rawdma', fn, n))
        return ev

    def barrier(self):
        evs = []
        for e in CENG:
            assert not self.pending_inc[e], e
            if self.cnt[e] > 0:
                evs.append(('c_' + e, self.cnt[e], e, {}))
        for n, ev in self.dma_last.items():
            evs.append(ev)
        for e in ENG:
            for ev in evs:
                self._need(e, ev)
        self.lastw = {}
        self.readers = {}

    def flush(self):
        ops = self.ops
        sems = self.sems

        def emit(e, name):
            for o in ops[name]:
                if o[0] == 'wait':
                    e.wait_ge(sems[o[1]], o[2])
                elif o[0] == 'op':
                    ins = o[1](e)
                    if o[2]:
                        ins.then_inc(sems['c_' + name], 1)
                elif o[0] == 'rawdma':
                    o[1](e).then_inc(sems[o[2]], 16)
                else:
                    _, out, in_, n, kw = o
                    e.dma_start(out=out, in_=in_, **kw).then_inc(sems[n], 16)

        with self.nc.Block() as block:
            @block.tensor
            def _(e):
                emit(e, 'pe')

            @block.scalar
            def _(e):
                emit(e, 'act')

            @block.vector
            def _(e):
                emit(e, 'dve')

            @block.gpsimd
            def _(e):
                emit(e, 'pool')

            @block.sync
            def _(e):
                emit(e, 'sp')
        self.ops = {e: [] for e in ENG}

    def end_phase(self):
        self.barrier()
        self.flush()

    def mm(self, out, lhsT, rhs, start=True, stop=True, r=(), w=(), inc=None):
        if inc is None:
            inc = stop
        return self.op('pe', lambda e: e.matmul(out, lhsT=lhsT, rhs=rhs, start=start, stop=stop,
                                                skip_group_check=True), r, w, inc)

    def tr(self, out, in_, ident, r=(), w=(), inc=True):
        return self.op('pe', lambda e: e.transpose(out=out, in_=in_, identity=ident), r, w, inc)

    def act(self, out, in_, func, r=(), w=(), scale=None, bias=None, eng='act'):
        kw = {}
        if scale is not None:
            kw['scale'] = scale
        if bias is not None:
            kw['bias'] = bias
        return self.op('act', lambda e: e.activation(out=out, in_=in_, func=func, **kw), r, w)

    def tt(self, eng, out, in0, in1, op, r=(), w=()):
        return self.op(eng, lambda e: e.tensor_tensor(out=out, in0=in0, in1=in1, op=op), r, w)

    def ts(self, eng, out, in0, s1, op0, s2=None, op1=None, r=(), w=()):
        if op1 is None:
            return self.op(eng, lambda e: e.tensor_scalar(out=out, in0=in0, scalar1=s1, scalar2=None, op0=op0), r, w)
        return self.op(eng, lambda e: e.tensor_scalar(out=out, in0=in0, scalar1=s1, scalar2=s2, op0=op0, op1=op1), r, w)

    def stt(self, out, in0, scalar, in1, op0, op1, r=(), w=()):
        return self.op('dve', lambda e: e.scalar_tensor_tensor(out=out, in0=in0, scalar=scalar, in1=in1,
                                                               op0=op0, op1=op1), r, w)

    def copy(self, eng, out, in_, r=(), w=()):
        if eng == 'act':
            return self.op('act', lambda e: e.copy(out=out, in_=in_), r, w)
        return self.op(eng, lambda e: e.tensor_copy(out=out, in_=in_), r, w)

    def recip(self, out, in_, r=(), w=()):
        return self.op('dve', lambda e: e.reciprocal(out=out, in_=in_), r, w)

    def memset(self, eng, ap, val, w=()):
        return self.op(eng, lambda e: e.memset(ap, val), (), w)

    def reduce(self, out, in_, op, r=(), w=()):
        return self.op('dve', lambda e: e.tensor_reduce(out=out, in_=in_, axis=AX.X, op=op), r, w)

    def sumsq(self, junk, in_, acc, r=(), w=()):
        return self.op('dve', lambda e: e.scalar_tensor_tensor(out=junk, in0=in_, scalar=1.0, in1=in_,
                                                               op0=ALU.mult, op1=ALU.mult, accum_out=acc), r, w)


def bc(ap, shape):
    return ap.to_broadcast(list(shape))


class Builder:
    def __init__(self, upto='all', debug=False, preload=()):
        self.preload = set(preload)
        self.upto = upto
        self.debug = debug
        nc = bass.Bass("TRN2", target_bir_lowering=False)
        self.nc = nc
        self.P = Prog(nc)
        self.I = {}
        self.dbg = {}

    def inp(self, name, shape, dtype=F32):
        t = self.nc.dram_tensor(name, list(shape), dtype, kind="ExternalInput").ap()
        self.I[name] = t
        return t

    def dump(self, name, ap, shape, dtype=F32, reads=()):
        if not self.debug:
            return
        t = self.nc.dram_tensor('dbg_' + name, list(shape), dtype, kind="ExternalOutput").ap()
        self.dbg[name] = t
        idx = tuple(slice(None) for _ in shape)
        self.P.dma('sp', t[idx], ap, reads=list(reads), writes=['dbg_' + name])

    def scratch(self, name, shape, dtype=F32):
        if name in self.preload:
            return self.inp(name, shape, dtype)
        if self.debug:
            t = self.nc.dram_tensor(name, list(shape), dtype, kind="ExternalOutput").ap()
            self.dbg[name] = t
            return t
        return self.nc.dram_tensor(name, list(shape), dtype).ap()

    def declare(self):
        inp = self.inp
        inp('x', [2, SEQ, D]); inp('ctx', [2, NCTX, D]); inp('condT', [128, 8, 3])
        inp('w_mod', [2, D, 6 * D]); inp('b_mod', [2, 6 * D]); inp('gnT', [128, 2, 2, 8])
        inp('w_in_ab', [D, ABW]); inp('g_q', [1, 64]); inp('g_k', [1, 64])
        inp('w2f', [17, 256]); inp('w2b', [17, 256]); inp('g_gla', [1, 128])
        inp('w_out_ab', [D, D]); inp('w_ff_gate', [D, DFF]); inp('w_ff_up', [D, DFF]); inp('w_ff_down', [DFF, D])
        inp('w_in_c', [D, 3 * D]); inp('nabias', [16, 128, 14 * 64]); inp('w_out_c', [D, D])
        inp('w_router', [128, 8, NEXP])
        inp('w_moe_gate', [NEXP, D, DFF]); inp('w_moe_up', [NEXP, D, DFF]); inp('w_moe_down', [NEXP, DFF, D])
        inp('g_final', [1, D])
        inp('ident', [128, 128]); inp('tri_f', [128, 128]); inp('tri_b', [128, 128])
        inp('ropeC', [128, 16, 64]); inp('ropeS', [128, 16, 64])
        inp('stri', [128, 128]); inp('thr', [128, 64]); inp('kv', [128, NKT * 8]); inp('tokid', [128, 32])
        inp('dflt', [128, (NSLOT // 128) * 4]); inp('gn2_nat', [1, D])
        inp('rowc4', [128, 32]); inp('rowf', [128, NFT])
        self.out = self.nc.dram_tensor('out', [2, SEQ, D], F32, kind="ExternalOutput").ap()
        sc = self.scratch
        self.mod_d = sc('mod_d', [2, 3, 6, D])
        self.resA = sc('resA', [2, TOT, D])
        self.resB = sc('resB', [2, TOT, D])
        self.resC = sc('resC', [2, SEQ, D])
        self.qkb_d = sc('qkb_d', [2, TOT, 512])
        self.vb_d = sc('vb_d', [2, TOT, 512], BF16)
        self.rb_d = sc('rb_d', [2, TOT, 512])
        self.h_d = sc('h_d', [2 * SEQ, D], BF16)
        self.tab = sc('tab', [NSLOT, 4])
        self.y12 = sc('y12', [4 * SEQ + NSLOT, D])

    def consts(self):
        P, I = self.P, self.I
        self.ident_f = P.sb('ident_f', [128, 128], F32)
        self.ident_b = P.sb('ident_b', [128, 128], BF16)
        self.tri = {'f': P.sb('tri_f', [128, 128], F32), 'b': P.sb('tri_b', [128, 128], F32)}
        self.ones_f = P.sb('ones_f', [128, 8], F32)
        self.modAB = P.sb('modAB', [128, 2, 3, 2, 2, 8], F32)
        self.epsb = P.sb('epsb', [128, 1], F32)
        P.dma('sp', self.ident_f[:], I['ident'][:, :], writes=['ident_f'])
        P.dma('sp', self.tri['f'][:], I['tri_f'][:, :], writes=['tri_f'])
        P.dma('sp', self.tri['b'][:], I['tri_b'][:, :], writes=['tri_b'])
        P.copy('dve', self.ident_b[:], self.ident_f[:], r=['ident_f'], w=['ident_b'])
        P.memset('dve', self.ones_f[:], 1.0, w=['ones_f'])
        P.memset('dve', self.epsb[:], EPS, w=['epsb'])

    def phase_mod(self):
        P, I = self.P, self.I
        with ExitStack() as st:
            condT = P.sb('condT', [128, 8, 3], F32, st)
            scond = P.sb('scond', [128, 8, 3], F32, st)
            bmod = P.sb('bmod', [3, 6 * D], F32, st)
            mt = P.sb('mt', [3, 6 * D], F32, st)
            wbuf = [P.sb('wm%d' % i, [128, 8, 512], F32, st) for i in range(3)]
            psm = [P.ps('psm%d' % i, [128, 512], F32, st) for i in range(2)]
            modF = P.sb('modF', [128, 2, 3, 6, 8], F32, st)
            gn = P.sb('gn', [128, 2, 2, 8], F32, st)
            P.dma('sp', condT[:], I['condT'][:, :, :], writes=['condT'])
            P.dma('sp', gn[:], I['gnT'][:, :, :, :], writes=['gn'])
            P.act(scond[:], condT[:], AF.Silu, r=['condT'], w=['scond'])
            k = 0
            for i in range(2):
                P.dma('sp', bmod[:], I['b_mod'][i:i + 1, :].partition_broadcast(3), writes=['bmod'])
                wv = I['w_mod'][i].rearrange("(c p) f -> p c f", p=128)
                for n in range(12):
                    wb = wbuf[k % 3]
                    wk = 'wm%d' % (k % 3)
                    P.dma('sp' if k % 2 == 0 else 'act', wb[:], wv[:, :, n * 512:(n + 1) * 512], writes=[wk])
                    pk = 'psm%d' % (k % 2)
                    for c in range(8):
                        P.mm(psm[k % 2][0:3, :], scond[:, c, :], wb[:, c, :], start=(c == 0), stop=(c == 7),
                             r=['scond', wk], w=[pk])
                    P.tt('dve', mt[:, n * 512:(n + 1) * 512], psm[k % 2][0:3, :], bmod[:, n * 512:(n + 1) * 512],
                         ALU.add, r=[pk, 'bmod'], w=['mt'])
                    k += 1
                P.dma('sp', self.mod_d[i].rearrange("j k d -> j (k d)"), mt[:], reads=['mt'], writes=['mod_d'])
                for j in range(3):
                    P.dma('sp', modF[:, i, j, :, :], self.mod_d[i, j].rearrange("k (c p) -> p k c", p=128),
                          reads=['mod_d'], writes=['modF'], allow_slow_non_contiguous=True)
            for i in range(2):
                for j in range(3):
                    for n in range(2):
                        P.stt(self.modAB[:, i, j, n, 0, :], modF[:, i, j, 1 + 3 * n, :], 1.0, gn[:, i, n, :],
                              ALU.add, ALU.mult, r=['modF', 'gn'], w=['modAB'])
                        P.copy('dve', self.modAB[:, i, j, n, 1, :], modF[:, i, j, 3 * n, :], r=['modF'], w=['modAB'])
            P.end_phase()

    def load_gate(self, tile, key, layer, cond, which, q='sp'):
        k = 2 if which == 1 else 5
        self.P.dma(q, tile[:], self.mod_d[layer, cond, k:k + 1, :].partition_broadcast(128),
                   reads=['mod_d'], writes=[key])

    def norm_mod(self, st, tag, src_fn, ntiles, cond_fn, layer, norm, hT, hkey, want32=None, tm_cb=None, post_cb=None):
        P = self.P
        f32path = want32 is not None
        xt = [P.sb('%s_xt%d' % (tag, i), [128, D], F32, st) for i in range(3)]
        junk = P.sb(tag + '_junk', [128, D], BF16, st)
        ss = P.sb(tag + '_ss', [128, 2], F32, st)
        dt_n = F32 if f32path else BF16
        xn = [P.sb('%s_xn%d' % (tag, i), [128, D], dt_n, st) for i in range(2)]
        nps = 1 if f32path else 2
        pst = [P.ps('%s_pst%d' % (tag, i), [128, 8, 128], dt_n, st) for i in range(nps)]
        tm = [P.sb('%s_tm%d' % (tag, i), [128, 8, 128], F32, st) for i in range(2)]
        h32 = [P.sb('%s_h32_%d' % (tag, i), [128, 8, 128], F32, st) for i in range(2)] if f32path else None
        ident = self.ident_f if f32path else self.ident_b
        ikey = 'ident_f' if f32path else 'ident_b'
        def stage_a(t):
            x_ = xt[t % 3]; xk = '%s_xt%d' % (tag, t % 3)
            P.dma('sp', x_[:], src_fn(t), reads=['src_' + tag], writes=[xk])
            sk = tag + '_ss'
            P.sumsq(junk[:], x_[:], ss[:, 0:1], r=[xk], w=[tag + '_junk', sk + '0'])
            P.act(ss[:, 1:2], ss[:, 0:1], AF.Sqrt, r=[sk + '0', 'epsb'], w=[sk + '1'], scale=1.0 / D, bias=self.epsb[:, 0:1])
            P.recip(ss[:, 0:1], ss[:, 1:2], r=[sk + '1'], w=[sk + '0'])
            n_ = xn[t % 2]; nk = '%s_xn%d' % (tag, t % 2)
            P.act(n_[:], x_[:], AF.Identity, r=[xk, sk + '0'], w=[nk], scale=ss[:, 0:1])
            if tm_cb is not None:
                tm_cb(t, n_, nk)

        def stage_b(t):
            n_ = xn[t % 2]; nk = '%s_xn%d' % (tag, t % 2)
            ps_ = pst[t % nps]; pk = '%s_pst%d' % (tag, t % nps)
            for c in range(8):
                P.tr(ps_[:, c, :], n_[:, c * 128:(c + 1) * 128], ident[:], r=[nk, ikey], w=[pk], inc=(c == 7))
            cond = cond_fn(t)
            A = self.modAB[:, layer, cond, norm, 0, :]
            B = self.modAB[:, layer, cond, norm, 1, :]
            tm_ = tm[t % 2]; tk = '%s_tm%d' % (tag, t % 2)
            P.tt('dve', tm_[:], ps_[:], bc(A.unsqueeze(2), [128, 8, 128]), ALU.mult, r=[pk, 'modAB'], w=[tk])
            if f32path:
                h_ = h32[t % 2]; hk32 = '%s_h32_%d' % (tag, t % 2)
                P.tt('pool', h_[:], tm_[:], bc(B.unsqueeze(2), [128, 8, 128]), ALU.add, r=[tk, 'modAB'], w=[hk32])
                if hT is not None:
                    P.copy('act', hT[:, :, t * 128:(t + 1) * 128], h_[:], r=[hk32], w=[hkey + str(t)])
                want32(t, h_, hk32)
            else:
                P.tt('pool', hT[:, :, t * 128:(t + 1) * 128], tm_[:], bc(B.unsqueeze(2), [128, 8, 128]), ALU.add,
                     r=[tk, 'modAB'], w=[hkey + str(t)])

        for t in range(ntiles):
            stage_a(t)
            if t >= 1:
                stage_b(t - 1)
                if post_cb is not None:
                    post_cb(t - 1)
        stage_b(ntiles - 1)
        if post_cb is not None:
            post_cb(ntiles - 1)


    def layer0_mixer(self, b):
        P = self.P
        with ExitStack() as st0:
            o_tm = P.sb('o_tm', [128, NT, 512], BF16, st0)
            zaug = {d: P.sb('zaug_' + d, [17, TOT], F32, st0) for d in 'fb'}
            with ExitStack() as st1:
                qT = P.sb('qT', [64, 8, TOT], BF16, st1)
                kT = P.sb('kT', [64, 2, TOT], BF16, st1)
                v_sb = P.sb('v_sb', [128, NT, 2, 65], BF16, st1)
                with ExitStack() as st1a:
                    self.l0_inproj(b, st1a, qT, kT, v_sb, zaug)
                if self.upto in ('l0_norm', 'l0_inproj'):
                    return
                with ExitStack() as st1b:
                    self.l0_gqa(b, st1b, qT, kT, v_sb, o_tm)
                if self.upto == 'l0_gqa':
                    self.dump('o_tm', o_tm[:], [128, NT, 512], BF16)
                    P.end_phase()
                    return
            with ExitStack() as st2:
                oT = P.sb('oT', [128, 8, TOT], BF16, st2)
                with ExitStack() as st2a:
                    self.l0_gla(b, st2a, o_tm, zaug, oT)
                if self.upto == 'l0_gla':
                    self.dump('oT', oT[:], [128, 8, TOT], BF16)
                    P.end_phase()
                    return
                with ExitStack() as st2b:
                    self.l0_outproj(b, st2b, oT)

    def l0_inproj(self, b, st, qT, kT, v_sb, zaug):
        P, I = self.P, self.I
        hT = P.sb('hT', [128, 8, TOT], BF16, st)
        with ExitStack() as stn:
            def src(t):
                return I['ctx'][b, t * 128:(t + 1) * 128, :] if t < 2 else I['x'][b, (t - 2) * 128:(t - 1) * 128, :]
            import os
            self.norm_mod(stn, 'n1', src, int(os.environ.get('NTILES', NT)), lambda t: 2 if t < 2 else b, 0, 0, hT, 'hT')
            P.end_phase()
        if self.upto == 'l0_norm':
            if not os.environ.get('NODUMP'):
                self.dump('hT', hT[:], [128, 8, TOT], BF16)
            P.end_phase()
            return
        hkeys = ['hT%d' % t for t in range(NT)]
        w_in = P.sb('w_in', [128, 8, ABW], BF16, st)
        wv = I['w_in_ab'].rearrange("(c p) f -> p c f", p=128)
        for c in range(8):
            P.dma('pool', w_in[:, c, :], wv[:, c, :], writes=['w_in%d' % c])
        wkeys = ['w_in%d' % c for c in range(8)]
        gqk = P.sb('gqk', [128, 10, 64], F32, st)
        ropeC = P.sb('ropeC', [128, 16, 64], F32, st)
        ropeS = P.sb('ropeS', [128, 16, 64], F32, st)
        P.dma('sp', ropeC[:], I['ropeC'][:, :, :], writes=['ropeC'])
        P.dma('sp', ropeS[:], I['ropeS'][:, :, :], writes=['ropeS'])
        for h in range(8):
            P.dma('sp', gqk[:, h, :], I['g_q'][0:1, :].partition_broadcast(128), writes=['gqk'])
        for h in range(2):
            P.dma('sp', gqk[:, 8 + h, :], I['g_k'][0:1, :].partition_broadcast(128), writes=['gqk'])
        P.ts('dve', gqk[:, 0:8, :], gqk[:, 0:8, :], 0.125, ALU.mult, r=['gqk'], w=['gqk'])
        P.memset('dve', v_sb[:], 1.0, w=['v_sb'])
        for d in 'fb':
            P.memset('dve', zaug[d][:], 1.0, w=['zaug_' + d])
        pa0 = P.ps('pa0', [128, 512], F32, st)
        pa1 = P.bank('pa1', F32, st)[:, 0:256]
        pb = [P.ps('pb%d' % i, [128, 512], F32, st) for i in range(3)]
        ptq = P.bank('ptq', BF16, st)[0:64, :].rearrange("p (h d) -> p h d", d=128)
        ptk = P.bank('ptk', BF16, st)[0:64, 0:256].rearrange("p (h d) -> p h d", d=128)
        sq = P.sb('sq', [128, 640], F32, st)
        s10 = P.sb('s10', [128, 3, 10], F32, st)
        qk = P.sb('qk', [128, 10, 64], F32, st)
        qk2 = P.sb('qk2', [128, 10, 64], F32, st)
        sw = P.sb('sw', [128, 10, 64], F32, st)
        qkb = P.sb('qkb', [128, 10, 64], BF16, st)
        stq = [P.sb('stq%d' % i, [128, 512], F32, st) for i in range(2)]
        stv = [P.sb('stv%d' % i, [128, 512], BF16, st) for i in range(2)]
        str_ = [P.sb('str%d' % i, [128, 512], F32, st) for i in range(2)]
        blks = [(0, 512), (512, 512), (1024, 512), (1536, 512), (2048, 256)]
        for (t0, n) in blks:
            hk = hkeys[t0 // 128:(t0 + n) // 128]
            for di, d in enumerate('fb'):
                for c in range(8):
                    P.mm(pb[0][0:16, 0:n], w_in[:, c, 2304 + 16 * di:2320 + 16 * di], hT[:, c, t0:t0 + n],
                         start=(c == 0), stop=(c == 7), r=hk + [wkeys[c]], w=['pb0'])
                P.copy('act', zaug[d][0:16, t0:t0 + n], pb[0][0:16, 0:n], r=['pb0'], w=['zaug_' + d])
        qkr = [P.sb('qkr%d' % i, [128, 640], F32, st) for i in range(2)]

        def st_mm(t):
            hk = [hkeys[t]]
            tok = slice(t * 128, (t + 1) * 128)
            i2 = t % 2
            for c in range(8):
                P.mm(pa0[:], hT[:, c, tok], w_in[:, c, 0:512], start=(c == 0), stop=(c == 7),
                     r=hk + [wkeys[c]], w=['pa0'])
            for c in range(8):
                P.mm(pa1, hT[:, c, tok], w_in[:, c, 512:768], start=(c == 0), stop=(c == 7),
                     r=hk + [wkeys[c]], w=['pa1'])
            for gi, c0 in enumerate((768, 1280, 1792)):
                for c in range(8):
                    P.mm(pb[gi][:], hT[:, c, tok], w_in[:, c, c0:c0 + 512], start=(c == 0), stop=(c == 7),
                         r=hk + [wkeys[c]], w=['pb%d' % gi])
            P.copy('act', qkr[i2][:, 0:512], pa0[:], r=['pa0'], w=['qkr%d_a' % i2])
            P.copy('act', qkr[i2][:, 512:640], pa1[:, 0:128], r=['pa1'], w=['qkr%d_b' % i2])
            P.copy('act', v_sb[:, t, :, 0:64], pa1[:, 128:256].rearrange("p (h d) -> p h d", d=64), r=['pa1'], w=['v_sb'])
            P.copy('act', stq[i2][:], pb[0][:], r=['pb0'], w=['stq%d' % i2])
            P.copy('dve', stv[i2][:], pb[1][:], r=['pb1'], w=['stv%d' % i2])
            P.act(str_[i2][:], pb[2][:], AF.Silu, r=['pb2'], w=['str%d' % i2])

        def st_store(t):
            tok = slice(t * 128, (t + 1) * 128)
            i2 = t % 2
            P.dma('pool', self.qkb_d[b, tok, :], stq[i2][:], reads=['stq%d' % i2], writes=['qkb_d'])
            P.dma('pool', self.vb_d[b, tok, :], stv[i2][:], reads=['stv%d' % i2], writes=['vb_d'])
            P.dma('pool', self.rb_d[b, tok, :], str_[i2][:], reads=['str%d' % i2], writes=['rb_d'])

        def st_chain(t):
            i2 = t % 2
            qa = 'qkr%d_a' % i2; qb_ = 'qkr%d_b' % i2
            src = qkr[i2]
            P.act(sq[:], src[:], AF.Square, r=[qa, qb_], w=['sq'])
            P.reduce(s10[:, 0, :], sq[:].rearrange("p (h d) -> p h d", d=64), ALU.add, r=['sq'], w=['s10a'])
            P.act(s10[:, 1, :], s10[:, 0, :], AF.Sqrt, r=['s10a', 'epsb'], w=['s10b'], scale=1.0 / 64, bias=self.epsb[:, 0:1])
            P.recip(s10[:, 2, :], s10[:, 1, :], r=['s10b'], w=['s10c'])
            P.tt('dve', qk[:], src[:].rearrange("p (h d) -> p h d", d=64),
                 bc(s10[:, 2, :].unsqueeze(2), [128, 10, 64]), ALU.mult, r=[qa, qb_, 's10c'], w=['qk'])
            if t < 2:
                P.tt('pool', qkb[:], qk[:], gqk[:], ALU.mult, r=['qk', 'gqk'], w=['qkb'])
            else:
                P.tt('pool', qk2[:], qk[:], gqk[:], ALU.mult, r=['qk', 'gqk'], w=['qk2'])
                C = bc(ropeC[:, t - 2, :].unsqueeze(1), [128, 10, 64])
                Sv = ropeS[:, t - 2, :].rearrange("p (j two) -> p j two", two=2)
                q2v = qk2[:].rearrange("p h (j two) -> p h j two", two=2)
                swv = sw[:].rearrange("p h (j two) -> p h j two", two=2)
                P.tt('pool', swv[:, :, :, 0], q2v[:, :, :, 1], bc(Sv[:, :, 0].unsqueeze(1), [128, 10, 32]),
                     ALU.mult, r=['qk2', 'ropeS'], w=['sw0'])
                P.tt('pool', swv[:, :, :, 1], q2v[:, :, :, 0], bc(Sv[:, :, 1].unsqueeze(1), [128, 10, 32]),
                     ALU.mult, r=['qk2', 'ropeS'], w=['sw1'])
                P.tt('dve', qk[:], qk2[:], C, ALU.mult, r=['qk2', 'ropeC'], w=['qk'])
                P.tt('dve', qkb[:], qk[:], sw[:], ALU.add, r=['qk', 'sw0', 'sw1'], w=['qkb'])

        def st_trans(t):
            tok = slice(t * 128, (t + 1) * 128)
            for h in range(8):
                P.tr(ptq[:, h, :], qkb[:, h, :], self.ident_b[:], r=['qkb', 'ident_b'], w=['ptq'], inc=(h == 7))
            for h in range(2):
                P.tr(ptk[:, h, :], qkb[:, 8 + h, :], self.ident_b[:], r=['qkb', 'ident_b'], w=['ptk'], inc=(h == 1))
            P.copy('act', qT[:, :, tok], ptq, r=['ptq'], w=['qT%d' % t])
            P.copy('dve', kT[:, :, tok], ptk, r=['ptk'], w=['kT%d' % t])

        st_mm(0)
        st_store(0)
        for t in range(NT):
            if t + 1 < NT:
                st_mm(t + 1)
            st_chain(t)
            if t + 1 < NT:
                st_store(t + 1)
            st_trans(t)
        P.end_phase()

    def l0_gqa(self, b, st, qT, kT, v_sb, o_tm):
        P = self.P
        pbuf = [P.sb('pbuf%d' % i, [128, NT, 512], BF16, st) for i in range(2)]
        pss = [P.ps('pss%d' % i, [128, 512], F32, st) for i in range(4)]
        po = [P.ps('po%d' % i, [128, 512], F32, st) for i in range(2)]
        rc = P.sb('rc', [128, 2], F32, st)
        ib = 0; isx = 0; io = 0
        for h in range(8):
            kv = h // 4
            jobs = [(0, 256, 2)] + [(256 + 512 * i, 512, NT) for i in range(4)]
            for (q0, nq, nk) in jobs:
                pb_ = pbuf[ib % 2]; pbk = 'pbuf%d' % (ib % 2); ib += 1
                for kt in range(nk):
                    ps_ = pss[isx % 4]; psk = 'pss%d' % (isx % 4); isx += 1
                    P.mm(ps_[:, 0:nq], kT[:, kv, kt * 128:(kt + 1) * 128], qT[:, h, q0:q0 + nq], r=[], w=[psk])
                    P.act(pb_[:, kt, 0:nq], ps_[:, 0:nq], AF.Exp, r=[psk], w=['%s_%d' % (pbk, kt)])
                for j in range(nq // 128):
                    po_ = po[io % 2]; pok = 'po%d' % (io % 2); rk = 'rc%d' % (io % 2)
                    for kt in range(nk):
                        P.mm(po_[:, 0:65], pb_[:, kt, j * 128:(j + 1) * 128], v_sb[:, kt, kv, :],
                             start=(kt == 0), stop=(kt == nk - 1), r=['%s_%d' % (pbk, kt)], w=[pok])
                    P.recip(rc[:, io % 2:io % 2 + 1], po_[:, 64:65], r=[pok], w=[rk])
                    tq = q0 // 128 + j
                    P.ts('dve', o_tm[:, tq, h * 64:(h + 1) * 64], po_[:, 0:64], rc[:, io % 2:io % 2 + 1], ALU.mult,
                         r=[pok, rk], w=['o_tm%d' % tq])
                    io += 1
        P.end_phase()

    def l0_gla(self, b, st, o_tm, zaug, oT):
        P, I = self.P, self.I
        ptb = self._ptb
        for t in range(NT):
            for c in range(4):
                P.tr(ptb[:, c, :], o_tm[:, t, c * 128:(c + 1) * 128], self.ident_b[:], r=['ident_b'], w=['ptb'], inc=(c == 3))
            P.copy('act', oT[:, 0:4, t * 128:(t + 1) * 128], ptb, r=['ptb'], w=['oTa%d' % t])
        qkb_s = P.sb('qkb_s', [128, NT, 512], F32, st)
        vb_s = P.sb('vb_s', [128, NT, 512], BF16, st)
        o_acc = P.sb('o_acc', [128, NT, 512], F32, st)
        w2 = {d: P.sb('w2' + d, [17, 256], F32, st) for d in 'fb'}
        ggla = P.sb('ggla', [128, 128], F32, st)
        S32 = P.sb('S32', [64, 4, 128], F32, st)
        Sbf = P.sb('Sbf', [64, 4, 128], BF16, st)
        tmpS = P.sb('tmpS', [64, 4, 128], F32, st)
        e1 = P.sb('e1', [128, 256], F32, st)
        sp_ = P.sb('sp_', [128, 256], F32, st)
        eq = P.sb('eq', [128, 256], F32, st)
        ek = P.sb('ek', [128, 256], F32, st)
        dec = P.sb('dec', [64, 4], F32, st)
        qd = P.sb('qd', [128, 256], BF16, st)
        ki = P.sb('ki', [128, 256], BF16, st)
        qkT = P.sb('qkT', [64, 8, 128], BF16, st)
        qdT = qkT[:, 0:4, :]
        kiT = qkT[:, 4:8, :]
        att = P.sb('att', [128, 4, 128], BF16, st)
        pg1 = P.bank('pg1', F32, st)
        pxg = pg1[:, 0:256]
        pcs = pg1[:, 256:512]
        ptot = P.bank('ptot', F32, st)[0:64, 0:4]
        ptqk = P.bank('ptqk', BF16, st)[0:64, :].rearrange("p (h d) -> p h d", d=128)
        patt = P.bank('patt', F32, st)[:, :].rearrange("p (h d) -> p h d", d=128)
        pog = P.bank('pog', F32, st)
        pds = P.bank('pds', F32, st)[0:64, :].rearrange("p (h d) -> p h d", d=128)
        P.dma('sp', w2['f'][:], I['w2f'][:, :], writes=['w2f'])
        P.dma('sp', w2['b'][:], I['w2b'][:, :], writes=['w2b'])
        P.dma('sp', ggla[:], I['g_gla'][0:1, :].partition_broadcast(128), writes=['ggla'])
        for t in range(NT):
            P.dma('sp', qkb_s[:, t, :], self.qkb_d[b, t * 128:(t + 1) * 128, :], writes=['qkb_s%d' % t])
            P.dma('sp', vb_s[:, t, :], self.vb_d[b, t * 128:(t + 1) * 128, :], writes=['vb_s%d' % t])
        e1b = [e1, P.sb('e1b', [128, 256], F32, st)]
        spb = [sp_, P.sb('spb', [128, 256], F32, st)]
        eqb = [eq, P.sb('eqb', [128, 256], F32, st)]
        ekb = [ek, P.sb('ekb', [128, 256], F32, st)]
        decb = [dec, P.sb('decb', [64, 4], F32, st)]
        qdb = [qd, P.sb('qdb', [128, 256], BF16, st)]
        kib = [ki, P.sb('kib', [128, 256], BF16, st)]
        qkTb = [qkT, P.sb('qkTb', [64, 8, 128], BF16, st)]
        for d in 'fb':
            order = list(range(NT)) if d == 'f' else [1, 0] + list(range(NT - 1, 1, -1))
            P.memset('dve', S32[:], 0.0, w=['S32'])
            P.memset('dve', Sbf[:], 0.0, w=['Sbf'])

            def prep(oi, d=d, order=order):
                n = order[oi]
                x = oi % 2
                sx = str(x)
                tok = slice(n * 128, (n + 1) * 128)
                P.mm(pxg, zaug[d][:, tok], w2[d][:], r=['w2' + d], w=['pg1'])
                P.act(e1b[x][:], pxg, AF.Exp, r=['pg1'], w=['e1' + sx], scale=-1.0)
                P.act(spb[x][:], e1b[x][:], AF.Ln, r=['e1' + sx], w=['sp_' + sx], bias=1.0)
                P.mm(pcs, self.tri[d][:], spb[x][:], r=['tri_' + d, 'sp_' + sx], w=['pg1'])
                for h in range(4):
                    P.mm(ptot[:, h:h + 1], spb[x][:, h * 64:(h + 1) * 64], self.ones_f[:, 0:1], r=['sp_' + sx, 'ones_f'],
                         w=['ptot'], inc=(h == 3))
                P.act(decb[x][:], ptot, AF.Exp, r=['ptot'], w=['dec' + sx], scale=-1.0 / 16)
                P.act(eqb[x][:], pcs, AF.Exp, r=['pg1'], w=['eq' + sx], scale=-1.0 / 16)
                P.act(ekb[x][:], pcs, AF.Exp, r=['pg1'], w=['ek' + sx], scale=1.0 / 16)
                P.stt(qdb[x][:], qkb_s[:, n, 0:256], 0.125, eqb[x][:], ALU.mult, ALU.mult, r=['qkb_s%d' % n, 'eq' + sx], w=['qd' + sx])
                P.tt('pool', kib[x][:], qkb_s[:, n, 256:512], ekb[x][:], ALU.mult, r=['qkb_s%d' % n, 'ek' + sx], w=['ki' + sx])
                for h in range(4):
                    P.tr(ptqk[:, h, :], qdb[x][:, h * 64:(h + 1) * 64], self.ident_b[:], r=['qd' + sx, 'ident_b'], w=['ptqk'], inc=False)
                for h in range(4):
                    P.tr(ptqk[:, 4 + h, :], kib[x][:, h * 64:(h + 1) * 64], self.ident_b[:], r=['ki' + sx, 'ident_b'], w=['ptqk'], inc=(h == 3))
                P.copy('act', qkTb[x][:], ptqk, r=['ptqk'], w=['qkT' + sx])

            def chain(oi, d=d, order=order):
                n = order[oi]
                x = oi % 2
                sx = str(x)
                qdT_ = qkTb[x][:, 0:4, :]
                kiT_ = qkTb[x][:, 4:8, :]
                for h in range(4):
                    P.mm(patt[:, h, :], kiT_[:, h, :], qdT_[:, h, :], r=['qkT' + sx], w=['patt'], inc=(h == 3))
                P.tt('dve', att[:], patt, bc(self.tri[d][:].unsqueeze(1), [128, 4, 128]), ALU.mult,
                     r=['patt', 'tri_' + d], w=['att'])
                for h in range(4):
                    hs = slice(h * 128, (h + 1) * 128)
                    P.mm(pog[:, hs], att[:, h, :], vb_s[:, n, hs], start=True, stop=(oi == 0),
                         r=['att', 'vb_s%d' % n], w=['pog'], inc=False)
                    if oi > 0:
                        P.mm(pog[:, hs], qdT_[:, h, :], Sbf[:, h, :], start=False, stop=True,
                             r=['qkT' + sx, 'Sbf'], w=['pog'], inc=False)
                for h in range(4):
                    hs = slice(h * 128, (h + 1) * 128)
                    P.mm(pds[:, h, :], kib[x][:, h * 64:(h + 1) * 64], vb_s[:, n, hs], r=['ki' + sx, 'vb_s%d' % n],
                         w=['pds'], inc=(h == 3))
                if d == 'f':
                    P.copy('act', o_acc[:, n, :], pog[:], r=['pog'], w=['o_acc%d' % n])
                else:
                    P.tt('dve', o_acc[:, n, :], o_acc[:, n, :], pog[:], ALU.add, r=['pog', 'o_acc%d' % n], w=['o_acc%d' % n])
                P.tt('dve', tmpS[:], pds, S32[:], ALU.add, r=['pds', 'S32'], w=['tmpS'])
                P.tt('dve', S32[:], tmpS[:], bc(decb[x][:].unsqueeze(2), [64, 4, 128]), ALU.mult, r=['tmpS', 'dec' + sx], w=['S32'])
                P.copy('act', Sbf[:], S32[:], r=['S32'], w=['Sbf'])

            prep(0)
            for oi in range(len(order)):
                if oi + 1 < len(order):
                    prep(oi + 1)
                chain(oi)
        sqo = P.sb('sqo', [128, 512], F32, st)
        s4 = P.sb('s4', [128, 3, 4], F32, st)
        on = P.sb('on', [128, 4, 128], F32, st)
        on2 = P.sb('on2', [128, 4, 128], F32, st)
        rt = [P.sb('rt%d' % i, [128, 512], F32, st) for i in range(2)]
        ob = P.sb('ob', [128, 512], BF16, st)
        for n in range(NT):
            i2 = n % 2
            P.dma('sp', rt[i2][:], self.rb_d[b, n * 128:(n + 1) * 128, :], writes=['rt%d' % i2])
            P.act(sqo[:], o_acc[:, n, :], AF.Square, r=['o_acc%d' % n], w=['sqo'])
            P.reduce(s4[:, 0, :], sqo[:].rearrange("p (h d) -> p h d", d=128), ALU.add, r=['sqo'], w=['s4a'])
            P.act(s4[:, 1, :], s4[:, 0, :], AF.Sqrt, r=['s4a', 'epsb'], w=['s4b'], scale=1.0 / 128, bias=self.epsb[:, 0:1])
            P.recip(s4[:, 2, :], s4[:, 1, :], r=['s4b'], w=['s4c'])
            P.tt('dve', on[:], o_acc[:, n, :].rearrange("p (h d) -> p h d", d=128),
                 bc(s4[:, 2, :].unsqueeze(2), [128, 4, 128]), ALU.mult, r=['o_acc%d' % n, 's4c'], w=['on'])
            P.tt('pool', on2[:], on[:], bc(ggla[:].unsqueeze(1), [128, 4, 128]), ALU.mult, r=['on', 'ggla'], w=['on2'])
            P.tt('dve', ob[:], on2[:].rearrange("p h d -> p (h d)"), rt[i2][:], ALU.mult, r=['on2', 'rt%d' % i2], w=['ob'])
            for c in range(4):
                P.tr(ptb[:, c, :], ob[:, c * 128:(c + 1) * 128], self.ident_b[:], r=['ob', 'ident_b'], w=['ptb'], inc=(c == 3))
            P.copy('act', oT[:, 4:8, n * 128:(n + 1) * 128], ptb, r=['ptb'], w=['oTb%d' % n])
        P.end_phase()

    def l0_outproj(self, b, st, oT):
        P, I = self.P, self.I
        wo = P.sb('wo', [128, 8, D], BF16, st)
        wv = I['w_out_ab'].rearrange("(c p) f -> p c f", p=128)
        for c in range(8):
            P.dma('pool', wo[:, c, :], wv[:, c, :], writes=['wo%d' % c])
        G = {j: P.sb('G%d' % j, [128, D], F32, st) for j in (2, b)}
        for j in (2, b):
            self.load_gate(G[j], 'G%d' % j, 0, j, 1)
        self.proj_residual(st, 'o0', oT, lambda t: [], NT, wo, ['wo%d' % c for c in range(8)],
                           lambda t: (I['ctx'][b, t * 128:(t + 1) * 128, :] if t < 2 else I['x'][b, (t - 2) * 128:(t - 1) * 128, :]),
                           lambda t: (G[2], 'G2') if t < 2 else (G[b], 'G%d' % b),
                           lambda t: self.resA[b, t * 128:(t + 1) * 128, :], 'resA')
        P.end_phase()

    def proj_residual(self, st, tag, oT, okeys, ntiles, wo, wokeys, xsrc, gate_fn, dst, dkey, tok0=0):
        P = self.P
        py = [P.ps('%s_py%d' % (tag, i), [128, 512], F32, st) for i in range(4)]
        xt = [P.sb('%s_x%d' % (tag, i), [128, D], F32, st) for i in range(2)]
        tmp = [P.sb('%s_t%d' % (tag, i), [128, D], F32, st) for i in range(2)]
        xo = [P.sb('%s_o%d' % (tag, i), [128, D], F32, st) for i in range(2)]
        for t in range(ntiles):
            i2 = t % 2
            tok = slice(tok0 + t * 128, tok0 + (t + 1) * 128)
            P.dma('sp', xt[i2][:], xsrc(t), reads=['src_' + tag], writes=['%s_x%d' % (tag, i2)])
            G, gk = gate_fn(t)
            for half in range(2):
                p_ = py[2 * i2 + half]; pk = '%s_py%d' % (tag, 2 * i2 + half)
                for c in range(8):
                    P.mm(p_[:], oT[:, c, tok], wo[:, c, half * 512:(half + 1) * 512], start=(c == 0), stop=(c == 7),
                         r=okeys(t) + [wokeys[c]], w=[pk])
                hs = slice(half * 512, (half + 1) * 512)
                P.tt('dve', tmp[i2][:, hs], p_[:], G[:, hs], ALU.mult, r=[pk, gk], w=['%s_t%d_%d' % (tag, i2, half)])
                P.tt('pool', xo[i2][:, hs], tmp[i2][:, hs], xt[i2][:, hs], ALU.add,
                     r=['%s_t%d_%d' % (tag, i2, half), '%s_x%d' % (tag, i2)], w=['%s_o%d_%d' % (tag, i2, half)])
            P.dma('pool', dst(t), xo[i2][:], reads=['%s_o%d_0' % (tag, i2), '%s_o%d_1' % (tag, i2)], writes=[dkey])


def _swiglu_gate_up(self, P, hT, ntok, blks, wg_src, wu_src, f_tiles, act, act_off, wbufs, pgs, pus, sgs, ctr):
    i = 0
    while i < len(f_tiles):
        nf = min(2, len(f_tiles) - i)
        f0 = f_tiles[i]
        k = ctr[0] % len(wbufs); ctr[0] += 1
        wg, wu = wbufs[k]
        P.dma('pool', wg[:, :, 0:nf * 128], wg_src[:, :, f0 * 128:(f0 + nf) * 128], writes=['wg%d' % k])
        P.dma('pool', wu[:, :, 0:nf * 128], wu_src[:, :, f0 * 128:(f0 + nf) * 128], writes=['wu%d' % k])
        for fi in range(nf):
            for (t0, n) in blks:
                j = ctr[1] % 2; ctr[1] += 1
                for c in range(8):
                    P.mm(pgs[j][:, 0:n], wg[:, c, fi * 128:(fi + 1) * 128], hT[:, c, t0:t0 + n], start=(c == 0), stop=(c == 7),
                         r=['wg%d' % k], w=['pg%d' % j])
                for c in range(8):
                    P.mm(pus[j][:, 0:n], wu[:, c, fi * 128:(fi + 1) * 128], hT[:, c, t0:t0 + n], start=(c == 0), stop=(c == 7),
                         r=['wu%d' % k], w=['pu%d' % j])
                P.act(sgs[j][:, 0:n], pgs[j][:, 0:n], AF.Silu, r=['pg%d' % j], w=['sg%d' % j])
                P.tt('dve', act[:, act_off + i + fi, t0:t0 + n], sgs[j][:, 0:n], pus[j][:, 0:n], ALU.mult,
                     r=['sg%d' % j, 'pu%d' % j], w=['act%d' % (act_off + i + fi)])
        i += nf


def _l0_ffn(self, b, half):
    P, I = self.P, self.I
    NTB = 9
    TB = NTB * 128
    r0 = half * TB
    with ExitStack() as st0:
        hT = P.sb('hT2', [128, 8, TB], BF16, st0)
        with ExitStack() as stn:
            self.norm_mod(stn, 'n2', lambda t: self.resA[b, r0 + t * 128:r0 + (t + 1) * 128, :], NTB,
                          lambda t: 2 if (half == 0 and t < 2) else b, 0, 1, hT, 'hT2')
            P.end_phase()
        with ExitStack() as st:
            act = P.sb('act', [128, NFT, TB], BF16, st)
            wbufs = [(P.sb('wg%d' % i, [128, 8, 256], BF16, st), P.sb('wu%d' % i, [128, 8, 256], BF16, st)) for i in range(3)]
            pgs = [P.bank('pg%d' % i, F32, st) for i in range(2)]
            pus = [P.bank('pu%d' % i, F32, st) for i in range(2)]
            sgs = [P.sb('sg%d' % i, [128, 512], F32, st) for i in range(2)]
            wd = [P.sb('wd%d' % i, [128, NFT, 512], BF16, st) for i in range(2)]
            pys = [P.bank('py%d' % i, F32, st) for i in range(2)]
            conds = [2, b] if half == 0 else [b]
            G = {j: P.sb('G2_%d' % j, [128, D], F32, st) for j in conds}
            for j in conds:
                self.load_gate(G[j], 'G2_%d' % j, 0, j, 2)
            xt = [P.sb('fx%d' % i, [128, 512], F32, st) for i in range(2)]
            tmp = [P.sb('ft%d' % i, [128, 512], F32, st) for i in range(2)]
            xo = [P.sb('fo%d' % i, [128, 512], F32, st) for i in range(2)]
            wg_src = I['w_ff_gate'].rearrange("(c p) f -> p c f", p=128)
            wu_src = I['w_ff_up'].rearrange("(c p) f -> p c f", p=128)
            wd_src = I['w_ff_down'].rearrange("(f p) d -> p f d", p=128)
            blks = [(0, 512), (512, 512), (1024, 128)]
            ctr = [0, 0]
            _swiglu_gate_up(self, P, hT, TB, blks, wg_src, wu_src, list(range(NFT)), act, 0, wbufs, pgs, pus, sgs, ctr)
            akeys = ['act%d' % f for f in range(NFT)]
            k = 0
            for dh in range(2):
                for q4 in range(4):
                    P.dma('pool', wd[dh][:, q4 * 7:(q4 + 1) * 7, :], wd_src[:, q4 * 7:(q4 + 1) * 7, dh * 512:(dh + 1) * 512],
                          writes=['wd%d_%d' % (dh, q4)])
                for t in range(NTB):
                    i2 = k % 2; k += 1
                    rows = slice(r0 + t * 128, r0 + (t + 1) * 128)
                    cs = slice(dh * 512, (dh + 1) * 512)
                    P.dma('sp', xt[i2][:], self.resA[b, rows, cs], writes=['fx%d' % i2])
                    for f in range(NFT):
                        P.mm(pys[i2][:], act[:, f, t * 128:(t + 1) * 128], wd[dh][:, f, :], start=(f == 0), stop=(f == NFT - 1),
                             r=[akeys[f], 'wd%d_%d' % (dh, f // 7)], w=['py%d' % i2])
                    j = 2 if (half == 0 and t < 2) else b
                    P.tt('dve', tmp[i2][:], pys[i2][:], G[j][:, cs], ALU.mult, r=['py%d' % i2, 'G2_%d' % j], w=['ft%d' % i2])
                    P.tt('pool', xo[i2][:], tmp[i2][:], xt[i2][:], ALU.add, r=['ft%d' % i2, 'fx%d' % i2], w=['fo%d' % i2])
                    P.dma('pool', self.resB[b, rows, cs], xo[i2][:], reads=['fo%d' % i2], writes=['resB'])
            P.end_phase()


def _na_pos(d0):
    return (d0 + 7) // 2 if d0 % 2 != 0 else 7 + (d0 + 6) // 2


def _layer1_mixer(self, b):
    P, I = self.P, self.I
    with ExitStack() as st0:
        oT = P.sb('oT1', [128, 8, SEQ], BF16, st0)
        with ExitStack() as st1:
            hT = P.sb('hT3', [128, 8, TOT], BF16, st1)
            with ExitStack() as stn:
                self.norm_mod(stn, 'n3', lambda t: self.resB[b, t * 128:(t + 1) * 128, :], NT,
                              lambda t: 2 if t < 2 else b, 1, 0, hT, 'hT3')
                P.end_phase()
            bias_sb = P.sb('bias_sb', [128, 16, 896], BF16, st1)
            for h in range(16):
                P.dma('pool', bias_sb[:, h, :], I['nabias'][h], writes=['bias%d' % h])
            wsrc = I['w_in_c'].rearrange("(c p) f -> p c f", p=128)
            for g in range(4):
                with ExitStack() as sg:
                    wq = P.sb('wq', [128, 8, 256], BF16, sg)
                    wk = P.sb('wk', [128, 8, 256], BF16, sg)
                    wv = P.sb('wv', [128, 8, 256], BF16, sg)
                    P.dma('pool', wq[:], wsrc[:, :, g * 256:(g + 1) * 256], writes=['wq'])
                    P.dma('pool', wk[:], wsrc[:, :, D + g * 256:D + (g + 1) * 256], writes=['wk'])
                    P.dma('pool', wv[:], wsrc[:, :, 2 * D + g * 256:2 * D + (g + 1) * 256], writes=['wv'])
                    qT = P.sb('qT1', [128, 2, SEQ], BF16, sg)
                    kT = P.sb('kT1', [128, 2, TOT], BF16, sg)
                    v_e = P.sb('v_e', [128, 16, 4, 65], BF16, sg)
                    v_o = P.sb('v_o', [128, 15, 4, 65], BF16, sg)
                    v_c = P.sb('v_c', [128, 2, 4, 65], BF16, sg)
                    o_g = P.sb('o_g', [64, 32, 256], BF16, sg)
                    pctx = [P.sb('pctx%d' % i, [128, 2, SEQ], BF16, sg) for i in range(2)]
                    ploc = [P.sb('ploc%d' % i, [128, 256], BF16, sg) for i in range(3)]
                    rc = P.sb('rc1', [64, 2], F32, sg)
                    pp_ = [P.bank('pp%d' % i, F32, sg) for i in range(3)]
                    psl = [P.bank('psl%d' % i, F32, sg) for i in range(2)]
                    pov = [P.bank('pov%d' % i, F32, sg) for i in range(2)]
                    P.memset('dve', v_e[:], 1.0, w=['v_e'])
                    P.memset('dve', v_o[:], 1.0, w=['v_o'])
                    P.memset('dve', v_c[:], 1.0, w=['v_c'])
                    ip = 0
                    for pp in range(2):
                        for blk in range(4):
                            p_ = pp_[ip % 3]; pk = 'pp%d' % (ip % 3); ip += 1
                            for c in range(8):
                                P.mm(p_[:], wq[:, c, pp * 128:(pp + 1) * 128], hT[:, c, NCTX + blk * 512:NCTX + (blk + 1) * 512],
                                     start=(c == 0), stop=(c == 7), r=['wq'], w=[pk])
                            P.act(qT[:, pp, blk * 512:(blk + 1) * 512], p_[:], AF.Identity, r=[pk], w=['qT1'], scale=0.125)
                        for (t0, n) in [(0, 256), (256, 512), (768, 512), (1280, 512), (1792, 512)]:
                            p_ = pp_[ip % 3]; pk = 'pp%d' % (ip % 3); ip += 1
                            for c in range(8):
                                P.mm(p_[:, 0:n], wk[:, c, pp * 128:(pp + 1) * 128], hT[:, c, t0:t0 + n],
                                     start=(c == 0), stop=(c == 7), r=['wk'], w=[pk])
                            P.copy('dve', kT[:, pp, t0:t0 + n], p_[:, 0:n], r=[pk], w=['kT1'])
                    vjobs = [(t * 128, v_c[:, t, :, 0:64], 'v_c') for t in range(2)]
                    vjobs += [(NCTX + t * 128, v_e[:, t, :, 0:64], 'v_e') for t in range(16)]
                    vjobs += [(NCTX + 64 + t * 128, v_o[:, t, :, 0:64], 'v_o') for t in range(15)]
                    for (t0, dst, dk) in vjobs:
                        p_ = pp_[ip % 3]; pk = 'pp%d' % (ip % 3); ip += 1
                        for c in range(8):
                            P.mm(p_[:, 0:256], hT[:, c, t0:t0 + 128], wv[:, c, :], start=(c == 0), stop=(c == 7), r=['wv'], w=[pk])
                        P.copy('act' if ip % 2 else 'dve', dst, p_[:, 0:256].rearrange("p (h d) -> p h d", d=64), r=[pk], w=[dk])
                    isl = 0; iov = 0; ipl = 0
                    for hh in range(4):
                        h = 4 * g + hh
                        pp = hh // 2
                        ps_ = slice((hh % 2) * 64, (hh % 2) * 64 + 64)
                        pc = pctx[hh % 2]; pck = 'pctx%d' % (hh % 2)
                        for ct in range(2):
                            for blk in range(4):
                                p_ = pp_[ip % 3]; pk = 'pp%d' % (ip % 3); ip += 1
                                P.mm(p_[:], kT[ps_, pp, ct * 128:(ct + 1) * 128], qT[ps_, pp, blk * 512:(blk + 1) * 512],
                                     r=['kT1', 'qT1'], w=[pk])
                                P.act(pc[:, ct, blk * 512:(blk + 1) * 512], p_[:], AF.Exp, r=[pk], w=[pck])
                        cnt = {'isl': isl, 'ipl': ipl, 'iov': iov}

                        def s_part(r, h=h, pp=pp, ps_=ps_, cnt=cnt):
                            rs = min(max(r - 4, 0), 24)
                            pos = _na_pos(rs - r)
                            sl = psl[cnt['isl'] % 2]; slk = 'psl%d' % (cnt['isl'] % 2); cnt['isl'] += 1
                            P.mm(sl[:, 0:256], self.ident_b[:], bias_sb[:, h, pos * 64:(pos + 4) * 64], start=True, stop=False,
                                 r=['bias%d' % h, 'ident_b'], w=[slk], inc=False)
                            for j in range(4):
                                kt0 = NCTX + (rs + 2 * j) * 64
                                P.mm(sl[:, j * 64:(j + 1) * 64], kT[ps_, pp, kt0:kt0 + 128], qT[ps_, pp, r * 64:(r + 1) * 64],
                                     start=False, stop=True, r=['kT1', 'qT1'], w=[slk], inc=(j == 3))
                            pl = ploc[cnt['ipl'] % 3]; plk = 'ploc%d' % (cnt['ipl'] % 3); cnt['ipl'] += 1
                            P.act(pl[:], sl[:, 0:256], AF.Exp, r=[slk], w=[plk])
                            return (rs, pl, plk)

                        def pv_part(r, st_, hh=hh, pc=pc, pck=pck, cnt=cnt):
                            rs, pl, plk = st_
                            iov_ = cnt['iov']
                            ov = pov[iov_ % 2]; ovk = 'pov%d' % (iov_ % 2); rk = 'rc1_%d' % (iov_ % 2)
                            for j in range(4):
                                row = rs + 2 * j
                                vt = v_e[:, row // 2, hh, :] if rs % 2 == 0 else v_o[:, (row - 1) // 2, hh, :]
                                P.mm(ov[0:64, 0:65], pl[:, j * 64:(j + 1) * 64], vt, start=(j == 0), stop=False,
                                     r=[plk, 'v_e', 'v_o'], w=[ovk], inc=False)
                            for ct in range(2):
                                P.mm(ov[0:64, 0:65], pc[:, ct, r * 64:(r + 1) * 64], v_c[:, ct, hh, :], start=False, stop=(ct == 1),
                                     r=[pck, 'v_c'], w=[ovk], inc=(ct == 1))
                            P.recip(rc[:, iov_ % 2:iov_ % 2 + 1], ov[0:64, 64:65], r=[ovk], w=[rk])
                            P.ts('dve', o_g[:, r, hh * 64:(hh + 1) * 64], ov[0:64, 0:64], rc[:, iov_ % 2:iov_ % 2 + 1], ALU.mult,
                                 r=[ovk, rk], w=['o_g%d' % r])
                            cnt['iov'] += 1

                        st_ = s_part(0)
                        for r in range(32):
                            nxt = s_part(r + 1) if r + 1 < 32 else None
                            pv_part(r, st_)
                            st_ = nxt
                        isl, ipl, iov = cnt['isl'], cnt['ipl'], cnt['iov']
                    ptb_full = self._ptb_bank
                    ptv = ptb_full[:, :].rearrange("p (c r q) -> p c r q", c=2, r=8)
                    for r8 in range(4):
                        for cc in range(2):
                            for rr in range(8):
                                r = r8 * 8 + rr
                                P.tr(ptv[:, cc, rr, :], o_g[:, r, cc * 128:(cc + 1) * 128], self.ident_b[0:64, 0:64],
                                     r=['o_g%d' % r, 'ident_b'], w=['ptb'], inc=(cc == 1 and rr == 7))
                        P.copy('act', oT[:, 2 * g:2 * g + 2, r8 * 512:(r8 + 1) * 512],
                               ptb_full[:, :].rearrange("p (c n) -> p c n", c=2), r=['ptb'], w=['oT1'])
                    P.end_phase()
        with ExitStack() as st2:
            wo = P.sb('wo1', [128, 8, D], BF16, st2)
            wv_ = I['w_out_c'].rearrange("(c p) f -> p c f", p=128)
            for c in range(8):
                P.dma('pool', wo[:, c, :], wv_[:, c, :], writes=['wo1_%d' % c])
            G = P.sb('G1b', [128, D], F32, st2)
            self.load_gate(G, 'G1b', 1, b, 1)
            self.proj_residual(st2, 'o1', oT, lambda t: [], 16, wo, ['wo1_%d' % c for c in range(8)],
                               lambda t: self.resB[b, NCTX + t * 128:NCTX + (t + 1) * 128, :],
                               lambda t: (G, 'G1b'),
                               lambda t: self.resC[b, t * 128:(t + 1) * 128, :], 'resC')
            P.end_phase()


def _layer1_moe(self, b):
    P, I = self.P, self.I
    NTL = 16
    with ExitStack() as st0:
        comb = P.sb('comb', [128, NTL, NEXP], F32, st0)
        y_acc = P.sb('y_acc', [128, NTL, D], F32, st0)
        with ExitStack() as stA:
            hT = P.sb('hT4', [128, 8, SEQ], BF16, stA)
            with ExitStack() as stn:
                wr = P.sb('wr', [128, 8, NEXP], F32, stn)
                P.dma('sp', wr[:], I['w_router'][:, :, :], writes=['wr'])
                plog = P.bank('plog', F32, stn)
                rt_ = P.sb('rt_', [128, 8, 8], F32, stn)

                def want32(t, h32, key):
                    for c in range(8):
                        P.mm(plog[:, 0:NEXP], h32[:, c, :], wr[:, c, :], start=(c == 0), stop=(c == 7), r=[key, 'wr'], w=['plog'])
                    lg, top8, dd, ex, mk, num, den = [rt_[:, i, :] for i in range(7)]
                    P.copy('dve', lg, plog[:, 0:NEXP], r=['plog'], w=['r_lg'])
                    P.op('dve', lambda e: e.max(out=top8, in_=lg), ['r_lg'], ['r_top'])
                    P.ts('dve', dd, lg, top8[:, 0:1], ALU.subtract, r=['r_lg', 'r_top'], w=['r_dd'])
                    P.act(ex, dd, AF.Exp, r=['r_dd'], w=['r_ex'])
                    P.ts('dve', mk, lg, top8[:, 1:2], ALU.is_ge, r=['r_lg', 'r_top'], w=['r_mk'])
                    P.tt('dve', num, ex, mk, ALU.mult, r=['r_ex', 'r_mk'], w=['r_num'])
                    P.reduce(den[:, 0:1], num, ALU.add, r=['r_num'], w=['r_den'])
                    P.recip(den[:, 1:2], den[:, 0:1], r=['r_den'], w=['r_rden'])
                    P.ts('dve', comb[:, t, :], num, den[:, 1:2], ALU.mult, r=['r_num', 'r_rden'], w=['comb%d' % t])

                self.norm_mod(stn, 'n4', lambda t: self.resC[b, t * 128:(t + 1) * 128, :], NTL, lambda t: b, 1, 1, hT, 'hT4',
                              want32=want32)
                P.end_phase()
            if self.upto == 'l1_router':
                self.dump('comb', comb[:], [128, NTL, NEXP], F32)
                P.end_phase()
                return
            with ExitStack() as st:
                NQ = 7
                act = P.sb('mact', [128, NQ, SEQ], BF16, st)
                wbufs = [(P.sb('wg%d' % i, [128, 8, 256], BF16, st), P.sb('wu%d' % i, [128, 8, 256], BF16, st)) for i in range(4)]
                wd = [P.sb('mwd%d' % i, [128, D], BF16, st) for i in range(8)]
                pgs = [P.bank('pg%d' % i, F32, st) for i in range(2)]
                pus = [P.bank('pu%d' % i, F32, st) for i in range(2)]
                pys = [P.bank('py%d' % i, F32, st) for i in range(2)]
                sgs = [P.sb('sg%d' % i, [128, 512], F32, st) for i in range(2)]
                for t in range(NTL):
                    P.memset('pool' if t % 2 else 'dve', y_acc[:, t, :], 0.0, w=['y%d_0' % t, 'y%d_1' % t])
                blks = [(i * 512, 512) for i in range(4)]
                ctr = [0, 0]
                iw = 0; iy = 0
                import os
                nexp = int(os.environ.get('MOE_NEXP', NEXP))
                for e in range(nexp):
                    wg_src = I['w_moe_gate'][e].rearrange("(c p) f -> p c f", p=128)
                    wu_src = I['w_moe_up'][e].rearrange("(c p) f -> p c f", p=128)
                    for q in range(NFT // NQ):
                        fts = list(range(q * NQ, (q + 1) * NQ))
                        self._moe_gate_up(P, hT, blks, wg_src, wu_src, fts, act, wbufs, pgs, pus, sgs, ctr)
                        wks = []
                        for fl in range(NQ):
                            k = iw % 8; iw += 1
                            P.dma('pool', wd[k][:], I['w_moe_down'][e, (q * NQ + fl) * 128:(q * NQ + fl + 1) * 128, :], writes=['mwd%d' % k])
                            wks.append(k)
                        for t in range(NTL):
                            for dh in range(2):
                                i2 = iy % 2; iy += 1
                                for fl in range(NQ):
                                    P.mm(pys[i2][:], act[:, fl, t * 128:(t + 1) * 128], wd[wks[fl]][:, dh * 512:(dh + 1) * 512],
                                         start=(fl == 0), stop=(fl == NQ - 1), r=['act%d' % fl, 'mwd%d' % wks[fl]], w=['py%d' % i2])
                                yk = 'y%d_%d' % (t, dh)
                                ys = y_acc[:, t, dh * 512:(dh + 1) * 512]
                                P.stt(ys, pys[i2][:], comb[:, t, e:e + 1], ys, ALU.mult, ALU.add, r=['py%d' % i2, yk], w=[yk])
                P.end_phase()
        with ExitStack() as st:
            G2 = P.sb('G2f', [128, D], F32, st)
            gfin = P.sb('gfin', [128, D], F32, st)
            self.load_gate(G2, 'G2f', 1, b, 2)
            P.dma('sp', gfin[:], I['g_final'][0:1, :].partition_broadcast(128), writes=['gfin'])
            xc = [P.sb('xc%d' % i, [128, D], F32, st) for i in range(2)]
            tmp = P.sb('ftmp', [128, D], F32, st)
            xo = P.sb('fxo', [128, D], F32, st)
            junk = P.sb('fjunk', [128, D], BF16, st)
            ss = P.sb('fss', [128, 2], F32, st)
            ot = [P.sb('fot%d' % i, [128, D], F32, st) for i in range(2)]
            for t in range(NTL):
                i2 = t % 2
                P.dma('sp', xc[i2][:], self.resC[b, t * 128:(t + 1) * 128, :], writes=['xc%d' % i2])
                P.tt('dve', tmp[:], y_acc[:, t, :], G2[:], ALU.mult, r=['G2f'], w=['ftmp'])
                P.tt('pool', xo[:], tmp[:], xc[i2][:], ALU.add, r=['ftmp', 'xc%d' % i2], w=['fxo'])
                P.sumsq(junk[:], xo[:], ss[:, 0:1], r=['fxo'], w=['fjunk', 'fss0'])
                P.act(ss[:, 1:2], ss[:, 0:1], AF.Sqrt, r=['fss0', 'epsb'], w=['fss1'], scale=1.0 / D, bias=self.epsb[:, 0:1])
                P.recip(ss[:, 0:1], ss[:, 1:2], r=['fss1'], w=['fss0'])
                P.stt(ot[i2][:], xo[:], ss[:, 0:1], gfin[:], ALU.mult, ALU.mult, r=['fxo', 'fss0', 'gfin'], w=['fot%d' % i2])
                P.dma('pool', self.out[b, t * 128:(t + 1) * 128, :], ot[i2][:], reads=['fot%d' % i2], writes=['out'])
            P.end_phase()


def _moe_gate_up(self, P, hT, blks, wg_src, wu_src, f_tiles, act, wbufs, pgs, pus, sgs, ctr):
    i = 0
    while i < len(f_tiles):
        nf = min(2, len(f_tiles) - i)
        f0 = f_tiles[i]
        k = ctr[0] % len(wbufs); ctr[0] += 1
        wg, wu = wbufs[k]
        P.dma('pool', wg[:, :, 0:nf * 128], wg_src[:, :, f0 * 128:(f0 + nf) * 128], writes=['wg%d' % k])
        P.dma('pool', wu[:, :, 0:nf * 128], wu_src[:, :, f0 * 128:(f0 + nf) * 128], writes=['wu%d' % k])
        for fi in range(nf):
            for (t0, n) in blks:
                j = ctr[1] % 2; ctr[1] += 1
                for c in range(8):
                    P.mm(pgs[j][:, 0:n], wg[:, c, fi * 128:(fi + 1) * 128], hT[:, c, t0:t0 + n], start=(c == 0), stop=(c == 7),
                         r=['wg%d' % k], w=['pg%d' % j])
                for c in range(8):
                    P.mm(pus[j][:, 0:n], wu[:, c, fi * 128:(fi + 1) * 128], hT[:, c, t0:t0 + n], start=(c == 0), stop=(c == 7),
                         r=['wu%d' % k], w=['pu%d' % j])
                P.act(sgs[j][:, 0:n], pgs[j][:, 0:n], AF.Silu, r=['pg%d' % j], w=['sg%d' % j])
                P.tt('dve', act[:, i + fi, t0:t0 + n], sgs[j][:, 0:n], pus[j][:, 0:n], ALU.mult,
                     r=['sg%d' % j, 'pu%d' % j], w=['act%d' % (i + fi)])
        i += nf


def _layer1_moe_sparse(self):
    P, I = self.P, self.I
    NTT = 32
    nc = self.nc
    with ExitStack() as st0:
        m12 = P.sb('m12', [128, 2, NTT, 8], F32, st0)
        pos = P.sb('pos', [128, NTT, 8], F32, st0)
        g12 = P.sb('g12', [128, 2, NTT], F32, st0)
        run = P.sb('run', [128, 8], F32, st0)
        ek_i = P.sb('ek_i', [128, NKT], I32, st0)
        idxg = P.sb('idxg', [128, NKT, 32], I32, st0)
        idxd = P.sb('idxd', [128, NKT, NFT], I32, st0)
        with ExitStack() as stn:
            wr = P.sb('wr', [128, 8, NEXP], F32, stn)
            P.dma('sp', wr[:], I['w_router'][:, :, :], writes=['wr'])
            stri = P.sb('stri', [128, 128], F32, stn)
            P.dma('sp', stri[:], I['stri'][:, :], writes=['stri'])
            ones128 = P.sb('ones128', [128, 128], F32, stn)
            P.memset('dve', ones128[:], 1.0, w=['ones128'])
            P.memset('dve', run[:], 0.0, w=['run'])
            plog = P.bank('plog', F32, stn)
            ppos = P.bank('ppos', F32, stn)
            lg_all = P.sb('lg_all', [128, NTT, 8], F32, stn)
            Abc = P.sb('Abc', [128, D], F32, stn)
            Bbc = P.sb('Bbc', [128, D], F32, stn)
            gnb = P.sb('gnb', [128, D], F32, stn)
            htm = [P.sb('htm%d' % i, [128, D], F32, stn) for i in range(2)]
            hbf = [P.sb('hbf%d' % i, [128, D], BF16, stn) for i in range(2)]
            P.dma('sp', gnb[:], I['gn2_nat'][0:1, :].partition_broadcast(128), writes=['gnb'])
            for b in range(2):
                P.dma('sp', Abc[:], self.mod_d[1, b, 4:5, :].partition_broadcast(128), writes=['Abc'])
                P.dma('sp', Bbc[:], self.mod_d[1, b, 3:4, :].partition_broadcast(128), writes=['Bbc'])
                P.stt(Abc[:], Abc[:], 1.0, gnb[:], ALU.add, ALU.mult, r=['Abc', 'gnb'], w=['Abc'])

                def tm_cb(t, xn, xk, b=b):
                    tt = b * 16 + t
                    i2 = tt % 2
                    P.tt('pool', htm[i2][:], xn[:], Abc[:], ALU.mult, r=[xk, 'Abc'], w=['htm%d' % i2])
                    P.tt('pool', hbf[i2][:], htm[i2][:], Bbc[:], ALU.add, r=['htm%d' % i2, 'Bbc'], w=['hbf%d' % i2])

                def post_cb(t, b=b):
                    tt = b * 16 + t
                    i2 = tt % 2
                    P.dma('act', self.h_d[tt * 128:(tt + 1) * 128, :], hbf[i2][:], reads=['hbf%d' % i2], writes=['h_d'])

                def want32(t, h32, key, b=b):
                    tt = b * 16 + t
                    for c in range(8):
                        P.mm(plog[:, 0:NEXP], h32[:, c, :], wr[:, c, :], start=(c == 0), stop=(c == 7), r=[key, 'wr'], w=['plog'])
                    P.copy('dve', lg_all[:, tt, :], plog[:, 0:NEXP], r=['plog'], w=['lg_all'])

                self.norm_mod(stn, 'n4_%d' % b, lambda t, b=b: self.resC[b, t * 128:(t + 1) * 128, :], 16, lambda t, b=b: b, 1, 1,
                              None, 'hT4', want32=want32, tm_cb=tm_cb, post_cb=post_cb) if b == 0 else \
                    self.norm_mod(stn, 'n5_%d' % b, lambda t, b=b: self.resC[b, t * 128:(t + 1) * 128, :], 16, lambda t, b=b: b, 1, 1,
                                  None, 'hT4', want32=want32, tm_cb=tm_cb, post_cb=post_cb)
            lg2 = P.sb('lg2', [128, NTT, 8], F32, stn)
            msum = P.sb('msum', [128, NTT, 8], F32, stn)
            tq = P.sb('tq', [128, 5, NTT], F32, stn)
            pp_sb = P.sb('pp_sb', [128, NTT, 16], F32, stn)
            runb = P.sb('runb', [128, NTT + 1, 8], F32, stn)
            pposv = ppos[:, :].rearrange("p (t w) -> p t w", w=16)
            P.reduce(tq[:, 0, :], lg_all[:], ALU.max, r=['lg_all'], w=['tq0'])
            P.tt('dve', m12[:, 0, :, :], lg_all[:], bc(tq[:, 0, :].unsqueeze(2), [128, NTT, 8]), ALU.is_equal, r=['lg_all', 'tq0'], w=['m12a'])
            P.stt(lg2[:], m12[:, 0, :, :], -1.0e30, lg_all[:], ALU.mult, ALU.add, r=['m12a', 'lg_all'], w=['lg2'])
            P.reduce(tq[:, 1, :], lg2[:], ALU.max, r=['lg2'], w=['tq1'])
            P.tt('dve', m12[:, 1, :, :], lg2[:], bc(tq[:, 1, :].unsqueeze(2), [128, NTT, 8]), ALU.is_equal, r=['lg2', 'tq1'], w=['m12b'])
            P.tt('dve', tq[:, 2, :], tq[:, 1, :], tq[:, 0, :], ALU.subtract, r=['tq0', 'tq1'], w=['tq2'])
            P.act(tq[:, 3, :], tq[:, 2, :], AF.Exp, r=['tq2'], w=['tq3'])
            P.ts('dve', tq[:, 4, :], tq[:, 3, :], 1.0, ALU.add, r=['tq3'], w=['tq4'])
            P.recip(g12[:, 0, :], tq[:, 4, :], r=['tq4'], w=['g12a'])
            P.tt('dve', g12[:, 1, :], tq[:, 3, :], g12[:, 0, :], ALU.mult, r=['tq3', 'g12a'], w=['g12b'])
            P.tt('dve', msum[:], m12[:, 0, :, :], m12[:, 1, :, :], ALU.add, r=['m12a', 'm12b'], w=['msum'])
            for tt in range(NTT):
                P.mm(pposv[:, tt, 0:8], stri[:], msum[:, tt, :], r=['stri', 'msum'], w=['ppos'], inc=False)
                P.mm(pposv[:, tt, 8:16], ones128[:], msum[:, tt, :], r=['ones128', 'msum'], w=['ppos'], inc=(tt == NTT - 1))
            P.copy('dve', pp_sb[:], pposv, r=['ppos'], w=['pp_sb'])
            P.memset('dve', runb[:, 0, :], 0.0, w=['runb'])
            for tt in range(NTT):
                P.tt('dve', runb[:, tt + 1, :], runb[:, tt, :], pp_sb[:, tt, 8:16], ALU.add, r=['runb', 'pp_sb'], w=['runb'])
            P.tt('dve', pos[:], pp_sb[:, :, 0:8], runb[:, 0:NTT, :], ALU.add, r=['pp_sb', 'runb'], w=['pos'])
            P.copy('dve', run[:], runb[:, NTT, :], r=['runb'], w=['run'])
            thr = P.sb('thr', [128, 8, 8], F32, stn)
            kv = P.sb('kv', [128, NKT, 8], F32, stn)
            tokid = P.sb('tokid', [128, NTT], F32, stn)
            dflt = P.sb('dflt', [128, NSLOT // 128, 4], F32, stn)
            P.dma('sp', thr[:], I['thr'].rearrange("p (e m) -> p e m", m=8), writes=['thr'])
            P.dma('sp', kv[:], I['kv'].rearrange("p (k e) -> p k e", e=8), writes=['kv'])
            P.dma('sp', tokid[:], I['tokid'][:, :], writes=['tokid'])
            P.dma('sp', dflt[:], I['dflt'].rearrange("p (n w) -> p n w", w=4), writes=['dflt'])
            P.dma('sp', self.tab.rearrange("(p n) w -> p n w", p=128), dflt[:], reads=['dflt'], writes=['tab0'])
            cmp8 = P.sb('cmp8', [128, 8, 8], F32, stn)
            tl = P.sb('tl', [128, 4, 8], F32, stn)
            cmpk = P.sb('cmpk', [128, NKT, 8], F32, stn)
            ekf = P.sb('ekf', [128, NKT], F32, stn)
            big = P.sb('big', [128, NTT, 8], F32, stn)
            slf = P.sb('slf', [128, 2 * NTT], F32, stn)
            sli = P.sb('sli', [128, 2 * NTT], I32, stn)
            rowd = P.sb('rowd', [128, 2, NTT, 4], F32, stn)
            P.tt('dve', cmp8[:], thr[:], bc(run[:].unsqueeze(2), [128, 8, 8]), ALU.is_lt, r=['thr', 'run'], w=['cmp8'])
            P.reduce(tl[:, 0, :], cmp8[:], ALU.add, r=['cmp8'], w=['tl0'])
            P.copy('dve', tl[:, 1, 0:1], tl[:, 0, 0:1], r=['tl0'], w=['tl1'])
            for e_ in range(1, 8):
                P.tt('dve', tl[:, 1, e_:e_ + 1], tl[:, 1, e_ - 1:e_], tl[:, 0, e_:e_ + 1], ALU.add, r=['tl0', 'tl1'], w=['tl1'])
            P.tt('dve', tl[:, 2, :], tl[:, 1, :], tl[:, 0, :], ALU.subtract, r=['tl0', 'tl1'], w=['tl2'])
            P.ts('dve', tl[:, 2, :], tl[:, 2, :], float(SLOT_T), ALU.mult, r=['tl2'], w=['tl2'])
            P.tt('dve', cmpk[:], kv[:], bc(tl[:, 1, :].unsqueeze(1), [128, NKT, 8]), ALU.is_ge, r=['kv', 'tl1'], w=['cmpk'])
            P.reduce(ekf[:], cmpk[:], ALU.add, r=['cmpk'], w=['ekf'])
            P.ts('dve', ekf[:], ekf[:], 7.0, ALU.min, r=['ekf'], w=['ekf'])
            P.copy('dve', ek_i[:], ekf[:], r=['ekf'], w=['ek_i'])
            rowc4 = P.sb('rowc4', [128, 32], F32, stn)
            rowf = P.sb('rowf', [128, NFT], F32, stn)
            P.dma('sp', rowc4[:], I['rowc4'][:, :], writes=['rowc4'])
            P.dma('sp', rowf[:], I['rowf'][:, :], writes=['rowf'])
            idxg_f = P.sb('idxg_f', [128, NKT, 32], F32, stn)
            idxd_f = P.sb('idxd_f', [128, NKT, NFT], F32, stn)
            P.stt(idxg_f[:], bc(ekf[:].unsqueeze(2), [128, NKT, 32]), 4096.0, bc(rowc4[:].unsqueeze(1), [128, NKT, 32]),
                  ALU.mult, ALU.add, r=['ekf', 'rowc4'], w=['idxg_f'])
            P.stt(idxd_f[:], bc(ekf[:].unsqueeze(2), [128, NKT, NFT]), float(DFF), bc(rowf[:].unsqueeze(1), [128, NKT, NFT]),
                  ALU.mult, ALU.add, r=['ekf', 'rowf'], w=['idxd_f'])
            P.copy('dve', idxg[:], idxg_f[:], r=['idxg_f'], w=['idxg'])
            P.copy('dve', idxd[:], idxd_f[:], r=['idxd_f'], w=['idxd'])
            for r_ in range(2):
                P.tt('dve', big[:], pos[:], bc(tl[:, 2, :].unsqueeze(1), [128, NTT, 8]), ALU.add, r=['pos', 'tl2'], w=['big'])
                P.tt('dve', big[:], big[:], m12[:, r_, :, :], ALU.mult, r=['big', 'm12a', 'm12b'], w=['big'])
                P.reduce(slf[:, r_ * NTT:(r_ + 1) * NTT], big[:], ALU.add, r=['big'], w=['slf'])
                P.copy('dve', rowd[:, r_, :, 0], tokid[:], r=['tokid'], w=['rowd'])
                P.ts('dve', rowd[:, r_, :, 1], tokid[:], float(r_ * 2 * SEQ), ALU.add, r=['tokid'], w=['rowd'])
                P.copy('dve', rowd[:, r_, :, 2], g12[:, r_, :], r=['g12a', 'g12b'], w=['rowd'])
                P.memset('dve', rowd[:, r_, :, 3], 0.0, w=['rowd'])
            P.copy('dve', sli[:], slf[:], r=['slf'], w=['sli'])
            tab = self.tab
            for r_ in range(2):
                for tt in range(NTT):
                    j = r_ * NTT + tt

                    def sc(e, j=j, r_=r_, tt=tt):
                        return e.indirect_dma_start(out=tab[:, :], out_offset=bass.IndirectOffsetOnAxis(ap=sli[:, j:j + 1], axis=0),
                                                    in_=rowd[:, r_, tt, :], in_offset=None)
                    P.dma_raw('pool', sc, reads=['tab0', 'sli', 'rowd'], writes=['tab_s%d' % j])
            if self.debug:
                self.dump('ek_i', ek_i[:], [128, NKT], I32, reads=['ek_i'])
                self.dump('sli', sli[:], [128, 2 * NTT], I32, reads=['sli'])
                self.dump('run', run[:], [128, 8], F32, reads=['run'])
            P.end_phase()
        if self.upto == 'l1_router':
            return
        with ExitStack() as st:
            act = P.sb('mact', [128, NFT, SLOT_T], BF16, st)
            wbufs = [(P.sb('wg%d' % i, [128, 8, 896], BF16, st), P.sb('wu%d' % i, [128, 8, 896], BF16, st)) for i in range(2)]
            wd = [P.sb('mwd%d' % i, [128, 14, D], BF16, st) for i in range(2)]
            pgs = [P.bank('pg%d' % i, F32, st) for i in range(2)]
            pus = [P.bank('pu%d' % i, F32, st) for i in range(2)]
            pys = [P.bank('py%d' % i, F32, st) for i in range(2)]
            ptg = P.bank('ptg', BF16, st)
            ptgv = ptg[:, :].rearrange("p (c n) -> p c n", c=8)
            sgs = [P.sb('sg%d' % i, [128, 512], F32, st) for i in range(2)]
            tabt = [P.sb('tabt%d' % i, [128, 4, 4], F32, st) for i in range(2)]
            srci = [P.sb('srci%d' % i, [128, 4], I32, st) for i in range(2)]
            dsti = [P.sb('dsti%d' % i, [128, 4], I32, st) for i in range(2)]
            hg = P.sb('hg', [128, 4, D], BF16, st)
            hTg = [P.sb('hTg%d' % i, [128, 8, SLOT_T], BF16, st) for i in range(2)]
            ysb = P.sb('ysb', [128, 4, D], F32, st)
            h_d, y12 = self.h_d, self.y12
            wg4 = I['w_moe_gate'].rearrange("e r (q f) -> (e r q) f", q=4)
            wu4 = I['w_moe_up'].rearrange("e r (q f) -> (e r q) f", q=4)
            wd2 = I['w_moe_down'].rearrange("e r d -> (e r) d")
            ctr = [0, 0]
            iy = 0
            import os
            nkt = int(os.environ.get('MOE_NKT', NKT))
            pending = []

            def fetch(k):
                i2 = k % 2
                P.dma('sp', tabt[i2][:], self.tab[k * SLOT_T:(k + 1) * SLOT_T, :].rearrange("(j p) w -> p j w", p=128),
                      writes=['tabt%d' % i2])
                P.copy('dve', srci[i2][:], tabt[i2][:, :, 0], r=['tabt%d' % i2], w=['srci%d' % i2])
                P.copy('dve', dsti[i2][:], tabt[i2][:, :, 1], r=['tabt%d' % i2], w=['dsti%d' % i2])
                for j in range(4):
                    def ga(e, i2=i2, j=j):
                        return e.indirect_dma_start(out=hgs[i2][:, j, :], out_offset=None, in_=h_d[:, :],
                                                    in_offset=bass.IndirectOffsetOnAxis(ap=srci[i2][:, j:j + 1], axis=0))
                    P.dma_raw('pool', ga, reads=['srci%d' % i2], writes=['hg%d_%d' % (i2, j)])
                for j in range(4):
                    for c in range(8):
                        P.tr(ptgv[:, c, :], hgs[i2][:, j, c * 128:(c + 1) * 128], self.ident_b[:], r=['hg%d_%d' % (i2, j), 'ident_b'],
                             w=['ptg'], inc=(c == 7))
                    P.copy('act' if j % 2 else 'dve', hTg[i2][:, :, j * 128:(j + 1) * 128], ptgv, r=['ptg'], w=['hTg%d_%d' % (i2, j)])

            hgs = [hg, P.sb('hg_b', [128, 4, D], BF16, st)]
            fetch(0)
            for k in range(nkt):
                i2 = k % 2
                hkeys = ['hTg%d_%d' % (i2, j) for j in range(4)]
                for q in range(4):
                    kb = ctr[0] % 2; ctr[0] += 1
                    wg, wu = wbufs[kb]
                    for (wt, src4, nm) in ((wg, wg4, 'wg'), (wu, wu4, 'wu')):
                        for c in range(8):
                            def gw(e, wt=wt, src4=src4, k=k, c=c, q=q):
                                return e.indirect_dma_start(out=wt[:, c, :], out_offset=None, in_=src4[:, :],
                                                            in_offset=bass.IndirectOffsetOnAxis(ap=idxg[:, k, c * 4 + q:c * 4 + q + 1], axis=0))
                            P.dma_raw('pool', gw, reads=['idxg'], writes=['%s%d_%d' % (nm, kb, c)])
                    gk = ['wg%d_%d' % (kb, c) for c in range(8)]
                    uk = ['wu%d_%d' % (kb, c) for c in range(8)]
                    if q == 1:
                        for fn_, rd_, wr_ in pending:
                            P.dma_raw('pool', fn_, reads=rd_, writes=wr_)
                        pending = []
                    for fi in range(7):
                        f = q * 7 + fi
                        jj = ctr[1] % 2; ctr[1] += 1
                        for c in range(8):
                            P.mm(pgs[jj][:], wg[:, c, fi * 128:(fi + 1) * 128], hTg[i2][:, c, :], start=(c == 0), stop=(c == 7),
                                 r=[gk[c]] + hkeys, w=['pg%d' % jj])
                        for c in range(8):
                            P.mm(pus[jj][:], wu[:, c, fi * 128:(fi + 1) * 128], hTg[i2][:, c, :], start=(c == 0), stop=(c == 7),
                                 r=[uk[c]] + hkeys, w=['pu%d' % jj])
                        P.act(sgs[jj][:], pgs[jj][:], AF.Silu, r=['pg%d' % jj], w=['sg%d' % jj])
                        P.tt('dve', act[:, f, :], sgs[jj][:], pus[jj][:], ALU.mult, r=['sg%d' % jj, 'pu%d' % jj], w=['act%d' % f])
                if k + 1 < nkt:
                    fetch(k + 1)
                for fh in range(2):
                    for fl in range(14):
                        f = fh * 14 + fl
                        def gd(e, k=k, f=f, fh=fh, fl=fl):
                            return e.indirect_dma_start(out=wd[fh][:, fl, :], out_offset=None, in_=wd2[:, :],
                                                        in_offset=bass.IndirectOffsetOnAxis(ap=idxd[:, k, f:f + 1], axis=0))
                        P.dma_raw('pool', gd, reads=['idxd'], writes=['mwd%d_%d' % (fh, fl)])
                    for j in range(4):
                        for dh in range(2):
                            ip = iy % 2; iy += 1
                            for fl in range(14):
                                P.mm(pys[ip][:], act[:, fh * 14 + fl, j * 128:(j + 1) * 128], wd[fh][:, fl, dh * 512:(dh + 1) * 512],
                                     start=(fl == 0), stop=(fl == 13), r=['act%d' % (fh * 14 + fl), 'mwd%d_%d' % (fh, fl)], w=['py%d' % ip])
                            ys = ysb[:, j, dh * 512:(dh + 1) * 512]
                            yk = 'ysb%d_%d' % (j, dh)
                            if fh == 0:
                                P.ts('dve', ys, pys[ip][:], tabt[i2][:, j, 2:3], ALU.mult, r=['py%d' % ip, 'tabt%d' % i2], w=[yk])
                            else:
                                P.stt(ys, pys[ip][:], tabt[i2][:, j, 2:3], ys, ALU.mult, ALU.add, r=['py%d' % ip, 'tabt%d' % i2, yk], w=[yk])
                for j in range(4):
                    def scy(e, i2=i2, j=j):
                        return e.indirect_dma_start(out=y12[:, :], out_offset=bass.IndirectOffsetOnAxis(ap=dsti[i2][:, j:j + 1], axis=0),
                                                    in_=ysb[:, j, :], in_offset=None)
                    pending.append((scy, ['dsti%d' % i2, 'ysb%d_0' % j, 'ysb%d_1' % j], ['y12_%d_%d' % (k, j)]))
            for fn_, rd_, wr_ in pending:
                P.dma_raw('pool', fn_, reads=rd_, writes=wr_)
            P.end_phase()
        if self.upto == 'l1_experts':
            return
        with ExitStack() as st:
            G2 = [P.sb('G2f%d' % b, [128, D], F32, st) for b in range(2)]
            gfin = P.sb('gfin', [128, D], F32, st)
            for b in range(2):
                self.load_gate(G2[b], 'G2f%d' % b, 1, b, 2)
            P.dma('sp', gfin[:], I['g_final'][0:1, :].partition_broadcast(128), writes=['gfin'])
            xc = [P.sb('xc%d' % i, [128, D], F32, st) for i in range(2)]
            y1 = [P.sb('y1_%d' % i, [128, D], F32, st) for i in range(2)]
            y2 = [P.sb('y2_%d' % i, [128, D], F32, st) for i in range(2)]
            tmp = [P.sb('ftmp%d' % i, [128, D], F32, st) for i in range(2)]
            xo = [P.sb('fxo%d' % i, [128, D], F32, st) for i in range(2)]
            junk = P.sb('fjunk', [128, D], BF16, st)
            ss = P.sb('fss', [128, 2, 2], F32, st)
            ot = [P.sb('fot%d' % i, [128, D], F32, st) for i in range(2)]
            for tt in range(NTT):
                b, t = tt // 16, tt % 16
                i2 = tt % 2
                P.dma('sp', xc[i2][:], self.resC[b, t * 128:(t + 1) * 128, :], writes=['xc%d' % i2])
                P.dma('sp', y1[i2][:], self.y12[tt * 128:(tt + 1) * 128, :], writes=['y1_%d' % i2])
                P.dma('sp', y2[i2][:], self.y12[2 * SEQ + tt * 128:2 * SEQ + (tt + 1) * 128, :], writes=['y2_%d' % i2])
                P.tt('dve', y1[i2][:], y1[i2][:], y2[i2][:], ALU.add, r=['y1_%d' % i2, 'y2_%d' % i2], w=['y1_%d' % i2])
                P.tt('dve', tmp[i2][:], y1[i2][:], G2[b][:], ALU.mult, r=['y1_%d' % i2, 'G2f%d' % b], w=['ftmp%d' % i2])
                P.tt('pool', xo[i2][:], tmp[i2][:], xc[i2][:], ALU.add, r=['ftmp%d' % i2, 'xc%d' % i2], w=['fxo%d' % i2])
                P.sumsq(junk[:], xo[i2][:], ss[:, i2, 0:1], r=['fxo%d' % i2], w=['fjunk', 'fss0_%d' % i2])
                P.act(ss[:, i2, 1:2], ss[:, i2, 0:1], AF.Sqrt, r=['fss0_%d' % i2, 'epsb'], w=['fss1_%d' % i2], scale=1.0 / D, bias=self.epsb[:, 0:1])
                P.recip(ss[:, i2, 0:1], ss[:, i2, 1:2], r=['fss1_%d' % i2], w=['fss0_%d' % i2])
                P.stt(ot[i2][:], xo[i2][:], ss[:, i2, 0:1], gfin[:], ALU.mult, ALU.mult, r=['fxo%d' % i2, 'fss0_%d' % i2, 'gfin'], w=['fot%d' % i2])
                P.dma('act', self.out[b, t * 128:(t + 1) * 128, :], ot[i2][:], reads=['fot%d' % i2], writes=['out'])
            P.end_phase()


Builder.layer1_moe_sparse = _layer1_moe_sparse
Builder.l0_ffn = _l0_ffn
Builder.layer1_mixer = _layer1_mixer
Builder.layer1_moe = _layer1_moe
Builder._moe_gate_up = _moe_gate_up


def build(upto='all', debug=False):
    import os
    phases = {'all': ('l0a', 'l0f', 'l1a', 'l1f'), 'l0f_only': ('l0f',), 'l1a_only': ('l1a',), 'l1f_only': ('l1f',),
              'l1_router': ('l1f',), 'l1_experts': ('l1f',), 'l0': ('l0a', 'l0f'), 'mod': ()}.get(upto, ('l0a',))
    preload = {'l0f_only': ['resA'], 'l1a_only': ['resB'], 'l1f_only': ['resC'], 'l1_router': ['resC'], 'l1_experts': ['resC']}.get(upto, [])
    B = Builder(upto, debug, preload)
    B.declare()
    B.consts()
    B._ptb_bank = B.P.bank('ptb', BF16)
    B._ptb = B._ptb_bank[:, 0:512].rearrange("p (h d) -> p h d", d=128)
    B.phase_mod()
    nb = int(os.environ.get('NB', 2))
    if 'l0a' in phases:
        for b in range(nb):
            B.layer0_mixer(b)
            if upto in ('l0a_b0', 'l0_norm', 'l0_inproj', 'l0_gqa', 'l0_gla'):
                break
    if 'l0f' in phases:
        for b in range(nb):
            for half in range(2):
                B.l0_ffn(b, half)
    if 'l1a' in phases:
        for b in range(nb):
            B.layer1_mixer(b)
    if 'l1f' in phases:
        if os.environ.get('MOE_DENSE'):
            for b in range(nb):
                B.layer1_moe(b)
        else:
            B.layer1_moe_sparse()
    B.P.end_phase()
    return B


def _rope_tables():
    t = np.arange(SEQ, dtype=np.int32)
    row = (t // 64).astype(np.float32)
    col = (t % 64).astype(np.float32)
    n_axis = 16
    inv = np.power(np.float32(10000.0), -np.arange(n_axis, dtype=np.float32) / np.float32(n_axis)).astype(np.float32)
    ang = np.concatenate([row[:, None] * inv, col[:, None] * inv], axis=-1).astype(np.float32)
    cos = np.cos(ang).astype(np.float32)
    sin = np.sin(ang).astype(np.float32)
    C = np.repeat(cos, 2, axis=1)
    S = np.stack([-sin, sin], axis=-1).reshape(SEQ, 64)
    C = C.reshape(16, 128, 64).transpose(1, 0, 2)
    S = S.reshape(16, 128, 64).transpose(1, 0, 2)
    return np.ascontiguousarray(C), np.ascontiguousarray(S)


def _na_bias(rpb):
    NEG = np.float32(-30000.0)
    cols = np.arange(64)
    col_start = np.clip(cols - 8, 0, 48)
    in_win = (cols[None, :] >= col_start[:, None]) & (cols[None, :] < col_start[:, None] + 16)
    col_idx = np.clip(cols[None, :] - cols[:, None] + 15, 0, 30)
    dlist = list(range(-7, 7, 2)) + list(range(-6, 8, 2))
    out = np.empty((16, 128, 14, 64), np.float32)
    for di, d in enumerate(dlist):
        for i in range(2):
            dr = d + i + 7
            dr_c = min(max(dr, 0), 14)
            blk = rpb[:, dr_c][:, col_idx]
            blk = np.where(in_win[None], blk, NEG)
            out[:, 64 * i:64 * (i + 1), di, :] = blk.transpose(0, 2, 1)
    return np.ascontiguousarray(out.reshape(16, 128, 14 * 64))


def prep_inputs(inp, ncores=NCORES):
    f = lambda a: np.ascontiguousarray(np.asarray(a, dtype=np.float32))
    shared = {}
    shared['w_mod'] = f(inp['w_mod']); shared['b_mod'] = f(inp['b_mod'])
    gn = np.stack([f(inp['g_norm1']), f(inp['g_norm2'])], axis=1)
    shared['gnT'] = np.ascontiguousarray(gn.reshape(2, 2, 8, 128).transpose(3, 0, 1, 2))
    shared['w_in_ab'] = f(inp['w_in_ab'][0]); shared['g_q'] = f(inp['g_q']); shared['g_k'] = f(inp['g_k'])
    shared['w2f'] = f(np.concatenate([inp['w_a2_f'][0], inp['b_a_f'][0][None]], 0))
    shared['w2b'] = f(np.concatenate([inp['w_a2_b'][0], inp['b_a_b'][0][None]], 0))
    shared['g_gla'] = f(inp['g_gla'])
    shared['w_out_ab'] = f(inp['w_out_ab'][0])
    shared['w_ff_gate'] = f(inp['w_ff_gate'][0]); shared['w_ff_up'] = f(inp['w_ff_up'][0]); shared['w_ff_down'] = f(inp['w_ff_down'][0])
    shared['w_in_c'] = f(inp['w_in_c'][0]); shared['nabias'] = _na_bias(f(inp['rpb_c'][0])); shared['w_out_c'] = f(inp['w_out_c'][0])
    shared['w_router'] = np.ascontiguousarray(f(inp['w_router'][0]).reshape(8, 128, NEXP).transpose(1, 0, 2))
    shared['w_moe_gate'] = f(inp['w_moe_gate'][0]); shared['w_moe_up'] = f(inp['w_moe_up'][0]); shared['w_moe_down'] = f(inp['w_moe_down'][0])
    shared['g_final'] = f(inp['g_final']).reshape(1, D)
    shared['ident'] = np.eye(128, dtype=np.float32)
    idx = np.arange(128)
    shared['tri_f'] = (idx[:, None] <= idx[None, :]).astype(np.float32)
    shared['tri_b'] = (idx[:, None] >= idx[None, :]).astype(np.float32)
    shared['ropeC'], shared['ropeS'] = _rope_tables()
    shared['stri'] = (idx[:, None] < idx[None, :]).astype(np.float32)
    shared['thr'] = np.ascontiguousarray(np.broadcast_to((np.arange(8, dtype=np.float32) * SLOT_T)[None, None, :], (128, 8, 8)).reshape(128, 64))
    shared['kv'] = np.ascontiguousarray(np.broadcast_to(np.arange(NKT, dtype=np.float32)[None, :, None], (128, NKT, 8)).reshape(128, NKT * 8))
    shared['tokid'] = np.ascontiguousarray((np.arange(32, dtype=np.float32)[None, :] * 128 + np.arange(128, dtype=np.float32)[:, None]))
    dfl = np.zeros((NSLOT, 4), np.float32)
    dfl[:, 1] = 4 * SEQ + np.arange(NSLOT, dtype=np.float32)
    shared['dflt'] = np.ascontiguousarray(dfl.reshape(128, -1))
    shared['gn2_nat'] = f(inp['g_norm2'][1]).reshape(1, D)
    pp = np.arange(128, dtype=np.float32)[:, None, None]
    shared['rowc4'] = np.ascontiguousarray((4.0 * (np.arange(8, dtype=np.float32)[None, :, None] * 128 + pp)
                                            + np.arange(4, dtype=np.float32)[None, None, :]).reshape(128, 32))
    shared['rowf'] = np.ascontiguousarray(np.arange(NFT, dtype=np.float32)[None, :] * 128 + np.arange(128, dtype=np.float32)[:, None])
    x = f(inp['x']); ctx = f(inp['ctx']); c = f(inp['c']); cc = f(inp['c_ctx'])
    maps = []
    for k in range(ncores):
        m = dict(shared)
        m['x'] = x[2 * k:2 * k + 2]
        m['ctx'] = ctx[2 * k:2 * k + 2]
        cond = np.stack([c[2 * k], c[2 * k + 1], cc], axis=1)
        m['condT'] = np.ascontiguousarray(cond.reshape(8, 128, 3).transpose(1, 0, 2))
        maps.append(m)
    return maps


_CACHE = {}


def kernel(**inputs):
    if 'B' not in _CACHE:
        _CACHE['B'] = build()
    B = _CACHE['B']
    maps = prep_inputs(inputs)
    maps = [{k: v for k, v in m.items() if k in B.I} for m in maps]
    res = run_bass_kernel_spmd(B.nc, maps, core_ids=list(range(NCORES)))
    out = np.concatenate([np.asarray(r['out']) for r in res.results], axis=0)
    return out.astype(np.float32)
```

```python
import numpy as np
from contextlib import ExitStack
import concourse.bass as bass
import concourse.mybir as mybir
from concourse.bass_utils import run_bass_kernel_spmd

F32 = mybir.dt.float32
BF16 = mybir.dt.bfloat16
AF = mybir.ActivationFunctionType
ALU = mybir.AluOpType
AX = mybir.AxisListType

CENG = ('pe', 'act', 'dve', 'pool')
ENG = ('pe', 'act', 'dve', 'pool', 'sp')
NDSEM = 4
EPS = 1e-6
NCORES = 8
D = 1024
SEQ = 2048
NCTX = 256
TOT = SEQ + NCTX
NT = TOT // 128
DFF = 3584
NFT = DFF // 128
ABW = 2336
NEXP = 8
SLOT_T = 512
NKT = 24
NSLOT = NKT * SLOT_T
BIGIDX = 1.0e6
I32 = mybir.dt.int32


class Prog:
    def __init__(self, nc):
        self.nc = nc
        self.stack = ExitStack()
        self.ops = {e: [] for e in ENG}
        self.cnt = {e: 0 for e in CENG}
        self.pending_inc = {e: False for e in CENG}
        self.clock = {e: {} for e in ENG}
        self.lastw = {}
        self.readers = {}
        self.dma_rr = {q: 0 for q in ('sp', 'act', 'pool')}
        self.dma_cnt = {}
        self.dma_last = {}
        self.sems = {}
        for e in CENG:
            self.sems['c_' + e] = self.stack.enter_context(nc.semaphore('c_' + e))
        for q in ('sp', 'act', 'pool'):
            for i in range(NDSEM):
                n = 'd_%s_%d' % (q, i)
                self.sems[n] = self.stack.enter_context(nc.semaphore(n))
                self.dma_cnt[n] = 0
        self._cur = None
        self.psum_keys = set()

    def _uname(self, name):
        self._uid = getattr(self, '_uid', 0) + 1
        return 's%d_%s' % (self._uid, name)

    def sb(self, name, shape, dtype, stack=None):
        return (stack or self.stack).enter_context(self.nc.sbuf_tensor(self._uname(name), list(shape), dtype))

    def ps(self, name, shape, dtype, stack=None):
        self.psum_keys.add(name)
        return (stack or self.stack).enter_context(self.nc.psum_tensor(self._uname(name), list(shape), dtype))

    def bank(self, name, dtype, stack=None):
        return self.ps(name, [128, 512] if dtype == F32 else [128, 1024], dtype, stack)

    def _need(self, eng, ev):
        s, v, origin, clk = ev
        c = self.clock[eng]
        if c.get(s, 0) >= v:
            return
        self.ops[eng].append(('wait', s, v))
        c[s] = v
        for s2, v2 in clk.items():
            if c.get(s2, 0) < v2:
                c[s2] = v2

    def _deps(self, eng, reads, writes, is_dma=False):
        for r in reads:
            ev = self.lastw.get(r)
            if ev is not None:
                if (not is_dma) and eng == 'pe' and ev[2] == 'pe':
                    continue
                self._need(eng, ev)
            if r in self.psum_keys:
                rd = self.readers.get(r)
                if rd:
                    for ev2 in list(rd.values()):
                        if ev2[2] != eng:
                            self._need(eng, ev2)
        for w in writes:
            ev = self.lastw.get(w)
            if ev is not None and (is_dma or ev[2] != eng or eng != 'pe'):
                self._need(eng, ev)
            rd = self.readers.get(w)
            if rd:
                for ev in rd.values():
                    if is_dma or ev[2] != eng or eng != 'pe':
                        self._need(eng, ev)

    def _record(self, ev, reads, writes):
        for w in writes:
            self.lastw[w] = ev
            self.readers[w] = {}
        for r in reads:
            d = self.readers.setdefault(r, {})
            old = d.get(ev[0])
            if old is None or old[1] < ev[1]:
                d[ev[0]] = ev

    def op(self, eng, fn, reads=(), writes=(), inc=True):
        self._deps(eng, reads, writes)
        v = self.cnt[eng] + 1
        if inc:
            self.cnt[eng] = v
            self.pending_inc[eng] = False
        else:
            self.pending_inc[eng] = True
        ev = ('c_' + eng, v, eng, dict(self.clock[eng]))
        self._record(ev, reads, writes)
        self.ops[eng].append(('op', fn, inc))
        return ev

    def dma(self, q, out, in_, reads=(), writes=(), **kw):
        self._deps(q, reads, writes, is_dma=True)
        i = self.dma_rr[q]
        self.dma_rr[q] = (i + 1) % NDSEM
        n = 'd_%s_%d' % (q, i)
        last = self.dma_last.get(n)
        if last is not None:
            self._need(q, last)
        v = self.dma_cnt[n] + 16
        self.dma_cnt[n] = v
        ev = (n, v, 'dma', dict(self.clock[q]))
        self.dma_last[n] = ev
        self._record(ev, reads, writes)
        self.ops[q].append(('dma', out, in_, n, kw))
        return ev

    def dma_raw(self, q, fn, reads=(), writes=()):
        self._deps(q, reads, writes, is_dma=True)
        i = self.dma_rr[q]
        self.dma_rr[q] = (i + 1) % NDSEM
        n = 'd_%s_%d' % (q, i)
        last = self.dma_last.get(n)
        if last is not None:
            self._need(q, last)
        v = self.dma_cnt[n] + 16
        self.dma_cnt[n] = v
        ev = (n, v, 'dma', dict(self.clock[q]))
        self.dma_last[n] = ev
        self._record(ev, reads, writes)
        self.ops[q].append(('rawdma', fn, n))
        return ev

    def barrier(self):
        evs = []
        for e in CENG:
            assert not self.pending_inc[e], e
            if self.cnt[e] > 0:
                evs.append(('c_' + e, self.cnt[e], e, {}))
        for n, ev in self.dma_last.items():
            evs.append(ev)
        for e in ENG:
            for ev in evs:
                self._need(e, ev)
        self.lastw = {}
        self.readers = {}

    def flush(self):
        ops = self.ops
        sems = self.sems

        def emit(e, name):
            for o in ops[name]:
                if o[0] == 'wait':
                    e.wait_ge(sems[o[1]], o[2])
                elif o[0] == 'op':
                    ins = o[1](e)
                    if o[2]:
                        ins.then_inc(sems['c_' + name], 1)
                elif o[0] == 'rawdma':
                    o[1](e).then_inc(sems[o[2]], 16)
                else:
                    _, out, in_, n, kw = o
                    e.dma_start(out=out, in_=in_, **kw).then_inc(sems[n], 16)

        with self.nc.Block() as block:
            @block.tensor
            def _(e):
                emit(e, 'pe')

            @block.scalar
            def _(e):
                emit(e, 'act')

            @block.vector
            def _(e):
                emit(e, 'dve')

            @block.gpsimd
            def _(e):
                emit(e, 'pool')

            @block.sync
            def _(e):
                emit(e, 'sp')
        self.ops = {e: [] for e in ENG}

    def end_phase(self):
        self.barrier()
        self.flush()

    def mm(self, out, lhsT, rhs, start=True, stop=True, r=(), w=(), inc=None):
        if inc is None:
            inc = stop
        return self.op('pe', lambda e: e.matmul(out, lhsT=lhsT, rhs=rhs, start=start, stop=stop,
                                                skip_group_check=True), r, w, inc)

    def tr(self, out, in_, ident, r=(), w=(), inc=True):
        return self.op('pe', lambda e: e.transpose(out=out, in_=in_, identity=ident), r, w, inc)

    def act(self, out, in_, func, r=(), w=(), scale=None, bias=None, eng='act'):
        kw = {}
        if scale is not None:
            kw['scale'] = scale
        if bias is not None:
            kw['bias'] = bias
        return self.op('act', lambda e: e.activation(out=out, in_=in_, func=func, **kw), r, w)

    def tt(self, eng, out, in0, in1, op, r=(), w=()):
        return self.op(eng, lambda e: e.tensor_tensor(out=out, in0=in0, in1=in1, op=op), r, w)

    def ts(self, eng, out, in0, s1, op0, s2=None, op1=None, r=(), w=()):
        if op1 is None:
            return self.op(eng, lambda e: e.tensor_scalar(out=out, in0=in0, scalar1=s1, scalar2=None, op0=op0), r, w)
        return self.op(eng, lambda e: e.tensor_scalar(out=out, in0=in0, scalar1=s1, scalar2=s2, op0=op0, op1=op1), r, w)

    def stt(self, out, in0, scalar, in1, op0, op1, r=(), w=()):
        return self.op('dve', lambda e: e.scalar_tensor_tensor(out=out, in0=in0, scalar=scalar, in1=in1,
                                                               op0=op0, op1=op1), r, w)

    def copy(self, eng, out, in_, r=(), w=()):
        if eng == 'act':
            return self.op('act', lambda e: e.copy(out=out, in_=in_), r, w)
        return self.op(eng, lambda e: e.tensor_copy(out=out, in_=in_), r, w)

    def recip(self, out, in_, r=(), w=()):
        return self.op('dve', lambda e: e.reciprocal(out=out, in_=in_), r, w)

    def memset(self, eng, ap, val, w=()):
        return self.op(eng, lambda e: e.memset(ap, val), (), w)

    def reduce(self, out, in_, op, r=(), w=()):
        return self.op('dve', lambda e: e.tensor_reduce(out=out, in_=in_, axis=AX.X, op=op), r, w)

    def sumsq(self, junk, in_, acc, r=(), w=()):
        return self.op('dve', lambda e: e.scalar_tensor_tensor(out=junk, in0=in_, scalar=1.0, in1=in_,
                                                               op0=ALU.mult, op1=ALU.mult, accum_out=acc), r, w)


def bc(ap, shape):
    return ap.to_broadcast(list(shape))


class Builder:
    def __init__(self, upto='all', debug=False, preload=()):
        self.preload = set(preload)
        self.upto = upto
        self.debug = debug
        nc = bass.Bass("TRN2", target_bir_lowering=False)
        self.nc = nc
        self.P = Prog(nc)
        self.I = {}
        self.dbg = {}

    def inp(self, name, shape, dtype=F32):
        t = self.nc.dram_tensor(name, list(shape), dtype, kind="ExternalInput").ap()
        self.I[name] = t
        return t

    def dump(self, name, ap, shape, dtype=F32, reads=()):
        if not self.debug:
            return
        t = self.nc.dram_tensor('dbg_' + name, list(shape), dtype, kind="ExternalOutput").ap()
        self.dbg[name] = t
        idx = tuple(slice(None) for _ in shape)
        self.P.dma('sp', t[idx], ap, reads=list(reads), writes=['dbg_' + name])

    def scratch(self, name, shape, dtype=F32):
        if name in self.preload:
            return self.inp(name, shape, dtype)
        if self.debug:
            t = self.nc.dram_tensor(name, list(shape), dtype, kind="ExternalOutput").ap()
            self.dbg[name] = t
            return t
        return self.nc.dram_tensor(name, list(shape), dtype).ap()

    def declare(self):
        inp = self.inp
        inp('x', [2, SEQ, D]); inp('ctx', [2, NCTX, D]); inp('condT', [128, 8, 3])
        inp('w_mod', [2, D, 6 * D]); inp('b_mod', [2, 6 * D]); inp('gnT', [128, 2, 2, 8])
        inp('w_in_ab', [D, ABW]); inp('g_q', [1, 64]); inp('g_k', [1, 64])
        inp('w2f', [17, 256]); inp('w2b', [17, 256]); inp('g_gla', [1, 128])
        inp('w_out_ab', [D, D]); inp('w_ff_gate', [D, DFF]); inp('w_ff_up', [D, DFF]); inp('w_ff_down', [DFF, D])
        inp('w_in_c', [D, 3 * D]); inp('nabias', [16, 128, 14 * 64]); inp('w_out_c', [D, D])
        inp('w_router', [128, 8, NEXP])
        inp('w_moe_gate', [NEXP, D, DFF]); inp('w_moe_up', [NEXP, D, DFF]); inp('w_moe_down', [NEXP, DFF, D])
        inp('g_final', [1, D])
        inp('ident', [128, 128]); inp('tri_f', [128, 128]); inp('tri_b', [128, 128])
        inp('ropeC', [128, 16, 64]); inp('ropeS', [128, 16, 64])
        inp('stri', [128, 128]); inp('thr', [128, 64]); inp('kv', [128, NKT * 8]); inp('tokid', [128, 32])
        inp('dflt', [128, (NSLOT // 128) * 4]); inp('gn2_nat', [1, D])
        inp('rowc4', [128, 32]); inp('rowf', [128, NFT])
        self.out = self.nc.dram_tensor('out', [2, SEQ, D], F32, kind="ExternalOutput").ap()
        sc = self.scratch
        self.mod_d = sc('mod_d', [2, 3, 6, D])
        self.resA = sc('resA', [2, TOT, D])
        self.resB = sc('resB', [2, TOT, D])
        self.resC = sc('resC', [2, SEQ, D])
        self.qkb_d = sc('qkb_d', [2, TOT, 512])
        self.vb_d = sc('vb_d', [2, TOT, 512], BF16)
        self.rb_d = sc('rb_d', [2, TOT, 512])
        self.h_d = sc('h_d', [2 * SEQ, D], BF16)
        self.tab = sc('tab', [NSLOT, 4])
        self.y12 = sc('y12', [4 * SEQ + NSLOT, D])

    def consts(self):
        P, I = self.P, self.I
        self.ident_f = P.sb('ident_f', [128, 128], F32)
        self.ident_b = P.sb('ident_b', [128, 128], BF16)
        self.tri = {'f': P.sb('tri_f', [128, 128], F32), 'b': P.sb('tri_b', [128, 128], F32)}
        self.ones_f = P.sb('ones_f', [128, 8], F32)
        self.modAB = P.sb('modAB', [128, 2, 3, 2, 2, 8], F32)
        self.epsb = P.sb('epsb', [128, 1], F32)
        P.dma('sp', self.ident_f[:], I['ident'][:, :], writes=['ident_f'])
        P.dma('sp', self.tri['f'][:], I['tri_f'][:, :], writes=['tri_f'])
        P.dma('sp', self.tri['b'][:], I['tri_b'][:, :], writes=['tri_b'])
        P.copy('dve', self.ident_b[:], self.ident_f[:], r=['ident_f'], w=['ident_b'])
        P.memset('dve', self.ones_f[:], 1.0, w=['ones_f'])
        P.memset('dve', self.epsb[:], EPS, w=['epsb'])

    def phase_mod(self):
        P, I = self.P, self.I
        with ExitStack() as st:
            condT = P.sb('condT', [128, 8, 3], F32, st)
            scond = P.sb('scond', [128, 8, 3], F32, st)
            bmod = P.sb('bmod', [3, 6 * D], F32, st)
            mt = P.sb('mt', [3, 6 * D], F32, st)
            wbuf = [P.sb('wm%d' % i, [128, 8, 512], F32, st) for i in range(3)]
            psm = [P.ps('psm%d' % i, [128, 512], F32, st) for i in range(2)]
            modF = P.sb('modF', [128, 2, 3, 6, 8], F32, st)
            gn = P.sb('gn', [128, 2, 2, 8], F32, st)
            P.dma('sp', condT[:], I['condT'][:, :, :], writes=['condT'])
            P.dma('sp', gn[:], I['gnT'][:, :, :, :], writes=['gn'])
            P.act(scond[:], condT[:], AF.Silu, r=['condT'], w=['scond'])
            k = 0
            for i in range(2):
                P.dma('sp', bmod[:], I['b_mod'][i:i + 1, :].partition_broadcast(3), writes=['bmod'])
                wv = I['w_mod'][i].rearrange("(c p) f -> p c f", p=128)
                for n in range(12):
                    wb = wbuf[k % 3]
                    wk = 'wm%d' % (k % 3)
                    P.dma('sp' if k % 2 == 0 else 'act', wb[:], wv[:, :, n * 512:(n + 1) * 512], writes=[wk])
                    pk = 'psm%d' % (k % 2)
                    for c in range(8):
                        P.mm(psm[k % 2][0:3, :], scond[:, c, :], wb[:, c, :], start=(c == 0), stop=(c == 7),
                             r=['scond', wk], w=[pk])
                    P.tt('dve', mt[:, n * 512:(n + 1) * 512], psm[k % 2][0:3, :], bmod[:, n * 512:(n + 1) * 512],
                         ALU.add, r=[pk, 'bmod'], w=['mt'])
                    k += 1
                P.dma('sp', self.mod_d[i].rearrange("j k d -> j (k d)"), mt[:], reads=['mt'], writes=['mod_d'])
                for j in range(3):
                    P.dma('sp', modF[:, i, j, :, :], self.mod_d[i, j].rearrange("k (c p) -> p k c", p=128),
                          reads=['mod_d'], writes=['modF'], allow_slow_non_contiguous=True)
            for i in range(2):
                for j in range(3):
                    for n in range(2):
                        P.stt(self.modAB[:, i, j, n, 0, :], modF[:, i, j, 1 + 3 * n, :], 1.0, gn[:, i, n, :],
                              ALU.add, ALU.mult, r=['modF', 'gn'], w=['modAB'])
                        P.copy('dve', self.modAB[:, i, j, n, 1, :], modF[:, i, j, 3 * n, :], r=['modF'], w=['modAB'])
            P.end_phase()

    def load_gate(self, tile, key, layer, cond, which, q='sp'):
        k = 2 if which == 1 else 5
        self.P.dma(q, tile[:], self.mod_d[layer, cond, k:k + 1, :].partition_broadcast(128),
                   reads=['mod_d'], writes=[key])

    def norm_mod(self, st, tag, src_fn, ntiles, cond_fn, layer, norm, hT, hkey, want32=None, tm_cb=None, post_cb=None):
        P = self.P
        f32path = want32 is not None
        xt = [P.sb('%s_xt%d' % (tag, i), [128, D], F32, st) for i in range(3)]
        junk = P.sb(tag + '_junk', [128, D], BF16, st)
        ss = P.sb(tag + '_ss', [128, 2], F32, st)
        dt_n = F32 if f32path else BF16
        xn = [P.sb('%s_xn%d' % (tag, i), [128, D], dt_n, st) for i in range(2)]
        nps = 1 if f32path else 2
        pst = [P.ps('%s_pst%d' % (tag, i), [128, 8, 128], dt_n, st) for i in range(nps)]
        tm = [P.sb('%s_tm%d' % (tag, i), [128, 8, 128], F32, st) for i in range(2)]
        h32 = [P.sb('%s_h32_%d' % (tag, i), [128, 8, 128], F32, st) for i in range(2)] if f32path else None
        ident = self.ident_f if f32path else self.ident_b
        ikey = 'ident_f' if f32path else 'ident_b'
        def stage_a(t):
            x_ = xt[t % 3]; xk = '%s_xt%d' % (tag, t % 3)
            P.dma('sp', x_[:], src_fn(t), reads=['src_' + tag], writes=[xk])
            sk = tag + '_ss'
            P.sumsq(junk[:], x_[:], ss[:, 0:1], r=[xk], w=[tag + '_junk', sk + '0'])
            P.act(ss[:, 1:2], ss[:, 0:1], AF.Sqrt, r=[sk + '0', 'epsb'], w=[sk + '1'], scale=1.0 / D, bias=self.epsb[:, 0:1])
            P.recip(ss[:, 0:1], ss[:, 1:2], r=[sk + '1'], w=[sk + '0'])
            n_ = xn[t % 2]; nk = '%s_xn%d' % (tag, t % 2)
            P.act(n_[:], x_[:], AF.Identity, r=[xk, sk + '0'], w=[nk], scale=ss[:, 0:1])
            if tm_cb is not None:
                tm_cb(t, n_, nk)

        def stage_b(t):
            n_ = xn[t % 2]; nk = '%s_xn%d' % (tag, t % 2)
            ps_ = pst[t % nps]; pk = '%s_pst%d' % (tag, t % nps)
            for c in range(8):
                P.tr(ps_[:, c, :], n_[:, c * 128:(c + 1) * 128], ident[:], r=[nk, ikey], w=[pk], inc=(c == 7))
            cond = cond_fn(t)
            A = self.modAB[:, layer, cond, norm, 0, :]
            B = self.modAB[:, layer, cond, norm, 1, :]
            tm_ = tm[t % 2]; tk = '%s_tm%d' % (tag, t % 2)
            P.tt('dve', tm_[:], ps_[:], bc(A.unsqueeze(2), [128, 8, 128]), ALU.mult, r=[pk, 'modAB'], w=[tk])
            if f32path:
                h_ = h32[t % 2]; hk32 = '%s_h32_%d' % (tag, t % 2)
                P.tt('pool', h_[:], tm_[:], bc(B.unsqueeze(2), [128, 8, 128]), ALU.add, r=[tk, 'modAB'], w=[hk32])
                if hT is not None:
                    P.copy('act', hT[:, :, t * 128:(t + 1) * 128], h_[:], r=[hk32], w=[hkey + str(t)])
                want32(t, h_, hk32)
            else:
                P.tt('pool', hT[:, :, t * 128:(t + 1) * 128], tm_[:], bc(B.unsqueeze(2), [128, 8, 128]), ALU.add,
                     r=[tk, 'modAB'], w=[hkey + str(t)])

        for t in range(ntiles):
            stage_a(t)
            if t >= 1:
                stage_b(t - 1)
                if post_cb is not None:
                    post_cb(t - 1)
        stage_b(ntiles - 1)
        if post_cb is not None:
            post_cb(ntiles - 1)


    def layer0_mixer(self, b):
        P = self.P
        with ExitStack() as st0:
            o_tm = P.sb('o_tm', [128, NT, 512], BF16, st0)
            zaug = {d: P.sb('zaug_' + d, [17, TOT], F32, st0) for d in 'fb'}
            with ExitStack() as st1:
                qT = P.sb('qT', [64, 8, TOT], BF16, st1)
                kT = P.sb('kT', [64, 2, TOT], BF16, st1)
                v_sb = P.sb('v_sb', [128, NT, 2, 65], BF16, st1)
                with ExitStack() as st1a:
                    self.l0_inproj(b, st1a, qT, kT, v_sb, zaug)
                if self.upto in ('l0_norm', 'l0_inproj'):
                    return
                with ExitStack() as st1b:
                    self.l0_gqa(b, st1b, qT, kT, v_sb, o_tm)
                if self.upto == 'l0_gqa':
                    self.dump('o_tm', o_tm[:], [128, NT, 512], BF16)
                    P.end_phase()
                    return
            with ExitStack() as st2:
                oT = P.sb('oT', [128, 8, TOT], BF16, st2)
                with ExitStack() as st2a:
                    self.l0_gla(b, st2a, o_tm, zaug, oT)
                if self.upto == 'l0_gla':
                    self.dump('oT', oT[:], [128, 8, TOT], BF16)
                    P.end_phase()
                    return
                with ExitStack() as st2b:
                    self.l0_outproj(b, st2b, oT)

    def l0_inproj(self, b, st, qT, kT, v_sb, zaug):
        P, I = self.P, self.I
        hT = P.sb('hT', [128, 8, TOT], BF16, st)
        with ExitStack() as stn:
            def src(t):
                return I['ctx'][b, t * 128:(t + 1) * 128, :] if t < 2 else I['x'][b, (t - 2) * 128:(t - 1) * 128, :]
            import os
            self.norm_mod(stn, 'n1', src, int(os.environ.get('NTILES', NT)), lambda t: 2 if t < 2 else b, 0, 0, hT, 'hT')
            P.end_phase()
        if self.upto == 'l0_norm':
            if not os.environ.get('NODUMP'):
                self.dump('hT', hT[:], [128, 8, TOT], BF16)
            P.end_phase()
            return
        hkeys = ['hT%d' % t for t in range(NT)]
        w_in = P.sb('w_in', [128, 8, ABW], BF16, st)
        wv = I['w_in_ab'].rearrange("(c p) f -> p c f", p=128)
        for c in range(8):
            P.dma('pool', w_in[:, c, :], wv[:, c, :], writes=['w_in%d' % c])
        wkeys = ['w_in%d' % c for c in range(8)]
        gqk = P.sb('gqk', [128, 10, 64], F32, st)
        ropeC = P.sb('ropeC', [128, 16, 64], F32, st)
        ropeS = P.sb('ropeS', [128, 16, 64], F32, st)
        P.dma('sp', ropeC[:], I['ropeC'][:, :, :], writes=['ropeC'])
        P.dma('sp', ropeS[:], I['ropeS'][:, :, :], writes=['ropeS'])
        for h in range(8):
            P.dma('sp', gqk[:, h, :], I['g_q'][0:1, :].partition_broadcast(128), writes=['gqk'])
        for h in range(2):
            P.dma('sp', gqk[:, 8 + h, :], I['g_k'][0:1, :].partition_broadcast(128), writes=['gqk'])
        P.ts('dve', gqk[:, 0:8, :], gqk[:, 0:8, :], 0.125, ALU.mult, r=['gqk'], w=['gqk'])
        P.memset('dve', v_sb[:], 1.0, w=['v_sb'])
        for d in 'fb':
            P.memset('dve', zaug[d][:], 1.0, w=['zaug_' + d])
        pa0 = P.ps('pa0', [128, 512], F32, st)
        pa1 = P.bank('pa1', F32, st)[:, 0:256]
        pb = [P.ps('pb%d' % i, [128, 512], F32, st) for i in range(3)]
        ptq = P.bank('ptq', BF16, st)[0:64, :].rearrange("p (h d) -> p h d", d=128)
        ptk = P.bank('ptk', BF16, st)[0:64, 0:256].rearrange("p (h d) -> p h d", d=128)
        sq = P.sb('sq', [128, 640], F32, st)
        s10 = P.sb('s10', [128, 3, 10], F32, st)
        qk = P.sb('qk', [128, 10, 64], F32, st)
        qk2 = P.sb('qk2', [128, 10, 64], F32, st)
        sw = P.sb('sw', [128, 10, 64], F32, st)
        qkb = P.sb('qkb', [128, 10, 64], BF16, st)
        stq = [P.sb('stq%d' % i, [128, 512], F32, st) for i in range(2)]
        stv = [P.sb('stv%d' % i, [128, 512], BF16, st) for i in range(2)]
        str_ = [P.sb('str%d' % i, [128, 512], F32, st) for i in range(2)]
        blks = [(0, 512), (512, 512), (1024, 512), (1536, 512), (2048, 256)]
        for (t0, n) in blks:
            hk = hkeys[t0 // 128:(t0 + n) // 128]
            for di, d in enumerate('fb'):
                for c in range(8):
                    P.mm(pb[0][0:16, 0:n], w_in[:, c, 2304 + 16 * di:2320 + 16 * di], hT[:, c, t0:t0 + n],
                         start=(c == 0), stop=(c == 7), r=hk + [wkeys[c]], w=['pb0'])
                P.copy('act', zaug[d][0:16, t0:t0 + n], pb[0][0:16, 0:n], r=['pb0'], w=['zaug_' + d])
        for t in range(NT):
            hk = [hkeys[t]]
            tok = slice(t * 128, (t + 1) * 128)
            i2 = t % 2
            for c in range(8):
                P.mm(pa0[:], hT[:, c, tok], w_in[:, c, 0:512], start=(c == 0), stop=(c == 7),
                     r=hk + [wkeys[c]], w=['pa0'])
            for c in range(8):
                P.mm(pa1, hT[:, c, tok], w_in[:, c, 512:768], start=(c == 0), stop=(c == 7),
                     r=hk + [wkeys[c]], w=['pa1'])
            for gi, c0 in enumerate((768, 1280, 1792)):
                for c in range(8):
                    P.mm(pb[gi][:], hT[:, c, tok], w_in[:, c, c0:c0 + 512], start=(c == 0), stop=(c == 7),
                         r=hk + [wkeys[c]], w=['pb%d' % gi])
            P.act(sq[:, 0:512], pa0[:], AF.Square, r=['pa0'], w=['sq'])
            P.act(sq[:, 512:640], pa1[:, 0:128], AF.Square, r=['pa1'], w=['sq'])
            P.reduce(s10[:, 0, :], sq[:].rearrange("p (h d) -> p h d", d=64), ALU.add, r=['sq'], w=['s10a'])
            P.act(s10[:, 1, :], s10[:, 0, :], AF.Sqrt, r=['s10a', 'epsb'], w=['s10b'], scale=1.0 / 64, bias=self.epsb[:, 0:1])
            P.recip(s10[:, 2, :], s10[:, 1, :], r=['s10b'], w=['s10c'])
            P.tt('dve', qk[:, 0:8, :], pa0[:].rearrange("p (h d) -> p h d", d=64),
                 bc(s10[:, 2, 0:8].unsqueeze(2), [128, 8, 64]), ALU.mult, r=['pa0', 's10c'], w=['qk'])
            P.tt('dve', qk[:, 8:10, :], pa1[:, 0:128].rearrange("p (h d) -> p h d", d=64),
                 bc(s10[:, 2, 8:10].unsqueeze(2), [128, 2, 64]), ALU.mult, r=['pa1', 's10c'], w=['qk'])
            P.copy('act', v_sb[:, t, :, 0:64], pa1[:, 128:256].rearrange("p (h d) -> p h d", d=64),
                   r=['pa1'], w=['v_sb'])
            if t < 2:
                P.tt('pool', qkb[:], qk[:], gqk[:], ALU.mult, r=['qk', 'gqk'], w=['qkb'])
            else:
                P.tt('pool', qk2[:], qk[:], gqk[:], ALU.mult, r=['qk', 'gqk'], w=['qk2'])
                C = bc(ropeC[:, t - 2, :].unsqueeze(1), [128, 10, 64])
                Sv = ropeS[:, t - 2, :].rearrange("p (j two) -> p j two", two=2)
                q2v = qk2[:].rearrange("p h (j two) -> p h j two", two=2)
                swv = sw[:].rearrange("p h (j two) -> p h j two", two=2)
                P.tt('pool', swv[:, :, :, 0], q2v[:, :, :, 1], bc(Sv[:, :, 0].unsqueeze(1), [128, 10, 32]),
                     ALU.mult, r=['qk2', 'ropeS'], w=['sw0'])
                P.tt('pool', swv[:, :, :, 1], q2v[:, :, :, 0], bc(Sv[:, :, 1].unsqueeze(1), [128, 10, 32]),
                     ALU.mult, r=['qk2', 'ropeS'], w=['sw1'])
                P.tt('dve', qk[:], qk2[:], C, ALU.mult, r=['qk2', 'ropeC'], w=['qk'])
                P.tt('dve', qkb[:], qk[:], sw[:], ALU.add, r=['qk', 'sw0', 'sw1'], w=['qkb'])
            for h in range(8):
                P.tr(ptq[:, h, :], qkb[:, h, :], self.ident_b[:], r=['qkb', 'ident_b'], w=['ptq'], inc=(h == 7))
            for h in range(2):
                P.tr(ptk[:, h, :], qkb[:, 8 + h, :], self.ident_b[:], r=['qkb', 'ident_b'], w=['ptk'], inc=(h == 1))
            P.copy('act', qT[:, :, tok], ptq, r=['ptq'], w=['qT%d' % t])
            P.copy('dve', kT[:, :, tok], ptk, r=['ptk'], w=['kT%d' % t])
            P.copy('act', stq[i2][:], pb[0][:], r=['pb0'], w=['stq%d' % i2])
            P.dma('pool', self.qkb_d[b, tok, :], stq[i2][:], reads=['stq%d' % i2], writes=['qkb_d'])
            P.copy('dve', stv[i2][:], pb[1][:], r=['pb1'], w=['stv%d' % i2])
            P.dma('pool', self.vb_d[b, tok, :], stv[i2][:], reads=['stv%d' % i2], writes=['vb_d'])
            P.act(str_[i2][:], pb[2][:], AF.Silu, r=['pb2'], w=['str%d' % i2])
            P.dma('pool', self.rb_d[b, tok, :], str_[i2][:], reads=['str%d' % i2], writes=['rb_d'])
        P.end_phase()

    def l0_gqa(self, b, st, qT, kT, v_sb, o_tm):
        P = self.P
        pbuf = [P.sb('pbuf%d' % i, [128, NT, 512], BF16, st) for i in range(2)]
        pss = [P.ps('pss%d' % i, [128, 512], F32, st) for i in range(4)]
        po = [P.ps('po%d' % i, [128, 512], F32, st) for i in range(2)]
        rc = P.sb('rc', [128, 2], F32, st)
        ib = 0; isx = 0; io = 0
        for h in range(8):
            kv = h // 4
            jobs = [(0, 256, 2)] + [(256 + 512 * i, 512, NT) for i in range(4)]
            for (q0, nq, nk) in jobs:
                pb_ = pbuf[ib % 2]; pbk = 'pbuf%d' % (ib % 2); ib += 1
                for kt in range(nk):
                    ps_ = pss[isx % 4]; psk = 'pss%d' % (isx % 4); isx += 1
                    P.mm(ps_[:, 0:nq], kT[:, kv, kt * 128:(kt + 1) * 128], qT[:, h, q0:q0 + nq], r=[], w=[psk])
                    P.act(pb_[:, kt, 0:nq], ps_[:, 0:nq], AF.Exp, r=[psk], w=['%s_%d' % (pbk, kt)])
                for j in range(nq // 128):
                    po_ = po[io % 2]; pok = 'po%d' % (io % 2); rk = 'rc%d' % (io % 2)
                    for kt in range(nk):
                        P.mm(po_[:, 0:65], pb_[:, kt, j * 128:(j + 1) * 128], v_sb[:, kt, kv, :],
                             start=(kt == 0), stop=(kt == nk - 1), r=['%s_%d' % (pbk, kt)], w=[pok])
                    P.recip(rc[:, io % 2:io % 2 + 1], po_[:, 64:65], r=[pok], w=[rk])
                    tq = q0 // 128 + j
                    P.ts('dve', o_tm[:, tq, h * 64:(h + 1) * 64], po_[:, 0:64], rc[:, io % 2:io % 2 + 1], ALU.mult,
                         r=[pok, rk], w=['o_tm%d' % tq])
                    io += 1
        P.end_phase()

    def l0_gla(self, b, st, o_tm, zaug, oT):
        P, I = self.P, self.I
        ptb = self._ptb
        for t in range(NT):
            for c in range(4):
                P.tr(ptb[:, c, :], o_tm[:, t, c * 128:(c + 1) * 128], self.ident_b[:], r=['ident_b'], w=['ptb'], inc=(c == 3))
            P.copy('act', oT[:, 0:4, t * 128:(t + 1) * 128], ptb, r=['ptb'], w=['oTa%d' % t])
        qkb_s = P.sb('qkb_s', [128, NT, 512], F32, st)
        vb_s = P.sb('vb_s', [128, NT, 512], BF16, st)
        o_acc = P.sb('o_acc', [128, NT, 512], F32, st)
        w2 = {d: P.sb('w2' + d, [17, 256], F32, st) for d in 'fb'}
        ggla = P.sb('ggla', [128, 128], F32, st)
        S32 = P.sb('S32', [64, 4, 128], F32, st)
        Sbf = P.sb('Sbf', [64, 4, 128], BF16, st)
        tmpS = P.sb('tmpS', [64, 4, 128], F32, st)
        e1 = P.sb('e1', [128, 256], F32, st)
        sp_ = P.sb('sp_', [128, 256], F32, st)
        eq = P.sb('eq', [128, 256], F32, st)
        ek = P.sb('ek', [128, 256], F32, st)
        dec = P.sb('dec', [64, 4], F32, st)
        qd = P.sb('qd', [128, 256], BF16, st)
        ki = P.sb('ki', [128, 256], BF16, st)
        qkT = P.sb('qkT', [64, 8, 128], BF16, st)
        qdT = qkT[:, 0:4, :]
        kiT = qkT[:, 4:8, :]
        att = P.sb('att', [128, 4, 128], BF16, st)
        pg1 = P.bank('pg1', F32, st)
        pxg = pg1[:, 0:256]
        pcs = pg1[:, 256:512]
        ptot = P.bank('ptot', F32, st)[0:64, 0:4]
        ptqk = P.bank('ptqk', BF16, st)[0:64, :].rearrange("p (h d) -> p h d", d=128)
        patt = P.bank('patt', F32, st)[:, :].rearrange("p (h d) -> p h d", d=128)
        pog = P.bank('pog', F32, st)
        pds = P.bank('pds', F32, st)[0:64, :].rearrange("p (h d) -> p h d", d=128)
        P.dma('sp', w2['f'][:], I['w2f'][:, :], writes=['w2f'])
        P.dma('sp', w2['b'][:], I['w2b'][:, :], writes=['w2b'])
        P.dma('sp', ggla[:], I['g_gla'][0:1, :].partition_broadcast(128), writes=['ggla'])
        for t in range(NT):
            P.dma('sp', qkb_s[:, t, :], self.qkb_d[b, t * 128:(t + 1) * 128, :], writes=['qkb_s%d' % t])
            P.dma('sp', vb_s[:, t, :], self.vb_d[b, t * 128:(t + 1) * 128, :], writes=['vb_s%d' % t])
        e1b = [e1, P.sb('e1b', [128, 256], F32, st)]
        spb = [sp_, P.sb('spb', [128, 256], F32, st)]
        eqb = [eq, P.sb('eqb', [128, 256], F32, st)]
        ekb = [ek, P.sb('ekb', [128, 256], F32, st)]
        decb = [dec, P.sb('decb', [64, 4], F32, st)]
        qdb = [qd, P.sb('qdb', [128, 256], BF16, st)]
        kib = [ki, P.sb('kib', [128, 256], BF16, st)]
        qkTb = [qkT, P.sb('qkTb', [64, 8, 128], BF16, st)]
        for d in 'fb':
            order = list(range(NT)) if d == 'f' else [1, 0] + list(range(NT - 1, 1, -1))
            P.memset('dve', S32[:], 0.0, w=['S32'])
            P.memset('dve', Sbf[:], 0.0, w=['Sbf'])

            def prep(oi, d=d, order=order):
                n = order[oi]
                x = oi % 2
                sx = str(x)
                tok = slice(n * 128, (n + 1) * 128)
                P.mm(pxg, zaug[d][:, tok], w2[d][:], r=['w2' + d], w=['pg1'])
                P.act(e1b[x][:], pxg, AF.Exp, r=['pg1'], w=['e1' + sx], scale=-1.0)
                P.act(spb[x][:], e1b[x][:], AF.Ln, r=['e1' + sx], w=['sp_' + sx], bias=1.0)
                P.mm(pcs, self.tri[d][:], spb[x][:], r=['tri_' + d, 'sp_' + sx], w=['pg1'])
                for h in range(4):
                    P.mm(ptot[:, h:h + 1], spb[x][:, h * 64:(h + 1) * 64], self.ones_f[:, 0:1], r=['sp_' + sx, 'ones_f'],
                         w=['ptot'], inc=(h == 3))
                P.act(decb[x][:], ptot, AF.Exp, r=['ptot'], w=['dec' + sx], scale=-1.0 / 16)
                P.act(eqb[x][:], pcs, AF.Exp, r=['pg1'], w=['eq' + sx], scale=-1.0 / 16)
                P.act(ekb[x][:], pcs, AF.Exp, r=['pg1'], w=['ek' + sx], scale=1.0 / 16)
                P.stt(qdb[x][:], qkb_s[:, n, 0:256], 0.125, eqb[x][:], ALU.mult, ALU.mult, r=['qkb_s%d' % n, 'eq' + sx], w=['qd' + sx])
                P.tt('pool', kib[x][:], qkb_s[:, n, 256:512], ekb[x][:], ALU.mult, r=['qkb_s%d' % n, 'ek' + sx], w=['ki' + sx])
                for h in range(4):
                    P.tr(ptqk[:, h, :], qdb[x][:, h * 64:(h + 1) * 64], self.ident_b[:], r=['qd' + sx, 'ident_b'], w=['ptqk'], inc=False)
                for h in range(4):
                    P.tr(ptqk[:, 4 + h, :], kib[x][:, h * 64:(h + 1) * 64], self.ident_b[:], r=['ki' + sx, 'ident_b'], w=['ptqk'], inc=(h == 3))
                P.copy('act', qkTb[x][:], ptqk, r=['ptqk'], w=['qkT' + sx])

            def chain(oi, d=d, order=order):
                n = order[oi]
                x = oi % 2
                sx = str(x)
                qdT_ = qkTb[x][:, 0:4, :]
                kiT_ = qkTb[x][:, 4:8, :]
                for h in range(4):
                    P.mm(patt[:, h, :], kiT_[:, h, :], qdT_[:, h, :], r=['qkT' + sx], w=['patt'], inc=(h == 3))
                P.tt('dve', att[:], patt, bc(self.tri[d][:].unsqueeze(1), [128, 4, 128]), ALU.mult,
                     r=['patt', 'tri_' + d], w=['att'])
                for h in range(4):
                    hs = slice(h * 128, (h + 1) * 128)
                    P.mm(pog[:, hs], att[:, h, :], vb_s[:, n, hs], start=True, stop=(oi == 0),
                         r=['att', 'vb_s%d' % n], w=['pog'], inc=False)
                    if oi > 0:
                        P.mm(pog[:, hs], qdT_[:, h, :], Sbf[:, h, :], start=False, stop=True,
                             r=['qkT' + sx, 'Sbf'], w=['pog'], inc=False)
                for h in range(4):
                    hs = slice(h * 128, (h + 1) * 128)
                    P.mm(pds[:, h, :], kib[x][:, h * 64:(h + 1) * 64], vb_s[:, n, hs], r=['ki' + sx, 'vb_s%d' % n],
                         w=['pds'], inc=(h == 3))
                if d == 'f':
                    P.copy('act', o_acc[:, n, :], pog[:], r=['pog'], w=['o_acc%d' % n])
                else:
                    P.tt('dve', o_acc[:, n, :], o_acc[:, n, :], pog[:], ALU.add, r=['pog', 'o_acc%d' % n], w=['o_acc%d' % n])
                P.tt('dve', tmpS[:], pds, S32[:], ALU.add, r=['pds', 'S32'], w=['tmpS'])
                P.tt('dve', S32[:], tmpS[:], bc(decb[x][:].unsqueeze(2), [64, 4, 128]), ALU.mult, r=['tmpS', 'dec' + sx], w=['S32'])
                P.copy('act', Sbf[:], S32[:], r=['S32'], w=['Sbf'])

            prep(0)
            for oi in range(len(order)):
                if oi + 1 < len(order):
                    prep(oi + 1)
                chain(oi)
        sqo = P.sb('sqo', [128, 512], F32, st)
        s4 = P.sb('s4', [128, 3, 4], F32, st)
        on = P.sb('on', [128, 4, 128], F32, st)
        on2 = P.sb('on2', [128, 4, 128], F32, st)
        rt = [P.sb('rt%d' % i, [128, 512], F32, st) for i in range(2)]
        ob = P.sb('ob', [128, 512], BF16, st)
        for n in range(NT):
            i2 = n % 2
            P.dma('sp', rt[i2][:], self.rb_d[b, n * 128:(n + 1) * 128, :], writes=['rt%d' % i2])
            P.act(sqo[:], o_acc[:, n, :], AF.Square, r=['o_acc%d' % n], w=['sqo'])
            P.reduce(s4[:, 0, :], sqo[:].rearrange("p (h d) -> p h d", d=128), ALU.add, r=['sqo'], w=['s4a'])
            P.act(s4[:, 1, :], s4[:, 0, :], AF.Sqrt, r=['s4a', 'epsb'], w=['s4b'], scale=1.0 / 128, bias=self.epsb[:, 0:1])
            P.recip(s4[:, 2, :], s4[:, 1, :], r=['s4b'], w=['s4c'])
            P.tt('dve', on[:], o_acc[:, n, :].rearrange("p (h d) -> p h d", d=128),
                 bc(s4[:, 2, :].unsqueeze(2), [128, 4, 128]), ALU.mult, r=['o_acc%d' % n, 's4c'], w=['on'])
            P.tt('pool', on2[:], on[:], bc(ggla[:].unsqueeze(1), [128, 4, 128]), ALU.mult, r=['on', 'ggla'], w=['on2'])
            P.tt('dve', ob[:], on2[:].rearrange("p h d -> p (h d)"), rt[i2][:], ALU.mult, r=['on2', 'rt%d' % i2], w=['ob'])
            for c in range(4):
                P.tr(ptb[:, c, :], ob[:, c * 128:(c + 1) * 128], self.ident_b[:], r=['ob', 'ident_b'], w=['ptb'], inc=(c == 3))
            P.copy('act', oT[:, 4:8, n * 128:(n + 1) * 128], ptb, r=['ptb'], w=['oTb%d' % n])
        P.end_phase()

    def l0_outproj(self, b, st, oT):
        P, I = self.P, self.I
        wo = P.sb('wo', [128, 8, D], BF16, st)
        wv = I['w_out_ab'].rearrange("(c p) f -> p c f", p=128)
        for c in range(8):
            P.dma('pool', wo[:, c, :], wv[:, c, :], writes=['wo%d' % c])
        G = {j: P.sb('G%d' % j, [128, D], F32, st) for j in (2, b)}
        for j in (2, b):
            self.load_gate(G[j], 'G%d' % j, 0, j, 1)
        self.proj_residual(st, 'o0', oT, lambda t: [], NT, wo, ['wo%d' % c for c in range(8)],
                           lambda t: (I['ctx'][b, t * 128:(t + 1) * 128, :] if t < 2 else I['x'][b, (t - 2) * 128:(t - 1) * 128, :]),
                           lambda t: (G[2], 'G2') if t < 2 else (G[b], 'G%d' % b),
                           lambda t: self.resA[b, t * 128:(t + 1) * 128, :], 'resA')
        P.end_phase()

    def proj_residual(self, st, tag, oT, okeys, ntiles, wo, wokeys, xsrc, gate_fn, dst, dkey, tok0=0):
        P = self.P
        py = [P.ps('%s_py%d' % (tag, i), [128, 512], F32, st) for i in range(4)]
        xt = [P.sb('%s_x%d' % (tag, i), [128, D], F32, st) for i in range(2)]
        tmp = [P.sb('%s_t%d' % (tag, i), [128, D], F32, st) for i in range(2)]
        xo = [P.sb('%s_o%d' % (tag, i), [128, D], F32, st) for i in range(2)]
        for t in range(ntiles):
            i2 = t % 2
            tok = slice(tok0 + t * 128, tok0 + (t + 1) * 128)
            P.dma('sp', xt[i2][:], xsrc(t), reads=['src_' + tag], writes=['%s_x%d' % (tag, i2)])
            G, gk = gate_fn(t)
            for half in range(2):
                p_ = py[2 * i2 + half]; pk = '%s_py%d' % (tag, 2 * i2 + half)
                for c in range(8):
                    P.mm(p_[:], oT[:, c, tok], wo[:, c, half * 512:(half + 1) * 512], start=(c == 0), stop=(c == 7),
                         r=okeys(t) + [wokeys[c]], w=[pk])
                hs = slice(half * 512, (half + 1) * 512)
                P.tt('dve', tmp[i2][:, hs], p_[:], G[:, hs], ALU.mult, r=[pk, gk], w=['%s_t%d_%d' % (tag, i2, half)])
                P.tt('pool', xo[i2][:, hs], tmp[i2][:, hs], xt[i2][:, hs], ALU.add,
                     r=['%s_t%d_%d' % (tag, i2, half), '%s_x%d' % (tag, i2)], w=['%s_o%d_%d' % (tag, i2, half)])
            P.dma('pool', dst(t), xo[i2][:], reads=['%s_o%d_0' % (tag, i2), '%s_o%d_1' % (tag, i2)], writes=[dkey])


def _swiglu_gate_up(self, P, hT, ntok, blks, wg_src, wu_src, f_tiles, act, act_off, wbufs, pgs, pus, sgs, ctr):
    i = 0
    while i < len(f_tiles):
        nf = min(2, len(f_tiles) - i)
        f0 = f_tiles[i]
        k = ctr[0] % len(wbufs); ctr[0] += 1
        wg, wu = wbufs[k]
        P.dma('pool', wg[:, :, 0:nf * 128], wg_src[:, :, f0 * 128:(f0 + nf) * 128], writes=['wg%d' % k])
        P.dma('pool', wu[:, :, 0:nf * 128], wu_src[:, :, f0 * 128:(f0 + nf) * 128], writes=['wu%d' % k])
        for fi in range(nf):
            for (t0, n) in blks:
                j = ctr[1] % 2; ctr[1] += 1
                for c in range(8):
                    P.mm(pgs[j][:, 0:n], wg[:, c, fi * 128:(fi + 1) * 128], hT[:, c, t0:t0 + n], start=(c == 0), stop=(c == 7),
                         r=['wg%d' % k], w=['pg%d' % j])
                for c in range(8):
                    P.mm(pus[j][:, 0:n], wu[:, c, fi * 128:(fi + 1) * 128], hT[:, c, t0:t0 + n], start=(c == 0), stop=(c == 7),
                         r=['wu%d' % k], w=['pu%d' % j])
                P.act(sgs[j][:, 0:n], pgs[j][:, 0:n], AF.Silu, r=['pg%d' % j], w=['sg%d' % j])
                P.tt('dve', act[:, act_off + i + fi, t0:t0 + n], sgs[j][:, 0:n], pus[j][:, 0:n], ALU.mult,
                     r=['sg%d' % j, 'pu%d' % j], w=['act%d' % (act_off + i + fi)])
        i += nf


def _l0_ffn(self, b, half):
    P, I = self.P, self.I
    NTB = 9
    TB = NTB * 128
    r0 = half * TB
    with ExitStack() as st0:
        hT = P.sb('hT2', [128, 8, TB], BF16, st0)
        with ExitStack() as stn:
            self.norm_mod(stn, 'n2', lambda t: self.resA[b, r0 + t * 128:r0 + (t + 1) * 128, :], NTB,
                          lambda t: 2 if (half == 0 and t < 2) else b, 0, 1, hT, 'hT2')
            P.end_phase()
        with ExitStack() as st:
            act = P.sb('act', [128, NFT, TB], BF16, st)
            wbufs = [(P.sb('wg%d' % i, [128, 8, 256], BF16, st), P.sb('wu%d' % i, [128, 8, 256], BF16, st)) for i in range(3)]
            pgs = [P.bank('pg%d' % i, F32, st) for i in range(2)]
            pus = [P.bank('pu%d' % i, F32, st) for i in range(2)]
            sgs = [P.sb('sg%d' % i, [128, 512], F32, st) for i in range(2)]
            wd = [P.sb('wd%d' % i, [128, NFT, 512], BF16, st) for i in range(2)]
            pys = [P.bank('py%d' % i, F32, st) for i in range(2)]
            conds = [2, b] if half == 0 else [b]
            G = {j: P.sb('G2_%d' % j, [128, D], F32, st) for j in conds}
            for j in conds:
                self.load_gate(G[j], 'G2_%d' % j, 0, j, 2)
            xt = [P.sb('fx%d' % i, [128, 512], F32, st) for i in range(2)]
            tmp = [P.sb('ft%d' % i, [128, 512], F32, st) for i in range(2)]
            xo = [P.sb('fo%d' % i, [128, 512], F32, st) for i in range(2)]
            wg_src = I['w_ff_gate'].rearrange("(c p) f -> p c f", p=128)
            wu_src = I['w_ff_up'].rearrange("(c p) f -> p c f", p=128)
            wd_src = I['w_ff_down'].rearrange("(f p) d -> p f d", p=128)
            blks = [(0, 512), (512, 512), (1024, 128)]
            ctr = [0, 0]
            _swiglu_gate_up(self, P, hT, TB, blks, wg_src, wu_src, list(range(NFT)), act, 0, wbufs, pgs, pus, sgs, ctr)
            akeys = ['act%d' % f for f in range(NFT)]
            k = 0
            for dh in range(2):
                for q4 in range(4):
                    P.dma('pool', wd[dh][:, q4 * 7:(q4 + 1) * 7, :], wd_src[:, q4 * 7:(q4 + 1) * 7, dh * 512:(dh + 1) * 512],
                          writes=['wd%d_%d' % (dh, q4)])
                for t in range(NTB):
                    i2 = k % 2; k += 1
                    rows = slice(r0 + t * 128, r0 + (t + 1) * 128)
                    cs = slice(dh * 512, (dh + 1) * 512)
                    P.dma('sp', xt[i2][:], self.resA[b, rows, cs], writes=['fx%d' % i2])
                    for f in range(NFT):
                        P.mm(pys[i2][:], act[:, f, t * 128:(t + 1) * 128], wd[dh][:, f, :], start=(f == 0), stop=(f == NFT - 1),
                             r=[akeys[f], 'wd%d_%d' % (dh, f // 7)], w=['py%d' % i2])
                    j = 2 if (half == 0 and t < 2) else b
                    P.tt('dve', tmp[i2][:], pys[i2][:], G[j][:, cs], ALU.mult, r=['py%d' % i2, 'G2_%d' % j], w=['ft%d' % i2])
                    P.tt('pool', xo[i2][:], tmp[i2][:], xt[i2][:], ALU.add, r=['ft%d' % i2, 'fx%d' % i2], w=['fo%d' % i2])
                    P.dma('pool', self.resB[b, rows, cs], xo[i2][:], reads=['fo%d' % i2], writes=['resB'])
            P.end_phase()


def _na_pos(d0):
    return (d0 + 7) // 2 if d0 % 2 != 0 else 7 + (d0 + 6) // 2


def _layer1_mixer(self, b):
    P, I = self.P, self.I
    with ExitStack() as st0:
        oT = P.sb('oT1', [128, 8, SEQ], BF16, st0)
        with ExitStack() as st1:
            hT = P.sb('hT3', [128, 8, TOT], BF16, st1)
            with ExitStack() as stn:
                self.norm_mod(stn, 'n3', lambda t: self.resB[b, t * 128:(t + 1) * 128, :], NT,
                              lambda t: 2 if t < 2 else b, 1, 0, hT, 'hT3')
                P.end_phase()
            bias_sb = P.sb('bias_sb', [128, 16, 896], BF16, st1)
            for h in range(16):
                P.dma('pool', bias_sb[:, h, :], I['nabias'][h], writes=['bias%d' % h])
            wsrc = I['w_in_c'].rearrange("(c p) f -> p c f", p=128)
            for g in range(4):
                with ExitStack() as sg:
                    wq = P.sb('wq', [128, 8, 256], BF16, sg)
                    wk = P.sb('wk', [128, 8, 256], BF16, sg)
                    wv = P.sb('wv', [128, 8, 256], BF16, sg)
                    P.dma('pool', wq[:], wsrc[:, :, g * 256:(g + 1) * 256], writes=['wq'])
                    P.dma('pool', wk[:], wsrc[:, :, D + g * 256:D + (g + 1) * 256], writes=['wk'])
                    P.dma('pool', wv[:], wsrc[:, :, 2 * D + g * 256:2 * D + (g + 1) * 256], writes=['wv'])
                    qT = P.sb('qT1', [128, 2, SEQ], BF16, sg)
                    kT = P.sb('kT1', [128, 2, TOT], BF16, sg)
                    v_e = P.sb('v_e', [128, 16, 4, 65], BF16, sg)
                    v_o = P.sb('v_o', [128, 15, 4, 65], BF16, sg)
                    v_c = P.sb('v_c', [128, 2, 4, 65], BF16, sg)
                    o_g = P.sb('o_g', [64, 32, 256], BF16, sg)
                    pctx = [P.sb('pctx%d' % i, [128, 2, SEQ], BF16, sg) for i in range(2)]
                    ploc = [P.sb('ploc%d' % i, [128, 256], BF16, sg) for i in range(3)]
                    rc = P.sb('rc1', [64, 2], F32, sg)
                    pp_ = [P.bank('pp%d' % i, F32, sg) for i in range(3)]
                    psl = [P.bank('psl%d' % i, F32, sg) for i in range(2)]
                    pov = [P.bank('pov%d' % i, F32, sg) for i in range(2)]
                    P.memset('dve', v_e[:], 1.0, w=['v_e'])
                    P.memset('dve', v_o[:], 1.0, w=['v_o'])
                    P.memset('dve', v_c[:], 1.0, w=['v_c'])
                    ip = 0
                    for pp in range(2):
                        for blk in range(4):
                            p_ = pp_[ip % 3]; pk = 'pp%d' % (ip % 3); ip += 1
                            for c in range(8):
                                P.mm(p_[:], wq[:, c, pp * 128:(pp + 1) * 128], hT[:, c, NCTX + blk * 512:NCTX + (blk + 1) * 512],
                                     start=(c == 0), stop=(c == 7), r=['wq'], w=[pk])
                            P.act(qT[:, pp, blk * 512:(blk + 1) * 512], p_[:], AF.Identity, r=[pk], w=['qT1'], scale=0.125)
                        for (t0, n) in [(0, 256), (256, 512), (768, 512), (1280, 512), (1792, 512)]:
                            p_ = pp_[ip % 3]; pk = 'pp%d' % (ip % 3); ip += 1
                            for c in range(8):
                                P.mm(p_[:, 0:n], wk[:, c, pp * 128:(pp + 1) * 128], hT[:, c, t0:t0 + n],
                                     start=(c == 0), stop=(c == 7), r=['wk'], w=[pk])
                            P.copy('dve', kT[:, pp, t0:t0 + n], p_[:, 0:n], r=[pk], w=['kT1'])
                    vjobs = [(t * 128, v_c[:, t, :, 0:64], 'v_c') for t in range(2)]
                    vjobs += [(NCTX + t * 128, v_e[:, t, :, 0:64], 'v_e') for t in range(16)]
                    vjobs += [(NCTX + 64 + t * 128, v_o[:, t, :, 0:64], 'v_o') for t in range(15)]
                    for (t0, dst, dk) in vjobs:
                        p_ = pp_[ip % 3]; pk = 'pp%d' % (ip % 3); ip += 1
                        for c in range(8):
                            P.mm(p_[:, 0:256], hT[:, c, t0:t0 + 128], wv[:, c, :], start=(c == 0), stop=(c == 7), r=['wv'], w=[pk])
                        P.copy('act' if ip % 2 else 'dve', dst, p_[:, 0:256].rearrange("p (h d) -> p h d", d=64), r=[pk], w=[dk])
                    isl = 0; iov = 0; ipl = 0
                    for hh in range(4):
                        h = 4 * g + hh
                        pp = hh // 2
                        ps_ = slice((hh % 2) * 64, (hh % 2) * 64 + 64)
                        pc = pctx[hh % 2]; pck = 'pctx%d' % (hh % 2)
                        for ct in range(2):
                            for blk in range(4):
                                p_ = pp_[ip % 3]; pk = 'pp%d' % (ip % 3); ip += 1
                                P.mm(p_[:], kT[ps_, pp, ct * 128:(ct + 1) * 128], qT[ps_, pp, blk * 512:(blk + 1) * 512],
                                     r=['kT1', 'qT1'], w=[pk])
                                P.act(pc[:, ct, blk * 512:(blk + 1) * 512], p_[:], AF.Exp, r=[pk], w=[pck])
                        cnt = {'isl': isl, 'ipl': ipl, 'iov': iov}

                        def s_part(r, h=h, pp=pp, ps_=ps_, cnt=cnt):
                            rs = min(max(r - 4, 0), 24)
                            pos = _na_pos(rs - r)
                            sl = psl[cnt['isl'] % 2]; slk = 'psl%d' % (cnt['isl'] % 2); cnt['isl'] += 1
                            P.mm(sl[:, 0:256], self.ident_b[:], bias_sb[:, h, pos * 64:(pos + 4) * 64], start=True, stop=False,
                                 r=['bias%d' % h, 'ident_b'], w=[slk], inc=False)
                            for j in range(4):
                                kt0 = NCTX + (rs + 2 * j) * 64
                                P.mm(sl[:, j * 64:(j + 1) * 64], kT[ps_, pp, kt0:kt0 + 128], qT[ps_, pp, r * 64:(r + 1) * 64],
                                     start=False, stop=True, r=['kT1', 'qT1'], w=[slk], inc=(j == 3))
                            pl = ploc[cnt['ipl'] % 3]; plk = 'ploc%d' % (cnt['ipl'] % 3); cnt['ipl'] += 1
                            P.act(pl[:], sl[:, 0:256], AF.Exp, r=[slk], w=[plk])
                            return (rs, pl, plk)

                        def pv_part(r, st_, hh=hh, pc=pc, pck=pck, cnt=cnt):
                            rs, pl, plk = st_
                            iov_ = cnt['iov']
                            ov = pov[iov_ % 2]; ovk = 'pov%d' % (iov_ % 2); rk = 'rc1_%d' % (iov_ % 2)
                            for j in range(4):
                                row = rs + 2 * j
                                vt = v_e[:, row // 2, hh, :] if rs % 2 == 0 else v_o[:, (row - 1) // 2, hh, :]
                                P.mm(ov[0:64, 0:65], pl[:, j * 64:(j + 1) * 64], vt, start=(j == 0), stop=False,
                                     r=[plk, 'v_e', 'v_o'], w=[ovk], inc=False)
                            for ct in range(2):
                                P.mm(ov[0:64, 0:65], pc[:, ct, r * 64:(r + 1) * 64], v_c[:, ct, hh, :], start=False, stop=(ct == 1),
                                     r=[pck, 'v_c'], w=[ovk], inc=(ct == 1))
                            P.recip(rc[:, iov_ % 2:iov_ % 2 + 1], ov[0:64, 64:65], r=[ovk], w=[rk])
                            P.ts('dve', o_g[:, r, hh * 64:(hh + 1) * 64], ov[0:64, 0:64], rc[:, iov_ % 2:iov_ % 2 + 1], ALU.mult,
                                 r=[ovk, rk], w=['o_g%d' % r])
                            cnt['iov'] += 1

                        st_ = s_part(0)
                        for r in range(32):
                            nxt = s_part(r + 1) if r + 1 < 32 else None
                            pv_part(r, st_)
                            st_ = nxt
                        isl, ipl, iov = cnt['isl'], cnt['ipl'], cnt['iov']
                    ptb_full = self._ptb_bank
                    ptv = ptb_full[:, :].rearrange("p (c r q) -> p c r q", c=2, r=8)
                    for r8 in range(4):
                        for cc in range(2):
                            for rr in range(8):
                                r = r8 * 8 + rr
                                P.tr(ptv[:, cc, rr, :], o_g[:, r, cc * 128:(cc + 1) * 128], self.ident_b[0:64, 0:64],
                                     r=['o_g%d' % r, 'ident_b'], w=['ptb'], inc=(cc == 1 and rr == 7))
                        P.copy('act', oT[:, 2 * g:2 * g + 2, r8 * 512:(r8 + 1) * 512],
                               ptb_full[:, :].rearrange("p (c n) -> p c n", c=2), r=['ptb'], w=['oT1'])
                    P.end_phase()
        with ExitStack() as st2:
            wo = P.sb('wo1', [128, 8, D], BF16, st2)
            wv_ = I['w_out_c'].rearrange("(c p) f -> p c f", p=128)
            for c in range(8):
                P.dma('pool', wo[:, c, :], wv_[:, c, :], writes=['wo1_%d' % c])
            G = P.sb('G1b', [128, D], F32, st2)
            self.load_gate(G, 'G1b', 1, b, 1)
            self.proj_residual(st2, 'o1', oT, lambda t: [], 16, wo, ['wo1_%d' % c for c in range(8)],
                               lambda t: self.resB[b, NCTX + t * 128:NCTX + (t + 1) * 128, :],
                               lambda t: (G, 'G1b'),
                               lambda t: self.resC[b, t * 128:(t + 1) * 128, :], 'resC')
            P.end_phase()


def _layer1_moe(self, b):
    P, I = self.P, self.I
    NTL = 16
    with ExitStack() as st0:
        comb = P.sb('comb', [128, NTL, NEXP], F32, st0)
        y_acc = P.sb('y_acc', [128, NTL, D], F32, st0)
        with ExitStack() as stA:
            hT = P.sb('hT4', [128, 8, SEQ], BF16, stA)
            with ExitStack() as stn:
                wr = P.sb('wr', [128, 8, NEXP], F32, stn)
                P.dma('sp', wr[:], I['w_router'][:, :, :], writes=['wr'])
                plog = P.bank('plog', F32, stn)
                rt_ = P.sb('rt_', [128, 8, 8], F32, stn)

                def want32(t, h32, key):
                    for c in range(8):
                        P.mm(plog[:, 0:NEXP], h32[:, c, :], wr[:, c, :], start=(c == 0), stop=(c == 7), r=[key, 'wr'], w=['plog'])
                    lg, top8, dd, ex, mk, num, den = [rt_[:, i, :] for i in range(7)]
                    P.copy('dve', lg, plog[:, 0:NEXP], r=['plog'], w=['r_lg'])
                    P.op('dve', lambda e: e.max(out=top8, in_=lg), ['r_lg'], ['r_top'])
                    P.ts('dve', dd, lg, top8[:, 0:1], ALU.subtract, r=['r_lg', 'r_top'], w=['r_dd'])
                    P.act(ex, dd, AF.Exp, r=['r_dd'], w=['r_ex'])
                    P.ts('dve', mk, lg, top8[:, 1:2], ALU.is_ge, r=['r_lg', 'r_top'], w=['r_mk'])
                    P.tt('dve', num, ex, mk, ALU.mult, r=['r_ex', 'r_mk'], w=['r_num'])
                    P.reduce(den[:, 0:1], num, ALU.add, r=['r_num'], w=['r_den'])
                    P.recip(den[:, 1:2], den[:, 0:1], r=['r_den'], w=['r_rden'])
                    P.ts('dve', comb[:, t, :], num, den[:, 1:2], ALU.mult, r=['r_num', 'r_rden'], w=['comb%d' % t])

                self.norm_mod(stn, 'n4', lambda t: self.resC[b, t * 128:(t + 1) * 128, :], NTL, lambda t: b, 1, 1, hT, 'hT4',
                              want32=want32)
                P.end_phase()
            if self.upto == 'l1_router':
                self.dump('comb', comb[:], [128, NTL, NEXP], F32)
                P.end_phase()
                return
            with ExitStack() as st:
                NQ = 7
                act = P.sb('mact', [128, NQ, SEQ], BF16, st)
                wbufs = [(P.sb('wg%d' % i, [128, 8, 256], BF16, st), P.sb('wu%d' % i, [128, 8, 256], BF16, st)) for i in range(4)]
                wd = [P.sb('mwd%d' % i, [128, D], BF16, st) for i in range(8)]
                pgs = [P.bank('pg%d' % i, F32, st) for i in range(2)]
                pus = [P.bank('pu%d' % i, F32, st) for i in range(2)]
                pys = [P.bank('py%d' % i, F32, st) for i in range(2)]
                sgs = [P.sb('sg%d' % i, [128, 512], F32, st) for i in range(2)]
                for t in range(NTL):
                    P.memset('pool' if t % 2 else 'dve', y_acc[:, t, :], 0.0, w=['y%d_0' % t, 'y%d_1' % t])
                blks = [(i * 512, 512) for i in range(4)]
                ctr = [0, 0]
                iw = 0; iy = 0
                import os
                nexp = int(os.environ.get('MOE_NEXP', NEXP))
                for e in range(nexp):
                    wg_src = I['w_moe_gate'][e].rearrange("(c p) f -> p c f", p=128)
                    wu_src = I['w_moe_up'][e].rearrange("(c p) f -> p c f", p=128)
                    for q in range(NFT // NQ):
                        fts = list(range(q * NQ, (q + 1) * NQ))
                        self._moe_gate_up(P, hT, blks, wg_src, wu_src, fts, act, wbufs, pgs, pus, sgs, ctr)
                        wks = []
                        for fl in range(NQ):
                            k = iw % 8; iw += 1
                            P.dma('pool', wd[k][:], I['w_moe_down'][e, (q * NQ + fl) * 128:(q * NQ + fl + 1) * 128, :], writes=['mwd%d' % k])
                            wks.append(k)
                        for t in range(NTL):
                            for dh in range(2):
                                i2 = iy % 2; iy += 1
                                for fl in range(NQ):
                                    P.mm(pys[i2][:], act[:, fl, t * 128:(t + 1) * 128], wd[wks[fl]][:, dh * 512:(dh + 1) * 512],
                                         start=(fl == 0), stop=(fl == NQ - 1), r=['act%d' % fl, 'mwd%d' % wks[fl]], w=['py%d' % i2])
                                yk = 'y%d_%d' % (t, dh)
                                ys = y_acc[:, t, dh * 512:(dh + 1) * 512]
                                P.stt(ys, pys[i2][:], comb[:, t, e:e + 1], ys, ALU.mult, ALU.add, r=['py%d' % i2, yk], w=[yk])
                P.end_phase()
        with ExitStack() as st:
            G2 = P.sb('G2f', [128, D], F32, st)
            gfin = P.sb('gfin', [128, D], F32, st)
            self.load_gate(G2, 'G2f', 1, b, 2)
            P.dma('sp', gfin[:], I['g_final'][0:1, :].partition_broadcast(128), writes=['gfin'])
            xc = [P.sb('xc%d' % i, [128, D], F32, st) for i in range(2)]
            tmp = P.sb('ftmp', [128, D], F32, st)
            xo = P.sb('fxo', [128, D], F32, st)
            junk = P.sb('fjunk', [128, D], BF16, st)
            ss = P.sb('fss', [128, 2], F32, st)
            ot = [P.sb('fot%d' % i, [128, D], F32, st) for i in range(2)]
            for t in range(NTL):
                i2 = t % 2
                P.dma('sp', xc[i2][:], self.resC[b, t * 128:(t + 1) * 128, :], writes=['xc%d' % i2])
                P.tt('dve', tmp[:], y_acc[:, t, :], G2[:], ALU.mult, r=['G2f'], w=['ftmp'])
                P.tt('pool', xo[:], tmp[:], xc[i2][:], ALU.add, r=['ftmp', 'xc%d' % i2], w=['fxo'])
                P.sumsq(junk[:], xo[:], ss[:, 0:1], r=['fxo'], w=['fjunk', 'fss0'])
                P.act(ss[:, 1:2], ss[:, 0:1], AF.Sqrt, r=['fss0', 'epsb'], w=['fss1'], scale=1.0 / D, bias=self.epsb[:, 0:1])
                P.recip(ss[:, 0:1], ss[:, 1:2], r=['fss1'], w=['fss0'])
                P.stt(ot[i2][:], xo[:], ss[:, 0:1], gfin[:], ALU.mult, ALU.mult, r=['fxo', 'fss0', 'gfin'], w=['fot%d' % i2])
                P.dma('pool', self.out[b, t * 128:(t + 1) * 128, :], ot[i2][:], reads=['fot%d' % i2], writes=['out'])
            P.end_phase()


def _moe_gate_up(self, P, hT, blks, wg_src, wu_src, f_tiles, act, wbufs, pgs, pus, sgs, ctr):
    i = 0
    while i < len(f_tiles):
        nf = min(2, len(f_tiles) - i)
        f0 = f_tiles[i]
        k = ctr[0] % len(wbufs); ctr[0] += 1
        wg, wu = wbufs[k]
        P.dma('pool', wg[:, :, 0:nf * 128], wg_src[:, :, f0 * 128:(f0 + nf) * 128], writes=['wg%d' % k])
        P.dma('pool', wu[:, :, 0:nf * 128], wu_src[:, :, f0 * 128:(f0 + nf) * 128], writes=['wu%d' % k])
        for fi in range(nf):
            for (t0, n) in blks:
                j = ctr[1] % 2; ctr[1] += 1
                for c in range(8):
                    P.mm(pgs[j][:, 0:n], wg[:, c, fi * 128:(fi + 1) * 128], hT[:, c, t0:t0 + n], start=(c == 0), stop=(c == 7),
                         r=['wg%d' % k], w=['pg%d' % j])
                for c in range(8):
                    P.mm(pus[j][:, 0:n], wu[:, c, fi * 128:(fi + 1) * 128], hT[:, c, t0:t0 + n], start=(c == 0), stop=(c == 7),
                         r=['wu%d' % k], w=['pu%d' % j])
                P.act(sgs[j][:, 0:n], pgs[j][:, 0:n], AF.Silu, r=['pg%d' % j], w=['sg%d' % j])
                P.tt('dve', act[:, i + fi, t0:t0 + n], sgs[j][:, 0:n], pus[j][:, 0:n], ALU.mult,
                     r=['sg%d' % j, 'pu%d' % j], w=['act%d' % (i + fi)])
        i += nf


def _layer1_moe_sparse(self):
    P, I = self.P, self.I
    NTT = 32
    nc = self.nc
    with ExitStack() as st0:
        m12 = P.sb('m12', [128, 2, NTT, 8], F32, st0)
        pos = P.sb('pos', [128, NTT, 8], F32, st0)
        g12 = P.sb('g12', [128, 2, NTT], F32, st0)
        run = P.sb('run', [128, 8], F32, st0)
        ek_i = P.sb('ek_i', [128, NKT], I32, st0)
        idxg = P.sb('idxg', [128, NKT, 32], I32, st0)
        idxd = P.sb('idxd', [128, NKT, NFT], I32, st0)
        with ExitStack() as stn:
            wr = P.sb('wr', [128, 8, NEXP], F32, stn)
            P.dma('sp', wr[:], I['w_router'][:, :, :], writes=['wr'])
            stri = P.sb('stri', [128, 128], F32, stn)
            P.dma('sp', stri[:], I['stri'][:, :], writes=['stri'])
            ones128 = P.sb('ones128', [128, 128], F32, stn)
            P.memset('dve', ones128[:], 1.0, w=['ones128'])
            P.memset('dve', run[:], 0.0, w=['run'])
            plog = P.bank('plog', F32, stn)
            ppos = P.bank('ppos', F32, stn)
            lg_all = P.sb('lg_all', [128, NTT, 8], F32, stn)
            Abc = P.sb('Abc', [128, D], F32, stn)
            Bbc = P.sb('Bbc', [128, D], F32, stn)
            gnb = P.sb('gnb', [128, D], F32, stn)
            htm = [P.sb('htm%d' % i, [128, D], F32, stn) for i in range(2)]
            hbf = [P.sb('hbf%d' % i, [128, D], BF16, stn) for i in range(2)]
            P.dma('sp', gnb[:], I['gn2_nat'][0:1, :].partition_broadcast(128), writes=['gnb'])
            for b in range(2):
                P.dma('sp', Abc[:], self.mod_d[1, b, 4:5, :].partition_broadcast(128), writes=['Abc'])
                P.dma('sp', Bbc[:], self.mod_d[1, b, 3:4, :].partition_broadcast(128), writes=['Bbc'])
                P.stt(Abc[:], Abc[:], 1.0, gnb[:], ALU.add, ALU.mult, r=['Abc', 'gnb'], w=['Abc'])

                def tm_cb(t, xn, xk, b=b):
                    tt = b * 16 + t
                    i2 = tt % 2
                    P.tt('pool', htm[i2][:], xn[:], Abc[:], ALU.mult, r=[xk, 'Abc'], w=['htm%d' % i2])
                    P.tt('pool', hbf[i2][:], htm[i2][:], Bbc[:], ALU.add, r=['htm%d' % i2, 'Bbc'], w=['hbf%d' % i2])

                def post_cb(t, b=b):
                    tt = b * 16 + t
                    i2 = tt % 2
                    P.dma('act', self.h_d[tt * 128:(tt + 1) * 128, :], hbf[i2][:], reads=['hbf%d' % i2], writes=['h_d'])

                def want32(t, h32, key, b=b):
                    tt = b * 16 + t
                    for c in range(8):
                        P.mm(plog[:, 0:NEXP], h32[:, c, :], wr[:, c, :], start=(c == 0), stop=(c == 7), r=[key, 'wr'], w=['plog'])
                    P.copy('dve', lg_all[:, tt, :], plog[:, 0:NEXP], r=['plog'], w=['lg_all'])

                self.norm_mod(stn, 'n4_%d' % b, lambda t, b=b: self.resC[b, t * 128:(t + 1) * 128, :], 16, lambda t, b=b: b, 1, 1,
                              None, 'hT4', want32=want32, tm_cb=tm_cb, post_cb=post_cb) if b == 0 else \
                    self.norm_mod(stn, 'n5_%d' % b, lambda t, b=b: self.resC[b, t * 128:(t + 1) * 128, :], 16, lambda t, b=b: b, 1, 1,
                                  None, 'hT4', want32=want32, tm_cb=tm_cb, post_cb=post_cb)
            lg2 = P.sb('lg2', [128, NTT, 8], F32, stn)
            msum = P.sb('msum', [128, NTT, 8], F32, stn)
            tq = P.sb('tq', [128, 5, NTT], F32, stn)
            pp_sb = P.sb('pp_sb', [128, NTT, 16], F32, stn)
            runb = P.sb('runb', [128, NTT + 1, 8], F32, stn)
            pposv = ppos[:, :].rearrange("p (t w) -> p t w", w=16)
            P.reduce(tq[:, 0, :], lg_all[:], ALU.max, r=['lg_all'], w=['tq0'])
            P.tt('dve', m12[:, 0, :, :], lg_all[:], bc(tq[:, 0, :].unsqueeze(2), [128, NTT, 8]), ALU.is_equal, r=['lg_all', 'tq0'], w=['m12a'])
            P.stt(lg2[:], m12[:, 0, :, :], -1.0e30, lg_all[:], ALU.mult, ALU.add, r=['m12a', 'lg_all'], w=['lg2'])
            P.reduce(tq[:, 1, :], lg2[:], ALU.max, r=['lg2'], w=['tq1'])
            P.tt('dve', m12[:, 1, :, :], lg2[:], bc(tq[:, 1, :].unsqueeze(2), [128, NTT, 8]), ALU.is_equal, r=['lg2', 'tq1'], w=['m12b'])
            P.tt('dve', tq[:, 2, :], tq[:, 1, :], tq[:, 0, :], ALU.subtract, r=['tq0', 'tq1'], w=['tq2'])
            P.act(tq[:, 3, :], tq[:, 2, :], AF.Exp, r=['tq2'], w=['tq3'])
            P.ts('dve', tq[:, 4, :], tq[:, 3, :], 1.0, ALU.add, r=['tq3'], w=['tq4'])
            P.recip(g12[:, 0, :], tq[:, 4, :], r=['tq4'], w=['g12a'])
            P.tt('dve', g12[:, 1, :], tq[:, 3, :], g12[:, 0, :], ALU.mult, r=['tq3', 'g12a'], w=['g12b'])
            P.tt('dve', msum[:], m12[:, 0, :, :], m12[:, 1, :, :], ALU.add, r=['m12a', 'm12b'], w=['msum'])
            for tt in range(NTT):
                P.mm(pposv[:, tt, 0:8], stri[:], msum[:, tt, :], r=['stri', 'msum'], w=['ppos'], inc=False)
                P.mm(pposv[:, tt, 8:16], ones128[:], msum[:, tt, :], r=['ones128', 'msum'], w=['ppos'], inc=(tt == NTT - 1))
            P.copy('dve', pp_sb[:], pposv, r=['ppos'], w=['pp_sb'])
            P.memset('dve', runb[:, 0, :], 0.0, w=['runb'])
            for tt in range(NTT):
                P.tt('dve', runb[:, tt + 1, :], runb[:, tt, :], pp_sb[:, tt, 8:16], ALU.add, r=['runb', 'pp_sb'], w=['runb'])
            P.tt('dve', pos[:], pp_sb[:, :, 0:8], runb[:, 0:NTT, :], ALU.add, r=['pp_sb', 'runb'], w=['pos'])
            P.copy('dve', run[:], runb[:, NTT, :], r=['runb'], w=['run'])
            thr = P.sb('thr', [128, 8, 8], F32, stn)
            kv = P.sb('kv', [128, NKT, 8], F32, stn)
            tokid = P.sb('tokid', [128, NTT], F32, stn)
            dflt = P.sb('dflt', [128, NSLOT // 128, 4], F32, stn)
            P.dma('sp', thr[:], I['thr'].rearrange("p (e m) -> p e m", m=8), writes=['thr'])
            P.dma('sp', kv[:], I['kv'].rearrange("p (k e) -> p k e", e=8), writes=['kv'])
            P.dma('sp', tokid[:], I['tokid'][:, :], writes=['tokid'])
            P.dma('sp', dflt[:], I['dflt'].rearrange("p (n w) -> p n w", w=4), writes=['dflt'])
            P.dma('sp', self.tab.rearrange("(p n) w -> p n w", p=128), dflt[:], reads=['dflt'], writes=['tab0'])
            cmp8 = P.sb('cmp8', [128, 8, 8], F32, stn)
            tl = P.sb('tl', [128, 4, 8], F32, stn)
            cmpk = P.sb('cmpk', [128, NKT, 8], F32, stn)
            ekf = P.sb('ekf', [128, NKT], F32, stn)
            big = P.sb('big', [128, NTT, 8], F32, stn)
            slf = P.sb('slf', [128, 2 * NTT], F32, stn)
            sli = P.sb('sli', [128, 2 * NTT], I32, stn)
            rowd = P.sb('rowd', [128, 2, NTT, 4], F32, stn)
            P.tt('dve', cmp8[:], thr[:], bc(run[:].unsqueeze(2), [128, 8, 8]), ALU.is_lt, r=['thr', 'run'], w=['cmp8'])
            P.reduce(tl[:, 0, :], cmp8[:], ALU.add, r=['cmp8'], w=['tl0'])
            P.copy('dve', tl[:, 1, 0:1], tl[:, 0, 0:1], r=['tl0'], w=['tl1'])
            for e_ in range(1, 8):
                P.tt('dve', tl[:, 1, e_:e_ + 1], tl[:, 1, e_ - 1:e_], tl[:, 0, e_:e_ + 1], ALU.add, r=['tl0', 'tl1'], w=['tl1'])
            P.tt('dve', tl[:, 2, :], tl[:, 1, :], tl[:, 0, :], ALU.subtract, r=['tl0', 'tl1'], w=['tl2'])
            P.ts('dve', tl[:, 2, :], tl[:, 2, :], float(SLOT_T), ALU.mult, r=['tl2'], w=['tl2'])
            P.tt('dve', cmpk[:], kv[:], bc(tl[:, 1, :].unsqueeze(1), [128, NKT, 8]), ALU.is_ge, r=['kv', 'tl1'], w=['cmpk'])
            P.reduce(ekf[:], cmpk[:], ALU.add, r=['cmpk'], w=['ekf'])
            P.ts('dve', ekf[:], ekf[:], 7.0, ALU.min, r=['ekf'], w=['ekf'])
            P.copy('dve', ek_i[:], ekf[:], r=['ekf'], w=['ek_i'])
            rowc4 = P.sb('rowc4', [128, 32], F32, stn)
            rowf = P.sb('rowf', [128, NFT], F32, stn)
            P.dma('sp', rowc4[:], I['rowc4'][:, :], writes=['rowc4'])
            P.dma('sp', rowf[:], I['rowf'][:, :], writes=['rowf'])
            idxg_f = P.sb('idxg_f', [128, NKT, 32], F32, stn)
            idxd_f = P.sb('idxd_f', [128, NKT, NFT], F32, stn)
            P.stt(idxg_f[:], bc(ekf[:].unsqueeze(2), [128, NKT, 32]), 4096.0, bc(rowc4[:].unsqueeze(1), [128, NKT, 32]),
                  ALU.mult, ALU.add, r=['ekf', 'rowc4'], w=['idxg_f'])
            P.stt(idxd_f[:], bc(ekf[:].unsqueeze(2), [128, NKT, NFT]), float(DFF), bc(rowf[:].unsqueeze(1), [128, NKT, NFT]),
                  ALU.mult, ALU.add, r=['ekf', 'rowf'], w=['idxd_f'])
            P.copy('dve', idxg[:], idxg_f[:], r=['idxg_f'], w=['idxg'])
            P.copy('dve', idxd[:], idxd_f[:], r=['idxd_f'], w=['idxd'])
            for r_ in range(2):
                P.tt('dve', big[:], pos[:], bc(tl[:, 2, :].unsqueeze(1), [128, NTT, 8]), ALU.add, r=['pos', 'tl2'], w=['big'])
                P.tt('dve', big[:], big[:], m12[:, r_, :, :], ALU.mult, r=['big', 'm12a', 'm12b'], w=['big'])
                P.reduce(slf[:, r_ * NTT:(r_ + 1) * NTT], big[:], ALU.add, r=['big'], w=['slf'])
                P.copy('dve', rowd[:, r_, :, 0], tokid[:], r=['tokid'], w=['rowd'])
                P.ts('dve', rowd[:, r_, :, 1], tokid[:], float(r_ * 2 * SEQ), ALU.add, r=['tokid'], w=['rowd'])
                P.copy('dve', rowd[:, r_, :, 2], g12[:, r_, :], r=['g12a', 'g12b'], w=['rowd'])
                P.memset('dve', rowd[:, r_, :, 3], 0.0, w=['rowd'])
            P.copy('dve', sli[:], slf[:], r=['slf'], w=['sli'])
            tab = self.tab
            for r_ in range(2):
                for tt in range(NTT):
                    j = r_ * NTT + tt

                    def sc(e, j=j, r_=r_, tt=tt):
                        return e.indirect_dma_start(out=tab[:, :], out_offset=bass.IndirectOffsetOnAxis(ap=sli[:, j:j + 1], axis=0),
                                                    in_=rowd[:, r_, tt, :], in_offset=None)
                    P.dma_raw('pool', sc, reads=['tab0', 'sli', 'rowd'], writes=['tab_s%d' % j])
            if self.debug:
                self.dump('ek_i', ek_i[:], [128, NKT], I32, reads=['ek_i'])
                self.dump('sli', sli[:], [128, 2 * NTT], I32, reads=['sli'])
                self.dump('run', run[:], [128, 8], F32, reads=['run'])
            P.end_phase()
        if self.upto == 'l1_router':
            return
        with ExitStack() as st:
            act = P.sb('mact', [128, NFT, SLOT_T], BF16, st)
            wbufs = [(P.sb('wg%d' % i, [128, 8, 896], BF16, st), P.sb('wu%d' % i, [128, 8, 896], BF16, st)) for i in range(2)]
            wd = [P.sb('mwd%d' % i, [128, 14, D], BF16, st) for i in range(2)]
            pgs = [P.bank('pg%d' % i, F32, st) for i in range(2)]
            pus = [P.bank('pu%d' % i, F32, st) for i in range(2)]
            pys = [P.bank('py%d' % i, F32, st) for i in range(2)]
            ptg = P.bank('ptg', BF16, st)
            ptgv = ptg[:, :].rearrange("p (c n) -> p c n", c=8)
            sgs = [P.sb('sg%d' % i, [128, 512], F32, st) for i in range(2)]
            tabt = [P.sb('tabt%d' % i, [128, 4, 4], F32, st) for i in range(2)]
            srci = [P.sb('srci%d' % i, [128, 4], I32, st) for i in range(2)]
            dsti = [P.sb('dsti%d' % i, [128, 4], I32, st) for i in range(2)]
            hg = P.sb('hg', [128, 4, D], BF16, st)
            hTg = [P.sb('hTg%d' % i, [128, 8, SLOT_T], BF16, st) for i in range(2)]
            ysb = P.sb('ysb', [128, 4, D], F32, st)
            h_d, y12 = self.h_d, self.y12
            wg4 = I['w_moe_gate'].rearrange("e r (q f) -> (e r q) f", q=4)
            wu4 = I['w_moe_up'].rearrange("e r (q f) -> (e r q) f", q=4)
            wd2 = I['w_moe_down'].rearrange("e r d -> (e r) d")
            ctr = [0, 0]
            iy = 0
            import os
            nkt = int(os.environ.get('MOE_NKT', NKT))
            pending = []

            def fetch(k):
                i2 = k % 2
                P.dma('sp', tabt[i2][:], self.tab[k * SLOT_T:(k + 1) * SLOT_T, :].rearrange("(j p) w -> p j w", p=128),
                      writes=['tabt%d' % i2])
                P.copy('dve', srci[i2][:], tabt[i2][:, :, 0], r=['tabt%d' % i2], w=['srci%d' % i2])
                P.copy('dve', dsti[i2][:], tabt[i2][:, :, 1], r=['tabt%d' % i2], w=['dsti%d' % i2])
                for j in range(4):
                    def ga(e, i2=i2, j=j):
                        return e.indirect_dma_start(out=hgs[i2][:, j, :], out_offset=None, in_=h_d[:, :],
                                                    in_offset=bass.IndirectOffsetOnAxis(ap=srci[i2][:, j:j + 1], axis=0))
                    P.dma_raw('pool', ga, reads=['srci%d' % i2], writes=['hg%d_%d' % (i2, j)])
                for j in range(4):
                    for c in range(8):
                        P.tr(ptgv[:, c, :], hgs[i2][:, j, c * 128:(c + 1) * 128], self.ident_b[:], r=['hg%d_%d' % (i2, j), 'ident_b'],
                             w=['ptg'], inc=(c == 7))
                    P.copy('act' if j % 2 else 'dve', hTg[i2][:, :, j * 128:(j + 1) * 128], ptgv, r=['ptg'], w=['hTg%d_%d' % (i2, j)])

            hgs = [hg, P.sb('hg_b', [128, 4, D], BF16, st)]
            fetch(0)
            for k in range(nkt):
                i2 = k % 2
                hkeys = ['hTg%d_%d' % (i2, j) for j in range(4)]
                for q in range(4):
                    kb = ctr[0] % 2; ctr[0] += 1
                    wg, wu = wbufs[kb]
                    for (wt, src4, nm) in ((wg, wg4, 'wg'), (wu, wu4, 'wu')):
                        for c in range(8):
                            def gw(e, wt=wt, src4=src4, k=k, c=c, q=q):
                                return e.indirect_dma_start(out=wt[:, c, :], out_offset=None, in_=src4[:, :],
                                                            in_offset=bass.IndirectOffsetOnAxis(ap=idxg[:, k, c * 4 + q:c * 4 + q + 1], axis=0))
                            P.dma_raw('pool', gw, reads=['idxg'], writes=['%s%d_%d' % (nm, kb, c)])
                    gk = ['wg%d_%d' % (kb, c) for c in range(8)]
                    uk = ['wu%d_%d' % (kb, c) for c in range(8)]
                    if q == 1:
                        for fn_, rd_, wr_ in pending:
                            P.dma_raw('pool', fn_, reads=rd_, writes=wr_)
                        pending = []
                    for fi in range(7):
                        f = q * 7 + fi
                        jj = ctr[1] % 2; ctr[1] += 1
                        for c in range(8):
                            P.mm(pgs[jj][:], wg[:, c, fi * 128:(fi + 1) * 128], hTg[i2][:, c, :], start=(c == 0), stop=(c == 7),
                                 r=[gk[c]] + hkeys, w=['pg%d' % jj])
                        for c in range(8):
                            P.mm(pus[jj][:], wu[:, c, fi * 128:(fi + 1) * 128], hTg[i2][:, c, :], start=(c == 0), stop=(c == 7),
                                 r=[uk[c]] + hkeys, w=['pu%d' % jj])
                        P.act(sgs[jj][:], pgs[jj][:], AF.Silu, r=['pg%d' % jj], w=['sg%d' % jj])
                        P.tt('dve', act[:, f, :], sgs[jj][:], pus[jj][:], ALU.mult, r=['sg%d' % jj, 'pu%d' % jj], w=['act%d' % f])
                if k + 1 < nkt:
                    fetch(k + 1)
                for fh in range(2):
                    for fl in range(14):
                        f = fh * 14 + fl
                        def gd(e, k=k, f=f, fh=fh, fl=fl):
                            return e.indirect_dma_start(out=wd[fh][:, fl, :], out_offset=None, in_=wd2[:, :],
                                                        in_offset=bass.IndirectOffsetOnAxis(ap=idxd[:, k, f:f + 1], axis=0))
                        P.dma_raw('pool', gd, reads=['idxd'], writes=['mwd%d_%d' % (fh, fl)])
                    for j in range(4):
                        for dh in range(2):
                            ip = iy % 2; iy += 1
                            for fl in range(14):
                                P.mm(pys[ip][:], act[:, fh * 14 + fl, j * 128:(j + 1) * 128], wd[fh][:, fl, dh * 512:(dh + 1) * 512],
                                     start=(fl == 0), stop=(fl == 13), r=['act%d' % (fh * 14 + fl), 'mwd%d_%d' % (fh, fl)], w=['py%d' % ip])
                            ys = ysb[:, j, dh * 512:(dh + 1) * 512]
                            yk = 'ysb%d_%d' % (j, dh)
                            if fh == 0:
                                P.ts('dve', ys, pys[ip][:], tabt[i2][:, j, 2:3], ALU.mult, r=['py%d' % ip, 'tabt%d' % i2], w=[yk])
                            else:
                                P.stt(ys, pys[ip][:], tabt[i2][:, j, 2:3], ys, ALU.mult, ALU.add, r=['py%d' % ip, 'tabt%d' % i2, yk], w=[yk])
                for j in range(4):
                    def scy(e, i2=i2, j=j):
                        return e.indirect_dma_start(out=y12[:, :], out_offset=bass.IndirectOffsetOnAxis(ap=dsti[i2][:, j:j + 1], axis=0),
                                                    in_=ysb[:, j, :], in_offset=None)
                    pending.append((scy, ['dsti%d' % i2, 'ysb%d_0' % j, 'ysb%d_1' % j], ['y12_%d_%d' % (k, j)]))
            for fn_, rd_, wr_ in pending:
                P.dma_raw('pool', fn_, reads=rd_, writes=wr_)
            P.end_phase()
        if self.upto == 'l1_experts':
            return
        with ExitStack() as st:
            G2 = [P.sb('G2f%d' % b, [128, D], F32, st) for b in range(2)]
            gfin = P.sb('gfin', [128, D], F32, st)
            for b in range(2):
                self.load_gate(G2[b], 'G2f%d' % b, 1, b, 2)
            P.dma('sp', gfin[:], I['g_final'][0:1, :].partition_broadcast(128), writes=['gfin'])
            xc = [P.sb('xc%d' % i, [128, D], F32, st) for i in range(2)]
            y1 = [P.sb('y1_%d' % i, [128, D], F32, st) for i in range(2)]
            y2 = [P.sb('y2_%d' % i, [128, D], F32, st) for i in range(2)]
            tmp = [P.sb('ftmp%d' % i, [128, D], F32, st) for i in range(2)]
            xo = [P.sb('fxo%d' % i, [128, D], F32, st) for i in range(2)]
            junk = P.sb('fjunk', [128, D], BF16, st)
            ss = P.sb('fss', [128, 2, 2], F32, st)
            ot = [P.sb('fot%d' % i, [128, D], F32, st) for i in range(2)]
            for tt in range(NTT):
                b, t = tt // 16, tt % 16
                i2 = tt % 2
                P.dma('sp', xc[i2][:], self.resC[b, t * 128:(t + 1) * 128, :], writes=['xc%d' % i2])
                P.dma('sp', y1[i2][:], self.y12[tt * 128:(tt + 1) * 128, :], writes=['y1_%d' % i2])
                P.dma('sp', y2[i2][:], self.y12[2 * SEQ + tt * 128:2 * SEQ + (tt + 1) * 128, :], writes=['y2_%d' % i2])
                P.tt('pool', y1[i2][:], y1[i2][:], y2[i2][:], ALU.add, r=['y1_%d' % i2, 'y2_%d' % i2], w=['y1_%d' % i2])
                P.tt('dve', tmp[i2][:], y1[i2][:], G2[b][:], ALU.mult, r=['y1_%d' % i2, 'G2f%d' % b], w=['ftmp%d' % i2])
                P.tt('pool', xo[i2][:], tmp[i2][:], xc[i2][:], ALU.add, r=['ftmp%d' % i2, 'xc%d' % i2], w=['fxo%d' % i2])
                P.sumsq(junk[:], xo[i2][:], ss[:, i2, 0:1], r=['fxo%d' % i2], w=['fjunk', 'fss0_%d' % i2])
                P.act(ss[:, i2, 1:2], ss[:, i2, 0:1], AF.Sqrt, r=['fss0_%d' % i2, 'epsb'], w=['fss1_%d' % i2], scale=1.0 / D, bias=self.epsb[:, 0:1])
                P.recip(ss[:, i2, 0:1], ss[:, i2, 1:2], r=['fss1_%d' % i2], w=['fss0_%d' % i2])
                P.stt(ot[i2][:], xo[i2][:], ss[:, i2, 0:1], gfin[:], ALU.mult, ALU.mult, r=['fxo%d' % i2, 'fss0_%d' % i2, 'gfin'], w=['fot%d' % i2])
                P.dma('act', self.out[b, t * 128:(t + 1) * 128, :], ot[i2][:], reads=['fot%d' % i2], writes=['out'])
            P.end_phase()


Builder.layer1_moe_sparse = _layer1_moe_sparse
Builder.l0_ffn = _l0_ffn
Builder.layer1_mixer = _layer1_mixer
Builder.layer1_moe = _layer1_moe
Builder._moe_gate_up = _moe_gate_up


def build(upto='all', debug=False):
    import os
    phases = {'all': ('l0a', 'l0f', 'l1a', 'l1f'), 'l0f_only': ('l0f',), 'l1a_only': ('l1a',), 'l1f_only': ('l1f',),
              'l1_router': ('l1f',), 'l1_experts': ('l1f',), 'l0': ('l0a', 'l0f'), 'mod': ()}.get(upto, ('l0a',))
    preload = {'l0f_only': ['resA'], 'l1a_only': ['resB'], 'l1f_only': ['resC'], 'l1_router': ['resC'], 'l1_experts': ['resC']}.get(upto, [])
    B = Builder(upto, debug, preload)
    B.declare()
    B.consts()
    B._ptb_bank = B.P.bank('ptb', BF16)
    B._ptb = B._ptb_bank[:, 0:512].rearrange("p (h d) -> p h d", d=128)
    B.phase_mod()
    nb = int(os.environ.get('NB', 2))
    if 'l0a' in phases:
        for b in range(nb):
            B.layer0_mixer(b)
            if upto in ('l0a_b0', 'l0_norm', 'l0_inproj', 'l0_gqa', 'l0_gla'):
                break
    if 'l0f' in phases:
        for b in range(nb):
            for half in range(2):
                B.l0_ffn(b, half)
    if 'l1a' in phases:
        for b in range(nb):
            B.layer1_mixer(b)
    if 'l1f' in phases:
        if os.environ.get('MOE_DENSE'):
            for b in range(nb):
                B.layer1_moe(b)
        else:
            B.layer1_moe_sparse()
    B.P.end_phase()
    return B


def _rope_tables():
    t = np.arange(SEQ, dtype=np.int32)
    row = (t // 64).astype(np.float32)
    col = (t % 64).astype(np.float32)
    n_axis = 16
    inv = np.power(np.float32(10000.0), -np.arange(n_axis, dtype=np.float32) / np.float32(n_axis)).astype(np.float32)
    ang = np.concatenate([row[:, None] * inv, col[:, None] * inv], axis=-1).astype(np.float32)
    cos = np.cos(ang).astype(np.float32)
    sin = np.sin(ang).astype(np.float32)
    C = np.repeat(cos, 2, axis=1)
    S = np.stack([-sin, sin], axis=-1).reshape(SEQ, 64)
    C = C.reshape(16, 128, 64).transpose(1, 0, 2)
    S = S.reshape(16, 128, 64).transpose(1, 0, 2)
    return np.ascontiguousarray(C), np.ascontiguousarray(S)


def _na_bias(rpb):
    NEG = np.float32(-30000.0)
    cols = np.arange(64)
    col_start = np.clip(cols - 8, 0, 48)
    in_win = (cols[None, :] >= col_start[:, None]) & (cols[None, :] < col_start[:, None] + 16)
    col_idx = np.clip(cols[None, :] - cols[:, None] + 15, 0, 30)
    dlist = list(range(-7, 7, 2)) + list(range(-6, 8, 2))
    out = np.empty((16, 128, 14, 64), np.float32)
    for di, d in enumerate(dlist):
        for i in range(2):
            dr = d + i + 7
            dr_c = min(max(dr, 0), 14)
            blk = rpb[:, dr_c][:, col_idx]
            blk = np.where(in_win[None], blk, NEG)
            out[:, 64 * i:64 * (i + 1), di, :] = blk.transpose(0, 2, 1)
    return np.ascontiguousarray(out.reshape(16, 128, 14 * 64))


def prep_inputs(inp, ncores=NCORES):
    f = lambda a: np.ascontiguousarray(np.asarray(a, dtype=np.float32))
    shared = {}
    shared['w_mod'] = f(inp['w_mod']); shared['b_mod'] = f(inp['b_mod'])
    gn = np.stack([f(inp['g_norm1']), f(inp['g_norm2'])], axis=1)
    shared['gnT'] = np.ascontiguousarray(gn.reshape(2, 2, 8, 128).transpose(3, 0, 1, 2))
    shared['w_in_ab'] = f(inp['w_in_ab'][0]); shared['g_q'] = f(inp['g_q']); shared['g_k'] = f(inp['g_k'])
    shared['w2f'] = f(np.concatenate([inp['w_a2_f'][0], inp['b_a_f'][0][None]], 0))
    shared['w2b'] = f(np.concatenate([inp['w_a2_b'][0], inp['b_a_b'][0][None]], 0))
    shared['g_gla'] = f(inp['g_gla'])
    shared['w_out_ab'] = f(inp['w_out_ab'][0])
    shared['w_ff_gate'] = f(inp['w_ff_gate'][0]); shared['w_ff_up'] = f(inp['w_ff_up'][0]); shared['w_ff_down'] = f(inp['w_ff_down'][0])
    shared['w_in_c'] = f(inp['w_in_c'][0]); shared['nabias'] = _na_bias(f(inp['rpb_c'][0])); shared['w_out_c'] = f(inp['w_out_c'][0])
    shared['w_router'] = np.ascontiguousarray(f(inp['w_router'][0]).reshape(8, 128, NEXP).transpose(1, 0, 2))
    shared['w_moe_gate'] = f(inp['w_moe_gate'][0]); shared['w_moe_up'] = f(inp['w_moe_up'][0]); shared['w_moe_down'] = f(inp['w_moe_down'][0])
    shared['g_final'] = f(inp['g_final']).reshape(1, D)
    shared['ident'] = np.eye(128, dtype=np.float32)
    idx = np.arange(128)
    shared['tri_f'] = (idx[:, None] <= idx[None, :]).astype(np.float32)
    shared['tri_b'] = (idx[:, None] >= idx[None, :]).astype(np.float32)
    shared['ropeC'], shared['ropeS'] = _rope_tables()
    shared['stri'] = (idx[:, None] < idx[None, :]).astype(np.float32)
    shared['thr'] = np.ascontiguousarray(np.broadcast_to((np.arange(8, dtype=np.float32) * SLOT_T)[None, None, :], (128, 8, 8)).reshape(128, 64))
    shared['kv'] = np.ascontiguousarray(np.broadcast_to(np.arange(NKT, dtype=np.float32)[None, :, None], (128, NKT, 8)).reshape(128, NKT * 8))
    shared['tokid'] = np.ascontiguousarray((np.arange(32, dtype=np.float32)[None, :] * 128 + np.arange(128, dtype=np.float32)[:, None]))
    dfl = np.zeros((NSLOT, 4), np.float32)
    dfl[:, 1] = 4 * SEQ + np.arange(NSLOT, dtype=np.float32)
    shared['dflt'] = np.ascontiguousarray(dfl.reshape(128, -1))
    shared['gn2_nat'] = f(inp['g_norm2'][1]).reshape(1, D)
    pp = np.arange(128, dtype=np.float32)[:, None, None]
    shared['rowc4'] = np.ascontiguousarray((4.0 * (np.arange(8, dtype=np.float32)[None, :, None] * 128 + pp)
                                            + np.arange(4, dtype=np.float32)[None, None, :]).reshape(128, 32))
    shared['rowf'] = np.ascontiguousarray(np.arange(NFT, dtype=np.float32)[None, :] * 128 + np.arange(128, dtype=np.float32)[:, None])
    x = f(inp['x']); ctx = f(inp['ctx']); c = f(inp['c']); cc = f(inp['c_ctx'])
    maps = []
    for k in range(ncores):
        m = dict(shared)
        m['x'] = x[2 * k:2 * k + 2]
        m['ctx'] = ctx[2 * k:2 * k + 2]
        cond = np.stack([c[2 * k], c[2 * k + 1], cc], axis=1)
        m['condT'] = np.ascontiguousarray(cond.reshape(8, 128, 3).transpose(1, 0, 2))
        maps.append(m)
    return maps


_CACHE = {}


def kernel(**inputs):
    if 'B' not in _CACHE:
        _CACHE['B'] = build()
    B = _CACHE['B']
    maps = prep_inputs(inputs)
    maps = [{k: v for k, v in m.items() if k in B.I} for m in maps]
    res = run_bass_kernel_spmd(B.nc, maps, core_ids=list(range(NCORES)))
    out = np.concatenate([np.asarray(r['out']) for r in res.results], axis=0)
    return out.astype(np.float32)
```

```python
import numpy as np
from contextlib import ExitStack
import concourse.bass as bass
import concourse.mybir as mybir
from concourse.bass_utils import run_bass_kernel_spmd

F32 = mybir.dt.float32
BF16 = mybir.dt.bfloat16
AF = mybir.ActivationFunctionType
ALU = mybir.AluOpType
AX = mybir.AxisListType

CENG = ('pe', 'act', 'dve', 'pool')
ENG = ('pe', 'act', 'dve', 'pool', 'sp')
NDSEM = 8
EPS = 1e-6
NCORES = 8
D = 1024
SEQ = 2048
NCTX = 256
TOT = SEQ + NCTX
NT = TOT // 128
DFF = 3584
NFT = DFF // 128
ABW = 2336
NEXP = 8
SLOT_T = 512
NKT = 24
NSLOT = NKT * SLOT_T
BIGIDX = 1.0e6
I32 = mybir.dt.int32


class Prog:
    def __init__(self, nc):
        self.nc = nc
        self.stack = ExitStack()
        self.ops = {e: [] for e in ENG}
        self.cnt = {e: 0 for e in CENG}
        self.pending_inc = {e: False for e in CENG}
        self.clock = {e: {} for e in ENG}
        self.lastw = {}
        self.readers = {}
        self.dma_rr = {q: 0 for q in ('sp', 'act', 'pool')}
        self.dma_cnt = {}
        self.dma_last = {}
        self.sems = {}
        for e in CENG:
            self.sems['c_' + e] = self.stack.enter_context(nc.semaphore('c_' + e))
        for q in ('sp', 'act', 'pool'):
            for i in range(NDSEM):
                n = 'd_%s_%d' % (q, i)
                self.sems[n] = self.stack.enter_context(nc.semaphore(n))
                self.dma_cnt[n] = 0
        self._cur = None
        self.psum_keys = set()

    def _uname(self, name):
        self._uid = getattr(self, '_uid', 0) + 1
        return 's%d_%s' % (self._uid, name)

    def sb(self, name, shape, dtype, stack=None):
        return (stack or self.stack).enter_context(self.nc.sbuf_tensor(self._uname(name), list(shape), dtype))

    def ps(self, name, shape, dtype, stack=None):
        self.psum_keys.add(name)
        return (stack or self.stack).enter_context(self.nc.psum_tensor(self._uname(name), list(shape), dtype))

    def bank(self, name, dtype, stack=None):
        return self.ps(name, [128, 512] if dtype == F32 else [128, 1024], dtype, stack)

    def _need(self, eng, ev):
        s, v, origin, clk = ev
        c = self.clock[eng]
        if c.get(s, 0) >= v:
            return
        self.ops[eng].append(('wait', s, v))
        c[s] = v
        for s2, v2 in clk.items():
            if c.get(s2, 0) < v2:
                c[s2] = v2

    def _deps(self, eng, reads, writes, is_dma=False):
        for r in reads:
            ev = self.lastw.get(r)
            if ev is not None:
                if (not is_dma) and eng == 'pe' and ev[2] == 'pe':
                    continue
                self._need(eng, ev)
            if r in self.psum_keys:
                rd = self.readers.get(r)
                if rd:
                    for ev2 in list(rd.values()):
                        if ev2[2] != eng:
                            self._need(eng, ev2)
        for w in writes:
            ev = self.lastw.get(w)
            if ev is not None and (is_dma or ev[2] != eng or eng != 'pe'):
                self._need(eng, ev)
            rd = self.readers.get(w)
            if rd:
                for ev in rd.values():
                    if is_dma or ev[2] != eng or eng != 'pe':
                        self._need(eng, ev)

    def _record(self, ev, reads, writes):
        for w in writes:
            self.lastw[w] = ev
            self.readers[w] = {}
        for r in reads:
            d = self.readers.setdefault(r, {})
            old = d.get(ev[0])
            if old is None or old[1] < ev[1]:
                d[ev[0]] = ev

    def op(self, eng, fn, reads=(), writes=(), inc=True):
        self._deps(eng, reads, writes)
        v = self.cnt[eng] + 1
        if inc:
            self.cnt[eng] = v
            self.pending_inc[eng] = False
        else:
            self.pending_inc[eng] = True
        ev = ('c_' + eng, v, eng, dict(self.clock[eng]))
        self._record(ev, reads, writes)
        self.ops[eng].append(('op', fn, inc))
        return ev

    def dma(self, q, out, in_, reads=(), writes=(), **kw):
        self._deps(q, reads, writes, is_dma=True)
        i = self.dma_rr[q]
        self.dma_rr[q] = (i + 1) % NDSEM
        n = 'd_%s_%d' % (q, i)
        last = self.dma_last.get(n)
        if last is not None:
            self._need(q, last)
        v = self.dma_cnt[n] + 16
        self.dma_cnt[n] = v
        ev = (n, v, 'dma', dict(self.clock[q]))
        self.dma_last[n] = ev
        self._record(ev, reads, writes)
        self.ops[q].append(('dma', out, in_, n, kw))
        return ev

    def dma_raw(self, q, fn, reads=(), writes=()):
        self._deps(q, reads, writes, is_dma=True)
        i = self.dma_rr[q]
        self.dma_rr[q] = (i + 1) % NDSEM
        n = 'd_%s_%d' % (q, i)
        last = self.dma_last.get(n)
        if last is not None:
            self._need(q, last)
        v = self.dma_cnt[n] + 16
        self.dma_cnt[n] = v
        ev = (n, v, 'dma', dict(self.clock[q]))
        self.dma_last[n] = ev
        self._record(ev, reads, writes)
        self.ops[q].append(('rawdma', fn, n))
        return ev

    def barrier(self):
        evs = []
        for e in CENG:
            assert not self.pending_inc[e], e
            if self.cnt[e] > 0:
                evs.append(('c_' + e, self.cnt[e], e, {}))
        for n, ev in self.dma_last.items():
            evs.append(ev)
        for e in ENG:
            for ev in evs:
                self._need(e, ev)
        self.lastw = {}
        self.readers = {}

    def flush(self):
        ops = self.ops
        sems = self.sems

        def emit(e, name):
            for o in ops[name]:
                if o[0] == 'wait':
                    e.wait_ge(sems[o[1]], o[2])
                elif o[0] == 'op':
                    ins = o[1](e)
                    if o[2]:
                        ins.then_inc(sems['c_' + name], 1)
                elif o[0] == 'rawdma':
                    o[1](e).then_inc(sems[o[2]], 16)
                else:
                    _, out, in_, n, kw = o
                    e.dma_start(out=out, in_=in_, **kw).then_inc(sems[n], 16)

        with self.nc.Block() as block:
            @block.tensor
            def _(e):
                emit(e, 'pe')

            @block.scalar
            def _(e):
                emit(e, 'act')

            @block.vector
            def _(e):
                emit(e, 'dve')

            @block.gpsimd
            def _(e):
                emit(e, 'pool')

            @block.sync
            def _(e):
                emit(e, 'sp')
        self.ops = {e: [] for e in ENG}

    def end_phase(self):
        self.barrier()
        self.flush()

    def mm(self, out, lhsT, rhs, start=True, stop=True, r=(), w=(), inc=None):
        if inc is None:
            inc = stop
        return self.op('pe', lambda e: e.matmul(out, lhsT=lhsT, rhs=rhs, start=start, stop=stop,
                                                skip_group_check=True), r, w, inc)

    def tr(self, out, in_, ident, r=(), w=(), inc=True):
        return self.op('pe', lambda e: e.transpose(out=out, in_=in_, identity=ident), r, w, inc)

    def act(self, out, in_, func, r=(), w=(), scale=None, bias=None, eng='act'):
        kw = {}
        if scale is not None:
            kw['scale'] = scale
        if bias is not None:
            kw['bias'] = bias
        return self.op('act', lambda e: e.activation(out=out, in_=in_, func=func, **kw), r, w)

    def tt(self, eng, out, in0, in1, op, r=(), w=()):
        return self.op(eng, lambda e: e.tensor_tensor(out=out, in0=in0, in1=in1, op=op), r, w)

    def ts(self, eng, out, in0, s1, op0, s2=None, op1=None, r=(), w=()):
        if op1 is None:
            return self.op(eng, lambda e: e.tensor_scalar(out=out, in0=in0, scalar1=s1, scalar2=None, op0=op0), r, w)
        return self.op(eng, lambda e: e.tensor_scalar(out=out, in0=in0, scalar1=s1, scalar2=s2, op0=op0, op1=op1), r, w)

    def stt(self, out, in0, scalar, in1, op0, op1, r=(), w=()):
        return self.op('dve', lambda e: e.scalar_tensor_tensor(out=out, in0=in0, scalar=scalar, in1=in1,
                                                               op0=op0, op1=op1), r, w)

    def copy(self, eng, out, in_, r=(), w=()):
        if eng == 'act':
            return self.op('act', lambda e: e.copy(out=out, in_=in_), r, w)
        return self.op(eng, lambda e: e.tensor_copy(out=out, in_=in_), r, w)

    def recip(self, out, in_, r=(), w=()):
        return self.op('dve', lambda e: e.reciprocal(out=out, in_=in_), r, w)

    def memset(self, eng, ap, val, w=()):
        return self.op(eng, lambda e: e.memset(ap, val), (), w)

    def reduce(self, out, in_, op, r=(), w=()):
        return self.op('dve', lambda e: e.tensor_reduce(out=out, in_=in_, axis=AX.X, op=op), r, w)

    def sumsq(self, junk, in_, acc, r=(), w=()):
        return self.op('dve', lambda e: e.scalar_tensor_tensor(out=junk, in0=in_, scalar=1.0, in1=in_,
                                                               op0=ALU.mult, op1=ALU.mult, accum_out=acc), r, w)


def bc(ap, shape):
    return ap.to_broadcast(list(shape))


class Builder:
    def __init__(self, upto='all', debug=False, preload=()):
        self.preload = set(preload)
        self.upto = upto
        self.debug = debug
        nc = bass.Bass("TRN2", target_bir_lowering=False)
        self.nc = nc
        self.P = Prog(nc)
        self.I = {}
        self.dbg = {}

    def inp(self, name, shape, dtype=F32):
        t = self.nc.dram_tensor(name, list(shape), dtype, kind="ExternalInput").ap()
        self.I[name] = t
        return t

    def dump(self, name, ap, shape, dtype=F32, reads=()):
        if not self.debug:
            return
        t = self.nc.dram_tensor('dbg_' + name, list(shape), dtype, kind="ExternalOutput").ap()
        self.dbg[name] = t
        idx = tuple(slice(None) for _ in shape)
        self.P.dma('sp', t[idx], ap, reads=list(reads), writes=['dbg_' + name])

    def scratch(self, name, shape, dtype=F32):
        if name in self.preload:
            return self.inp(name, shape, dtype)
        if self.debug:
            t = self.nc.dram_tensor(name, list(shape), dtype, kind="ExternalOutput").ap()
            self.dbg[name] = t
            return t
        return self.nc.dram_tensor(name, list(shape), dtype).ap()

    def declare(self):
        inp = self.inp
        inp('x', [2, SEQ, D]); inp('ctx', [2, NCTX, D]); inp('condT', [128, 8, 3])
        inp('w_mod', [2, D, 6 * D]); inp('b_mod', [2, 6 * D]); inp('gnT', [128, 2, 2, 8])
        inp('w_in_ab', [D, ABW]); inp('g_q', [1, 64]); inp('g_k', [1, 64])
        inp('w2f', [17, 256]); inp('w2b', [17, 256]); inp('g_gla', [1, 128])
        inp('w_out_ab', [D, D]); inp('w_ff_gate', [D, DFF]); inp('w_ff_up', [D, DFF]); inp('w_ff_down', [DFF, D])
        inp('w_in_c', [D, 3 * D]); inp('nabias', [16, 128, 14 * 64]); inp('w_out_c', [D, D])
        inp('w_router', [128, 8, NEXP])
        inp('w_moe_gate', [NEXP, D, DFF]); inp('w_moe_up', [NEXP, D, DFF]); inp('w_moe_down', [NEXP, DFF, D])
        inp('g_final', [1, D])
        inp('ident', [128, 128]); inp('tri_f', [128, 128]); inp('tri_b', [128, 128])
        inp('ropeC', [128, 16, 64]); inp('ropeS', [128, 16, 64])
        inp('stri', [128, 128]); inp('thr', [128, 64]); inp('kv', [128, NKT * 8]); inp('tokid', [128, 32])
        inp('dflt', [128, (NSLOT // 128) * 4]); inp('gn2_nat', [1, D])
        inp('rowc4', [128, 32]); inp('rowf', [128, NFT])
        self.out = self.nc.dram_tensor('out', [2, SEQ, D], F32, kind="ExternalOutput").ap()
        sc = self.scratch
        self.mod_d = sc('mod_d', [2, 3, 6, D])
        self.resA = sc('resA', [2, TOT, D])
        self.resB = sc('resB', [2, TOT, D])
        self.resC = sc('resC', [2, SEQ, D])
        self.qkb_d = sc('qkb_d', [2, TOT, 512])
        self.vb_d = sc('vb_d', [2, TOT, 512], BF16)
        self.rb_d = sc('rb_d', [2, TOT, 512])
        self.h_d = sc('h_d', [2 * SEQ, D], BF16)
        self.tab = sc('tab', [NSLOT, 4])
        self.y12 = sc('y12', [4 * SEQ + NSLOT, D])

    def consts(self):
        P, I = self.P, self.I
        self.ident_f = P.sb('ident_f', [128, 128], F32)
        self.ident_b = P.sb('ident_b', [128, 128], BF16)
        self.tri = {'f': P.sb('tri_f', [128, 128], F32), 'b': P.sb('tri_b', [128, 128], F32)}
        self.ones_f = P.sb('ones_f', [128, 8], F32)
        self.modAB = P.sb('modAB', [128, 2, 3, 2, 2, 8], F32)
        self.epsb = P.sb('epsb', [128, 1], F32)
        P.dma('sp', self.ident_f[:], I['ident'][:, :], writes=['ident_f'])
        P.dma('sp', self.tri['f'][:], I['tri_f'][:, :], writes=['tri_f'])
        P.dma('sp', self.tri['b'][:], I['tri_b'][:, :], writes=['tri_b'])
        P.copy('dve', self.ident_b[:], self.ident_f[:], r=['ident_f'], w=['ident_b'])
        P.memset('dve', self.ones_f[:], 1.0, w=['ones_f'])
        P.memset('dve', self.epsb[:], EPS, w=['epsb'])

    def phase_mod(self):
        P, I = self.P, self.I
        with ExitStack() as st:
            condT = P.sb('condT', [128, 8, 3], F32, st)
            scond = P.sb('scond', [128, 8, 3], F32, st)
            bmod = P.sb('bmod', [3, 6 * D], F32, st)
            mt = P.sb('mt', [3, 6 * D], F32, st)
            wbuf = [P.sb('wm%d' % i, [128, 8, 512], F32, st) for i in range(3)]
            psm = [P.ps('psm%d' % i, [128, 512], F32, st) for i in range(2)]
            modF = P.sb('modF', [128, 2, 3, 6, 8], F32, st)
            gn = P.sb('gn', [128, 2, 2, 8], F32, st)
            P.dma('sp', condT[:], I['condT'][:, :, :], writes=['condT'])
            P.dma('sp', gn[:], I['gnT'][:, :, :, :], writes=['gn'])
            P.act(scond[:], condT[:], AF.Silu, r=['condT'], w=['scond'])
            k = 0
            for i in range(2):
                P.dma('sp', bmod[:], I['b_mod'][i:i + 1, :].partition_broadcast(3), writes=['bmod'])
                wv = I['w_mod'][i].rearrange("(c p) f -> p c f", p=128)
                for n in range(12):
                    wb = wbuf[k % 3]
                    wk = 'wm%d' % (k % 3)
                    P.dma('sp' if k % 2 == 0 else 'act', wb[:], wv[:, :, n * 512:(n + 1) * 512], writes=[wk])
                    pk = 'psm%d' % (k % 2)
                    for c in range(8):
                        P.mm(psm[k % 2][0:3, :], scond[:, c, :], wb[:, c, :], start=(c == 0), stop=(c == 7),
                             r=['scond', wk], w=[pk])
                    P.tt('dve', mt[:, n * 512:(n + 1) * 512], psm[k % 2][0:3, :], bmod[:, n * 512:(n + 1) * 512],
                         ALU.add, r=[pk, 'bmod'], w=['mt'])
                    k += 1
                P.dma('sp', self.mod_d[i].rearrange("j k d -> j (k d)"), mt[:], reads=['mt'], writes=['mod_d'])
                for j in range(3):
                    P.dma('sp', modF[:, i, j, :, :], self.mod_d[i, j].rearrange("k (c p) -> p k c", p=128),
                          reads=['mod_d'], writes=['modF'], allow_slow_non_contiguous=True)
            for i in range(2):
                for j in range(3):
                    for n in range(2):
                        P.stt(self.modAB[:, i, j, n, 0, :], modF[:, i, j, 1 + 3 * n, :], 1.0, gn[:, i, n, :],
                              ALU.add, ALU.mult, r=['modF', 'gn'], w=['modAB'])
                        P.copy('dve', self.modAB[:, i, j, n, 1, :], modF[:, i, j, 3 * n, :], r=['modF'], w=['modAB'])
            P.end_phase()

    def load_gate(self, tile, key, layer, cond, which, q='sp'):
        k = 2 if which == 1 else 5
        self.P.dma(q, tile[:], self.mod_d[layer, cond, k:k + 1, :].partition_broadcast(128),
                   reads=['mod_d'], writes=[key])

    def norm_mod(self, st, tag, src_fn, ntiles, cond_fn, layer, norm, hT, hkey, want32=None, tm_cb=None, post_cb=None):
        P = self.P
        f32path = want32 is not None
        xt = [P.sb('%s_xt%d' % (tag, i), [128, D], F32, st) for i in range(3)]
        junk = P.sb(tag + '_junk', [128, D], BF16, st)
        ss = P.sb(tag + '_ss', [128, 2], F32, st)
        dt_n = F32 if f32path else BF16
        xn = [P.sb('%s_xn%d' % (tag, i), [128, D], dt_n, st) for i in range(2)]
        nps = 1 if f32path else 2
        pst = [P.ps('%s_pst%d' % (tag, i), [128, 8, 128], dt_n, st) for i in range(nps)]
        tm = [P.sb('%s_tm%d' % (tag, i), [128, 8, 128], F32, st) for i in range(2)]
        h32 = [P.sb('%s_h32_%d' % (tag, i), [128, 8, 128], F32, st) for i in range(2)] if f32path else None
        ident = self.ident_f if f32path else self.ident_b
        ikey = 'ident_f' if f32path else 'ident_b'
        def stage_a(t):
            x_ = xt[t % 3]; xk = '%s_xt%d' % (tag, t % 3)
            P.dma('sp', x_[:], src_fn(t), reads=['src_' + tag], writes=[xk])
            sk = tag + '_ss'
            P.sumsq(junk[:], x_[:], ss[:, 0:1], r=[xk], w=[tag + '_junk', sk + '0'])
            P.act(ss[:, 1:2], ss[:, 0:1], AF.Sqrt, r=[sk + '0', 'epsb'], w=[sk + '1'], scale=1.0 / D, bias=self.epsb[:, 0:1])
            P.recip(ss[:, 0:1], ss[:, 1:2], r=[sk + '1'], w=[sk + '0'])
            n_ = xn[t % 2]; nk = '%s_xn%d' % (tag, t % 2)
            P.act(n_[:], x_[:], AF.Identity, r=[xk, sk + '0'], w=[nk], scale=ss[:, 0:1])
            if tm_cb is not None:
                tm_cb(t, n_, nk)

        def stage_b(t):
            n_ = xn[t % 2]; nk = '%s_xn%d' % (tag, t % 2)
            ps_ = pst[t % nps]; pk = '%s_pst%d' % (tag, t % nps)
            for c in range(8):
                P.tr(ps_[:, c, :], n_[:, c * 128:(c + 1) * 128], ident[:], r=[nk, ikey], w=[pk], inc=(c == 7))
            cond = cond_fn(t)
            A = self.modAB[:, layer, cond, norm, 0, :]
            B = self.modAB[:, layer, cond, norm, 1, :]
            tm_ = tm[t % 2]; tk = '%s_tm%d' % (tag, t % 2)
            P.tt('dve', tm_[:], ps_[:], bc(A.unsqueeze(2), [128, 8, 128]), ALU.mult, r=[pk, 'modAB'], w=[tk])
            if f32path:
                h_ = h32[t % 2]; hk32 = '%s_h32_%d' % (tag, t % 2)
                P.tt('pool', h_[:], tm_[:], bc(B.unsqueeze(2), [128, 8, 128]), ALU.add, r=[tk, 'modAB'], w=[hk32])
                if hT is not None:
                    P.copy('act', hT[:, :, t * 128:(t + 1) * 128], h_[:], r=[hk32], w=[hkey + str(t)])
                want32(t, h_, hk32)
            else:
                P.tt('pool', hT[:, :, t * 128:(t + 1) * 128], tm_[:], bc(B.unsqueeze(2), [128, 8, 128]), ALU.add,
                     r=[tk, 'modAB'], w=[hkey + str(t)])

        for t in range(ntiles):
            stage_a(t)
            if t >= 1:
                stage_b(t - 1)
                if post_cb is not None:
                    post_cb(t - 1)
        stage_b(ntiles - 1)
        if post_cb is not None:
            post_cb(ntiles - 1)


    def layer0_mixer(self, b):
        P = self.P
        with ExitStack() as st0:
            o_tm = P.sb('o_tm', [128, NT, 512], BF16, st0)
            zaug = {d: P.sb('zaug_' + d, [17, TOT], F32, st0) for d in 'fb'}
            with ExitStack() as st1:
                qT = P.sb('qT', [64, 8, TOT], BF16, st1)
                kT = P.sb('kT', [64, 2, TOT], BF16, st1)
                v_sb = P.sb('v_sb', [128, NT, 2, 65], BF16, st1)
                with ExitStack() as st1a:
                    self.l0_inproj(b, st1a, qT, kT, v_sb, zaug)
                if self.upto in ('l0_norm', 'l0_inproj'):
                    return
                with ExitStack() as st1b:
                    self.l0_gqa(b, st1b, qT, kT, v_sb, o_tm)
                if self.upto == 'l0_gqa':
                    self.dump('o_tm', o_tm[:], [128, NT, 512], BF16)
                    P.end_phase()
                    return
            with ExitStack() as st2:
                oT = P.sb('oT', [128, 8, TOT], BF16, st2)
                with ExitStack() as st2a:
                    self.l0_gla(b, st2a, o_tm, zaug, oT)
                if self.upto == 'l0_gla':
                    self.dump('oT', oT[:], [128, 8, TOT], BF16)
                    P.end_phase()
                    return
                with ExitStack() as st2b:
                    self.l0_outproj(b, st2b, oT)

    def l0_inproj(self, b, st, qT, kT, v_sb, zaug):
        P, I = self.P, self.I
        hT = P.sb('hT', [128, 8, TOT], BF16, st)
        with ExitStack() as stn:
            def src(t):
                return I['ctx'][b, t * 128:(t + 1) * 128, :] if t < 2 else I['x'][b, (t - 2) * 128:(t - 1) * 128, :]
            import os
            self.norm_mod(stn, 'n1', src, int(os.environ.get('NTILES', NT)), lambda t: 2 if t < 2 else b, 0, 0, hT, 'hT')
            P.end_phase()
        if self.upto == 'l0_norm':
            if not os.environ.get('NODUMP'):
                self.dump('hT', hT[:], [128, 8, TOT], BF16)
            P.end_phase()
            return
        hkeys = ['hT%d' % t for t in range(NT)]
        w_in = P.sb('w_in', [128, 8, ABW], BF16, st)
        wv = I['w_in_ab'].rearrange("(c p) f -> p c f", p=128)
        for c in range(8):
            P.dma('pool', w_in[:, c, :], wv[:, c, :], writes=['w_in%d' % c])
        wkeys = ['w_in%d' % c for c in range(8)]
        gqk = P.sb('gqk', [128, 10, 64], F32, st)
        ropeC = P.sb('ropeC', [128, 16, 64], F32, st)
        ropeS = P.sb('ropeS', [128, 16, 64], F32, st)
        P.dma('sp', ropeC[:], I['ropeC'][:, :, :], writes=['ropeC'])
        P.dma('sp', ropeS[:], I['ropeS'][:, :, :], writes=['ropeS'])
        for h in range(8):
            P.dma('sp', gqk[:, h, :], I['g_q'][0:1, :].partition_broadcast(128), writes=['gqk'])
        for h in range(2):
            P.dma('sp', gqk[:, 8 + h, :], I['g_k'][0:1, :].partition_broadcast(128), writes=['gqk'])
        P.ts('dve', gqk[:, 0:8, :], gqk[:, 0:8, :], 0.125, ALU.mult, r=['gqk'], w=['gqk'])
        P.memset('dve', v_sb[:], 1.0, w=['v_sb'])
        for d in 'fb':
            P.memset('dve', zaug[d][:], 1.0, w=['zaug_' + d])
        pa0 = P.ps('pa0', [128, 512], F32, st)
        pa1 = P.bank('pa1', F32, st)[:, 0:256]
        pb = [P.ps('pb%d' % i, [128, 512], F32, st) for i in range(3)]
        ptq = P.bank('ptq', BF16, st)[0:64, :].rearrange("p (h d) -> p h d", d=128)
        ptk = P.bank('ptk', BF16, st)[0:64, 0:256].rearrange("p (h d) -> p h d", d=128)
        sq = P.sb('sq', [128, 640], F32, st)
        s10 = P.sb('s10', [128, 3, 10], F32, st)
        qk = P.sb('qk', [128, 10, 64], F32, st)
        qk2 = P.sb('qk2', [128, 10, 64], F32, st)
        sw = P.sb('sw', [128, 10, 64], F32, st)
        qkb = P.sb('qkb', [128, 10, 64], BF16, st)
        stq = [P.sb('stq%d' % i, [128, 512], F32, st) for i in range(2)]
        stv = [P.sb('stv%d' % i, [128, 512], BF16, st) for i in range(2)]
        str_ = [P.sb('str%d' % i, [128, 512], F32, st) for i in range(2)]
        blks = [(0, 512), (512, 512), (1024, 512), (1536, 512), (2048, 256)]
        for (t0, n) in blks:
            hk = hkeys[t0 // 128:(t0 + n) // 128]
            for di, d in enumerate('fb'):
                for c in range(8):
                    P.mm(pb[0][0:16, 0:n], w_in[:, c, 2304 + 16 * di:2320 + 16 * di], hT[:, c, t0:t0 + n],
                         start=(c == 0), stop=(c == 7), r=hk + [wkeys[c]], w=['pb0'])
                P.copy('act', zaug[d][0:16, t0:t0 + n], pb[0][0:16, 0:n], r=['pb0'], w=['zaug_' + d])
        for t in range(NT):
            hk = [hkeys[t]]
            tok = slice(t * 128, (t + 1) * 128)
            i2 = t % 2
            for c in range(8):
                P.mm(pa0[:], hT[:, c, tok], w_in[:, c, 0:512], start=(c == 0), stop=(c == 7),
                     r=hk + [wkeys[c]], w=['pa0'])
            for c in range(8):
                P.mm(pa1, hT[:, c, tok], w_in[:, c, 512:768], start=(c == 0), stop=(c == 7),
                     r=hk + [wkeys[c]], w=['pa1'])
            for gi, c0 in enumerate((768, 1280, 1792)):
                for c in range(8):
                    P.mm(pb[gi][:], hT[:, c, tok], w_in[:, c, c0:c0 + 512], start=(c == 0), stop=(c == 7),
                         r=hk + [wkeys[c]], w=['pb%d' % gi])
            P.act(sq[:, 0:512], pa0[:], AF.Square, r=['pa0'], w=['sq'])
            P.act(sq[:, 512:640], pa1[:, 0:128], AF.Square, r=['pa1'], w=['sq'])
            P.reduce(s10[:, 0, :], sq[:].rearrange("p (h d) -> p h d", d=64), ALU.add, r=['sq'], w=['s10a'])
            P.act(s10[:, 1, :], s10[:, 0, :], AF.Sqrt, r=['s10a', 'epsb'], w=['s10b'], scale=1.0 / 64, bias=self.epsb[:, 0:1])
            P.recip(s10[:, 2, :], s10[:, 1, :], r=['s10b'], w=['s10c'])
            P.tt('dve', qk[:, 0:8, :], pa0[:].rearrange("p (h d) -> p h d", d=64),
                 bc(s10[:, 2, 0:8].unsqueeze(2), [128, 8, 64]), ALU.mult, r=['pa0', 's10c'], w=['qk'])
            P.tt('dve', qk[:, 8:10, :], pa1[:, 0:128].rearrange("p (h d) -> p h d", d=64),
                 bc(s10[:, 2, 8:10].unsqueeze(2), [128, 2, 64]), ALU.mult, r=['pa1', 's10c'], w=['qk'])
            P.copy('act', v_sb[:, t, :, 0:64], pa1[:, 128:256].rearrange("p (h d) -> p h d", d=64),
                   r=['pa1'], w=['v_sb'])
            if t < 2:
                P.tt('pool', qkb[:], qk[:], gqk[:], ALU.mult, r=['qk', 'gqk'], w=['qkb'])
            else:
                P.tt('pool', qk2[:], qk[:], gqk[:], ALU.mult, r=['qk', 'gqk'], w=['qk2'])
                C = bc(ropeC[:, t - 2, :].unsqueeze(1), [128, 10, 64])
                Sv = ropeS[:, t - 2, :].rearrange("p (j two) -> p j two", two=2)
                q2v = qk2[:].rearrange("p h (j two) -> p h j two", two=2)
                swv = sw[:].rearrange("p h (j two) -> p h j two", two=2)
                P.tt('pool', swv[:, :, :, 0], q2v[:, :, :, 1], bc(Sv[:, :, 0].unsqueeze(1), [128, 10, 32]),
                     ALU.mult, r=['qk2', 'ropeS'], w=['sw0'])
                P.tt('pool', swv[:, :, :, 1], q2v[:, :, :, 0], bc(Sv[:, :, 1].unsqueeze(1), [128, 10, 32]),
                     ALU.mult, r=['qk2', 'ropeS'], w=['sw1'])
                P.tt('dve', qk[:], qk2[:], C, ALU.mult, r=['qk2', 'ropeC'], w=['qk'])
                P.tt('dve', qkb[:], qk[:], sw[:], ALU.add, r=['qk', 'sw0', 'sw1'], w=['qkb'])
            for h in range(8):
                P.tr(ptq[:, h, :], qkb[:, h, :], self.ident_b[:], r=['qkb', 'ident_b'], w=['ptq'], inc=(h == 7))
            for h in range(2):
                P.tr(ptk[:, h, :], qkb[:, 8 + h, :], self.ident_b[:], r=['qkb', 'ident_b'], w=['ptk'], inc=(h == 1))
            P.copy('act', qT[:, :, tok], ptq, r=['ptq'], w=['qT%d' % t])
            P.copy('dve', kT[:, :, tok], ptk, r=['ptk'], w=['kT%d' % t])
            P.copy('act', stq[i2][:], pb[0][:], r=['pb0'], w=['stq%d' % i2])
            P.dma('pool', self.qkb_d[b, tok, :], stq[i2][:], reads=['stq%d' % i2], writes=['qkb_d'])
            P.copy('dve', stv[i2][:], pb[1][:], r=['pb1'], w=['stv%d' % i2])
            P.dma('pool', self.vb_d[b, tok, :], stv[i2][:], reads=['stv%d' % i2], writes=['vb_d'])
            P.act(str_[i2][:], pb[2][:], AF.Silu, r=['pb2'], w=['str%d' % i2])
            P.dma('pool', self.rb_d[b, tok, :], str_[i2][:], reads=['str%d' % i2], writes=['rb_d'])
        P.end_phase()

    def l0_gqa(self, b, st, qT, kT, v_sb, o_tm):
        P = self.P
        pbuf = [P.sb('pbuf%d' % i, [128, NT, 512], BF16, st) for i in range(2)]
        pss = [P.ps('pss%d' % i, [128, 512], F32, st) for i in range(4)]
        po = [P.ps('po%d' % i, [128, 512], F32, st) for i in range(2)]
        rc = P.sb('rc', [128, 2], F32, st)
        ib = 0; isx = 0; io = 0
        for h in range(8):
            kv = h // 4
            jobs = [(0, 256, 2)] + [(256 + 512 * i, 512, NT) for i in range(4)]
            for (q0, nq, nk) in jobs:
                pb_ = pbuf[ib % 2]; pbk = 'pbuf%d' % (ib % 2); ib += 1
                for kt in range(nk):
                    ps_ = pss[isx % 4]; psk = 'pss%d' % (isx % 4); isx += 1
                    P.mm(ps_[:, 0:nq], kT[:, kv, kt * 128:(kt + 1) * 128], qT[:, h, q0:q0 + nq], r=[], w=[psk])
                    P.act(pb_[:, kt, 0:nq], ps_[:, 0:nq], AF.Exp, r=[psk], w=['%s_%d' % (pbk, kt)])
                for j in range(nq // 128):
                    po_ = po[io % 2]; pok = 'po%d' % (io % 2); rk = 'rc%d' % (io % 2)
                    for kt in range(nk):
                        P.mm(po_[:, 0:65], pb_[:, kt, j * 128:(j + 1) * 128], v_sb[:, kt, kv, :],
                             start=(kt == 0), stop=(kt == nk - 1), r=['%s_%d' % (pbk, kt)], w=[pok])
                    P.recip(rc[:, io % 2:io % 2 + 1], po_[:, 64:65], r=[pok], w=[rk])
                    tq = q0 // 128 + j
                    P.ts('dve', o_tm[:, tq, h * 64:(h + 1) * 64], po_[:, 0:64], rc[:, io % 2:io % 2 + 1], ALU.mult,
                         r=[pok, rk], w=['o_tm%d' % tq])
                    io += 1
        P.end_phase()

    def l0_gla(self, b, st, o_tm, zaug, oT):
        P, I = self.P, self.I
        ptb = self._ptb
        for t in range(NT):
            for c in range(4):
                P.tr(ptb[:, c, :], o_tm[:, t, c * 128:(c + 1) * 128], self.ident_b[:], r=['ident_b'], w=['ptb'], inc=(c == 3))
            P.copy('act', oT[:, 0:4, t * 128:(t + 1) * 128], ptb, r=['ptb'], w=['oTa%d' % t])
        qkb_s = P.sb('qkb_s', [128, NT, 512], F32, st)
        vb_s = P.sb('vb_s', [128, NT, 512], BF16, st)
        o_acc = P.sb('o_acc', [128, NT, 512], F32, st)
        w2 = {d: P.sb('w2' + d, [17, 256], F32, st) for d in 'fb'}
        ggla = P.sb('ggla', [128, 128], F32, st)
        S32 = P.sb('S32', [64, 4, 128], F32, st)
        Sbf = P.sb('Sbf', [64, 4, 128], BF16, st)
        tmpS = P.sb('tmpS', [64, 4, 128], F32, st)
        e1 = P.sb('e1', [128, 256], F32, st)
        sp_ = P.sb('sp_', [128, 256], F32, st)
        eq = P.sb('eq', [128, 256], F32, st)
        ek = P.sb('ek', [128, 256], F32, st)
        dec = P.sb('dec', [64, 4], F32, st)
        qd = P.sb('qd', [128, 256], BF16, st)
        ki = P.sb('ki', [128, 256], BF16, st)
        qkT = P.sb('qkT', [64, 8, 128], BF16, st)
        qdT = qkT[:, 0:4, :]
        kiT = qkT[:, 4:8, :]
        att = P.sb('att', [128, 4, 128], BF16, st)
        pg1 = P.bank('pg1', F32, st)
        pxg = pg1[:, 0:256]
        pcs = pg1[:, 256:512]
        ptot = P.bank('ptot', F32, st)[0:64, 0:4]
        ptqk = P.bank('ptqk', BF16, st)[0:64, :].rearrange("p (h d) -> p h d", d=128)
        patt = P.bank('patt', F32, st)[:, :].rearrange("p (h d) -> p h d", d=128)
        pog = P.bank('pog', F32, st)
        pds = P.bank('pds', F32, st)[0:64, :].rearrange("p (h d) -> p h d", d=128)
        P.dma('sp', w2['f'][:], I['w2f'][:, :], writes=['w2f'])
        P.dma('sp', w2['b'][:], I['w2b'][:, :], writes=['w2b'])
        P.dma('sp', ggla[:], I['g_gla'][0:1, :].partition_broadcast(128), writes=['ggla'])
        for t in range(NT):
            P.dma('sp', qkb_s[:, t, :], self.qkb_d[b, t * 128:(t + 1) * 128, :], writes=['qkb_s%d' % t])
            P.dma('sp', vb_s[:, t, :], self.vb_d[b, t * 128:(t + 1) * 128, :], writes=['vb_s%d' % t])
        e1b = [e1, P.sb('e1b', [128, 256], F32, st)]
        spb = [sp_, P.sb('spb', [128, 256], F32, st)]
        eqb = [eq, P.sb('eqb', [128, 256], F32, st)]
        ekb = [ek, P.sb('ekb', [128, 256], F32, st)]
        decb = [dec, P.sb('decb', [64, 4], F32, st)]
        qdb = [qd, P.sb('qdb', [128, 256], BF16, st)]
        kib = [ki, P.sb('kib', [128, 256], BF16, st)]
        qkTb = [qkT, P.sb('qkTb', [64, 8, 128], BF16, st)]
        for d in 'fb':
            order = list(range(NT)) if d == 'f' else [1, 0] + list(range(NT - 1, 1, -1))
            P.memset('dve', S32[:], 0.0, w=['S32'])
            P.memset('dve', Sbf[:], 0.0, w=['Sbf'])

            def prep(oi, d=d, order=order):
                n = order[oi]
                x = oi % 2
                sx = str(x)
                tok = slice(n * 128, (n + 1) * 128)
                P.mm(pxg, zaug[d][:, tok], w2[d][:], r=['w2' + d], w=['pg1'])
                P.act(e1b[x][:], pxg, AF.Exp, r=['pg1'], w=['e1' + sx], scale=-1.0)
                P.act(spb[x][:], e1b[x][:], AF.Ln, r=['e1' + sx], w=['sp_' + sx], bias=1.0)
                P.mm(pcs, self.tri[d][:], spb[x][:], r=['tri_' + d, 'sp_' + sx], w=['pg1'])
                for h in range(4):
                    P.mm(ptot[:, h:h + 1], spb[x][:, h * 64:(h + 1) * 64], self.ones_f[:, 0:1], r=['sp_' + sx, 'ones_f'],
                         w=['ptot'], inc=(h == 3))
                P.act(decb[x][:], ptot, AF.Exp, r=['ptot'], w=['dec' + sx], scale=-1.0 / 16)
                P.act(eqb[x][:], pcs, AF.Exp, r=['pg1'], w=['eq' + sx], scale=-1.0 / 16)
                P.act(ekb[x][:], pcs, AF.Exp, r=['pg1'], w=['ek' + sx], scale=1.0 / 16)
                P.stt(qdb[x][:], qkb_s[:, n, 0:256], 0.125, eqb[x][:], ALU.mult, ALU.mult, r=['qkb_s%d' % n, 'eq' + sx], w=['qd' + sx])
                P.tt('pool', kib[x][:], qkb_s[:, n, 256:512], ekb[x][:], ALU.mult, r=['qkb_s%d' % n, 'ek' + sx], w=['ki' + sx])
                for h in range(4):
                    P.tr(ptqk[:, h, :], qdb[x][:, h * 64:(h + 1) * 64], self.ident_b[:], r=['qd' + sx, 'ident_b'], w=['ptqk'], inc=False)
                for h in range(4):
                    P.tr(ptqk[:, 4 + h, :], kib[x][:, h * 64:(h + 1) * 64], self.ident_b[:], r=['ki' + sx, 'ident_b'], w=['ptqk'], inc=(h == 3))
                P.copy('act', qkTb[x][:], ptqk, r=['ptqk'], w=['qkT' + sx])

            def chain(oi, d=d, order=order):
                n = order[oi]
                x = oi % 2
                sx = str(x)
                qdT_ = qkTb[x][:, 0:4, :]
                kiT_ = qkTb[x][:, 4:8, :]
                for h in range(4):
                    P.mm(patt[:, h, :], kiT_[:, h, :], qdT_[:, h, :], r=['qkT' + sx], w=['patt'], inc=(h == 3))
                P.tt('dve', att[:], patt, bc(self.tri[d][:].unsqueeze(1), [128, 4, 128]), ALU.mult,
                     r=['patt', 'tri_' + d], w=['att'])
                for h in range(4):
                    hs = slice(h * 128, (h + 1) * 128)
                    P.mm(pog[:, hs], att[:, h, :], vb_s[:, n, hs], start=True, stop=(oi == 0),
                         r=['att', 'vb_s%d' % n], w=['pog'], inc=False)
                    if oi > 0:
                        P.mm(pog[:, hs], qdT_[:, h, :], Sbf[:, h, :], start=False, stop=True,
                             r=['qkT' + sx, 'Sbf'], w=['pog'], inc=False)
                for h in range(4):
                    hs = slice(h * 128, (h + 1) * 128)
                    P.mm(pds[:, h, :], kib[x][:, h * 64:(h + 1) * 64], vb_s[:, n, hs], r=['ki' + sx, 'vb_s%d' % n],
                         w=['pds'], inc=(h == 3))
                if d == 'f':
                    P.copy('act', o_acc[:, n, :], pog[:], r=['pog'], w=['o_acc%d' % n])
                else:
                    P.tt('dve', o_acc[:, n, :], o_acc[:, n, :], pog[:], ALU.add, r=['pog', 'o_acc%d' % n], w=['o_acc%d' % n])
                P.tt('dve', tmpS[:], pds, S32[:], ALU.add, r=['pds', 'S32'], w=['tmpS'])
                P.tt('dve', S32[:], tmpS[:], bc(decb[x][:].unsqueeze(2), [64, 4, 128]), ALU.mult, r=['tmpS', 'dec' + sx], w=['S32'])
                P.copy('act', Sbf[:], S32[:], r=['S32'], w=['Sbf'])

            prep(0)
            for oi in range(len(order)):
                if oi + 1 < len(order):
                    prep(oi + 1)
                chain(oi)
        sqo = P.sb('sqo', [128, 512], F32, st)
        s4 = P.sb('s4', [128, 3, 4], F32, st)
        on = P.sb('on', [128, 4, 128], F32, st)
        on2 = P.sb('on2', [128, 4, 128], F32, st)
        rt = [P.sb('rt%d' % i, [128, 512], F32, st) for i in range(2)]
        ob = P.sb('ob', [128, 512], BF16, st)
        for n in range(NT):
            i2 = n % 2
            P.dma('sp', rt[i2][:], self.rb_d[b, n * 128:(n + 1) * 128, :], writes=['rt%d' % i2])
            P.act(sqo[:], o_acc[:, n, :], AF.Square, r=['o_acc%d' % n], w=['sqo'])
            P.reduce(s4[:, 0, :], sqo[:].rearrange("p (h d) -> p h d", d=128), ALU.add, r=['sqo'], w=['s4a'])
            P.act(s4[:, 1, :], s4[:, 0, :], AF.Sqrt, r=['s4a', 'epsb'], w=['s4b'], scale=1.0 / 128, bias=self.epsb[:, 0:1])
            P.recip(s4[:, 2, :], s4[:, 1, :], r=['s4b'], w=['s4c'])
            P.tt('dve', on[:], o_acc[:, n, :].rearrange("p (h d) -> p h d", d=128),
                 bc(s4[:, 2, :].unsqueeze(2), [128, 4, 128]), ALU.mult, r=['o_acc%d' % n, 's4c'], w=['on'])
            P.tt('pool', on2[:], on[:], bc(ggla[:].unsqueeze(1), [128, 4, 128]), ALU.mult, r=['on', 'ggla'], w=['on2'])
            P.tt('dve', ob[:], on2[:].rearrange("p h d -> p (h d)"), rt[i2][:], ALU.mult, r=['on2', 'rt%d' % i2], w=['ob'])
            for c in range(4):
                P.tr(ptb[:, c, :], ob[:, c * 128:(c + 1) * 128], self.ident_b[:], r=['ob', 'ident_b'], w=['ptb'], inc=(c == 3))
            P.copy('act', oT[:, 4:8, n * 128:(n + 1) * 128], ptb, r=['ptb'], w=['oTb%d' % n])
        P.end_phase()

    def l0_outproj(self, b, st, oT):
        P, I = self.P, self.I
        wo = P.sb('wo', [128, 8, D], BF16, st)
        wv = I['w_out_ab'].rearrange("(c p) f -> p c f", p=128)
        for c in range(8):
            P.dma('pool', wo[:, c, :], wv[:, c, :], writes=['wo%d' % c])
        G = {j: P.sb('G%d' % j, [128, D], F32, st) for j in (2, b)}
        for j in (2, b):
            self.load_gate(G[j], 'G%d' % j, 0, j, 1)
        self.proj_residual(st, 'o0', oT, lambda t: [], NT, wo, ['wo%d' % c for c in range(8)],
                           lambda t: (I['ctx'][b, t * 128:(t + 1) * 128, :] if t < 2 else I['x'][b, (t - 2) * 128:(t - 1) * 128, :]),
                           lambda t: (G[2], 'G2') if t < 2 else (G[b], 'G%d' % b),
                           lambda t: self.resA[b, t * 128:(t + 1) * 128, :], 'resA')
        P.end_phase()

    def proj_residual(self, st, tag, oT, okeys, ntiles, wo, wokeys, xsrc, gate_fn, dst, dkey, tok0=0):
        P = self.P
        py = [P.ps('%s_py%d' % (tag, i), [128, 512], F32, st) for i in range(4)]
        xt = [P.sb('%s_x%d' % (tag, i), [128, D], F32, st) for i in range(2)]
        tmp = [P.sb('%s_t%d' % (tag, i), [128, D], F32, st) for i in range(2)]
        xo = [P.sb('%s_o%d' % (tag, i), [128, D], F32, st) for i in range(2)]
        for t in range(ntiles):
            i2 = t % 2
            tok = slice(tok0 + t * 128, tok0 + (t + 1) * 128)
            P.dma('sp', xt[i2][:], xsrc(t), reads=['src_' + tag], writes=['%s_x%d' % (tag, i2)])
            G, gk = gate_fn(t)
            for half in range(2):
                p_ = py[2 * i2 + half]; pk = '%s_py%d' % (tag, 2 * i2 + half)
                for c in range(8):
                    P.mm(p_[:], oT[:, c, tok], wo[:, c, half * 512:(half + 1) * 512], start=(c == 0), stop=(c == 7),
                         r=okeys(t) + [wokeys[c]], w=[pk])
                hs = slice(half * 512, (half + 1) * 512)
                P.tt('dve', tmp[i2][:, hs], p_[:], G[:, hs], ALU.mult, r=[pk, gk], w=['%s_t%d_%d' % (tag, i2, half)])
                P.tt('pool', xo[i2][:, hs], tmp[i2][:, hs], xt[i2][:, hs], ALU.add,
                     r=['%s_t%d_%d' % (tag, i2, half), '%s_x%d' % (tag, i2)], w=['%s_o%d_%d' % (tag, i2, half)])
            P.dma('pool', dst(t), xo[i2][:], reads=['%s_o%d_0' % (tag, i2), '%s_o%d_1' % (tag, i2)], writes=[dkey])


def _swiglu_gate_up(self, P, hT, ntok, blks, wg_src, wu_src, f_tiles, act, act_off, wbufs, pgs, pus, sgs, ctr):
    i = 0
    while i < len(f_tiles):
        nf = min(2, len(f_tiles) - i)
        f0 = f_tiles[i]
        k = ctr[0] % len(wbufs); ctr[0] += 1
        wg, wu = wbufs[k]
        P.dma('pool', wg[:, :, 0:nf * 128], wg_src[:, :, f0 * 128:(f0 + nf) * 128], writes=['wg%d' % k])
        P.dma('pool', wu[:, :, 0:nf * 128], wu_src[:, :, f0 * 128:(f0 + nf) * 128], writes=['wu%d' % k])
        for fi in range(nf):
            for (t0, n) in blks:
                j = ctr[1] % 2; ctr[1] += 1
                for c in range(8):
                    P.mm(pgs[j][:, 0:n], wg[:, c, fi * 128:(fi + 1) * 128], hT[:, c, t0:t0 + n], start=(c == 0), stop=(c == 7),
                         r=['wg%d' % k], w=['pg%d' % j])
                for c in range(8):
                    P.mm(pus[j][:, 0:n], wu[:, c, fi * 128:(fi + 1) * 128], hT[:, c, t0:t0 + n], start=(c == 0), stop=(c == 7),
                         r=['wu%d' % k], w=['pu%d' % j])
                P.act(sgs[j][:, 0:n], pgs[j][:, 0:n], AF.Silu, r=['pg%d' % j], w=['sg%d' % j])
                P.tt('dve', act[:, act_off + i + fi, t0:t0 + n], sgs[j][:, 0:n], pus[j][:, 0:n], ALU.mult,
                     r=['sg%d' % j, 'pu%d' % j], w=['act%d' % (act_off + i + fi)])
        i += nf


def _l0_ffn(self, b, half):
    P, I = self.P, self.I
    NTB = 9
    TB = NTB * 128
    r0 = half * TB
    with ExitStack() as st0:
        hT = P.sb('hT2', [128, 8, TB], BF16, st0)
        with ExitStack() as stn:
            self.norm_mod(stn, 'n2', lambda t: self.resA[b, r0 + t * 128:r0 + (t + 1) * 128, :], NTB,
                          lambda t: 2 if (half == 0 and t < 2) else b, 0, 1, hT, 'hT2')
            P.end_phase()
        with ExitStack() as st:
            act = P.sb('act', [128, NFT, TB], BF16, st)
            wbufs = [(P.sb('wg%d' % i, [128, 8, 256], BF16, st), P.sb('wu%d' % i, [128, 8, 256], BF16, st)) for i in range(3)]
            pgs = [P.bank('pg%d' % i, F32, st) for i in range(2)]
            pus = [P.bank('pu%d' % i, F32, st) for i in range(2)]
            sgs = [P.sb('sg%d' % i, [128, 512], F32, st) for i in range(2)]
            wd = [P.sb('wd%d' % i, [128, NFT, 512], BF16, st) for i in range(2)]
            pys = [P.bank('py%d' % i, F32, st) for i in range(2)]
            conds = [2, b] if half == 0 else [b]
            G = {j: P.sb('G2_%d' % j, [128, D], F32, st) for j in conds}
            for j in conds:
                self.load_gate(G[j], 'G2_%d' % j, 0, j, 2)
            xt = [P.sb('fx%d' % i, [128, 512], F32, st) for i in range(2)]
            tmp = [P.sb('ft%d' % i, [128, 512], F32, st) for i in range(2)]
            xo = [P.sb('fo%d' % i, [128, 512], F32, st) for i in range(2)]
            wg_src = I['w_ff_gate'].rearrange("(c p) f -> p c f", p=128)
            wu_src = I['w_ff_up'].rearrange("(c p) f -> p c f", p=128)
            wd_src = I['w_ff_down'].rearrange("(f p) d -> p f d", p=128)
            blks = [(0, 512), (512, 512), (1024, 128)]
            ctr = [0, 0]
            _swiglu_gate_up(self, P, hT, TB, blks, wg_src, wu_src, list(range(NFT)), act, 0, wbufs, pgs, pus, sgs, ctr)
            akeys = ['act%d' % f for f in range(NFT)]
            k = 0
            for dh in range(2):
                for q4 in range(4):
                    P.dma('pool', wd[dh][:, q4 * 7:(q4 + 1) * 7, :], wd_src[:, q4 * 7:(q4 + 1) * 7, dh * 512:(dh + 1) * 512],
                          writes=['wd%d_%d' % (dh, q4)])
                for t in range(NTB):
                    i2 = k % 2; k += 1
                    rows = slice(r0 + t * 128, r0 + (t + 1) * 128)
                    cs = slice(dh * 512, (dh + 1) * 512)
                    P.dma('sp', xt[i2][:], self.resA[b, rows, cs], writes=['fx%d' % i2])
                    for f in range(NFT):
                        P.mm(pys[i2][:], act[:, f, t * 128:(t + 1) * 128], wd[dh][:, f, :], start=(f == 0), stop=(f == NFT - 1),
                             r=[akeys[f], 'wd%d_%d' % (dh, f // 7)], w=['py%d' % i2])
                    j = 2 if (half == 0 and t < 2) else b
                    P.tt('dve', tmp[i2][:], pys[i2][:], G[j][:, cs], ALU.mult, r=['py%d' % i2, 'G2_%d' % j], w=['ft%d' % i2])
                    P.tt('pool', xo[i2][:], tmp[i2][:], xt[i2][:], ALU.add, r=['ft%d' % i2, 'fx%d' % i2], w=['fo%d' % i2])
                    P.dma('pool', self.resB[b, rows, cs], xo[i2][:], reads=['fo%d' % i2], writes=['resB'])
            P.end_phase()


def _na_pos(d0):
    return (d0 + 7) // 2 if d0 % 2 != 0 else 7 + (d0 + 6) // 2


def _layer1_mixer(self, b):
    P, I = self.P, self.I
    with ExitStack() as st0:
        oT = P.sb('oT1', [128, 8, SEQ], BF16, st0)
        with ExitStack() as st1:
            hT = P.sb('hT3', [128, 8, TOT], BF16, st1)
            with ExitStack() as stn:
                self.norm_mod(stn, 'n3', lambda t: self.resB[b, t * 128:(t + 1) * 128, :], NT,
                              lambda t: 2 if t < 2 else b, 1, 0, hT, 'hT3')
                P.end_phase()
            bias_sb = P.sb('bias_sb', [128, 16, 896], BF16, st1)
            for h in range(16):
                P.dma('pool', bias_sb[:, h, :], I['nabias'][h], writes=['bias%d' % h])
            wsrc = I['w_in_c'].rearrange("(c p) f -> p c f", p=128)
            for g in range(4):
                with ExitStack() as sg:
                    wq = P.sb('wq', [128, 8, 256], BF16, sg)
                    wk = P.sb('wk', [128, 8, 256], BF16, sg)
                    wv = P.sb('wv', [128, 8, 256], BF16, sg)
                    P.dma('pool', wq[:], wsrc[:, :, g * 256:(g + 1) * 256], writes=['wq'])
                    P.dma('pool', wk[:], wsrc[:, :, D + g * 256:D + (g + 1) * 256], writes=['wk'])
                    P.dma('pool', wv[:], wsrc[:, :, 2 * D + g * 256:2 * D + (g + 1) * 256], writes=['wv'])
                    qT = P.sb('qT1', [128, 2, SEQ], BF16, sg)
                    kT = P.sb('kT1', [128, 2, TOT], BF16, sg)
                    v_e = P.sb('v_e', [128, 16, 4, 65], BF16, sg)
                    v_o = P.sb('v_o', [128, 15, 4, 65], BF16, sg)
                    v_c = P.sb('v_c', [128, 2, 4, 65], BF16, sg)
                    o_g = P.sb('o_g', [64, 32, 256], BF16, sg)
                    pctx = [P.sb('pctx%d' % i, [128, 2, SEQ], BF16, sg) for i in range(2)]
                    ploc = [P.sb('ploc%d' % i, [128, 256], BF16, sg) for i in range(3)]
                    rc = P.sb('rc1', [64, 2], F32, sg)
                    pp_ = [P.bank('pp%d' % i, F32, sg) for i in range(3)]
                    psl = [P.bank('psl%d' % i, F32, sg) for i in range(2)]
                    pov = [P.bank('pov%d' % i, F32, sg) for i in range(2)]
                    P.memset('dve', v_e[:], 1.0, w=['v_e'])
                    P.memset('dve', v_o[:], 1.0, w=['v_o'])
                    P.memset('dve', v_c[:], 1.0, w=['v_c'])
                    ip = 0
                    for pp in range(2):
                        for blk in range(4):
                            p_ = pp_[ip % 3]; pk = 'pp%d' % (ip % 3); ip += 1
                            for c in range(8):
                                P.mm(p_[:], wq[:, c, pp * 128:(pp + 1) * 128], hT[:, c, NCTX + blk * 512:NCTX + (blk + 1) * 512],
                                     start=(c == 0), stop=(c == 7), r=['wq'], w=[pk])
                            P.act(qT[:, pp, blk * 512:(blk + 1) * 512], p_[:], AF.Identity, r=[pk], w=['qT1'], scale=0.125)
                        for (t0, n) in [(0, 256), (256, 512), (768, 512), (1280, 512), (1792, 512)]:
                            p_ = pp_[ip % 3]; pk = 'pp%d' % (ip % 3); ip += 1
                            for c in range(8):
                                P.mm(p_[:, 0:n], wk[:, c, pp * 128:(pp + 1) * 128], hT[:, c, t0:t0 + n],
                                     start=(c == 0), stop=(c == 7), r=['wk'], w=[pk])
                            P.copy('dve', kT[:, pp, t0:t0 + n], p_[:, 0:n], r=[pk], w=['kT1'])
                    vjobs = [(t * 128, v_c[:, t, :, 0:64], 'v_c') for t in range(2)]
                    vjobs += [(NCTX + t * 128, v_e[:, t, :, 0:64], 'v_e') for t in range(16)]
                    vjobs += [(NCTX + 64 + t * 128, v_o[:, t, :, 0:64], 'v_o') for t in range(15)]
                    for (t0, dst, dk) in vjobs:
                        p_ = pp_[ip % 3]; pk = 'pp%d' % (ip % 3); ip += 1
                        for c in range(8):
                            P.mm(p_[:, 0:256], hT[:, c, t0:t0 + 128], wv[:, c, :], start=(c == 0), stop=(c == 7), r=['wv'], w=[pk])
                        P.copy('act' if ip % 2 else 'dve', dst, p_[:, 0:256].rearrange("p (h d) -> p h d", d=64), r=[pk], w=[dk])
                    isl = 0; iov = 0; ipl = 0
                    for hh in range(4):
                        h = 4 * g + hh
                        pp = hh // 2
                        ps_ = slice((hh % 2) * 64, (hh % 2) * 64 + 64)
                        pc = pctx[hh % 2]; pck = 'pctx%d' % (hh % 2)
                        for ct in range(2):
                            for blk in range(4):
                                p_ = pp_[ip % 3]; pk = 'pp%d' % (ip % 3); ip += 1
                                P.mm(p_[:], kT[ps_, pp, ct * 128:(ct + 1) * 128], qT[ps_, pp, blk * 512:(blk + 1) * 512],
                                     r=['kT1', 'qT1'], w=[pk])
                                P.act(pc[:, ct, blk * 512:(blk + 1) * 512], p_[:], AF.Exp, r=[pk], w=[pck])
                        cnt = {'isl': isl, 'ipl': ipl, 'iov': iov}

                        def s_part(r, h=h, pp=pp, ps_=ps_, cnt=cnt):
                            rs = min(max(r - 4, 0), 24)
                            pos = _na_pos(rs - r)
                            sl = psl[cnt['isl'] % 2]; slk = 'psl%d' % (cnt['isl'] % 2); cnt['isl'] += 1
                            P.mm(sl[:, 0:256], self.ident_b[:], bias_sb[:, h, pos * 64:(pos + 4) * 64], start=True, stop=False,
                                 r=['bias%d' % h, 'ident_b'], w=[slk], inc=False)
                            for j in range(4):
                                kt0 = NCTX + (rs + 2 * j) * 64
                                P.mm(sl[:, j * 64:(j + 1) * 64], kT[ps_, pp, kt0:kt0 + 128], qT[ps_, pp, r * 64:(r + 1) * 64],
                                     start=False, stop=True, r=['kT1', 'qT1'], w=[slk], inc=(j == 3))
                            pl = ploc[cnt['ipl'] % 3]; plk = 'ploc%d' % (cnt['ipl'] % 3); cnt['ipl'] += 1
                            P.act(pl[:], sl[:, 0:256], AF.Exp, r=[slk], w=[plk])
                            return (rs, pl, plk)

                        def pv_part(r, st_, hh=hh, pc=pc, pck=pck, cnt=cnt):
                            rs, pl, plk = st_
                            iov_ = cnt['iov']
                            ov = pov[iov_ % 2]; ovk = 'pov%d' % (iov_ % 2); rk = 'rc1_%d' % (iov_ % 2)
                            for j in range(4):
                                row = rs + 2 * j
                                vt = v_e[:, row // 2, hh, :] if rs % 2 == 0 else v_o[:, (row - 1) // 2, hh, :]
                                P.mm(ov[0:64, 0:65], pl[:, j * 64:(j + 1) * 64], vt, start=(j == 0), stop=False,
                                     r=[plk, 'v_e', 'v_o'], w=[ovk], inc=False)
                            for ct in range(2):
                                P.mm(ov[0:64, 0:65], pc[:, ct, r * 64:(r + 1) * 64], v_c[:, ct, hh, :], start=False, stop=(ct == 1),
                                     r=[pck, 'v_c'], w=[ovk], inc=(ct == 1))
                            P.recip(rc[:, iov_ % 2:iov_ % 2 + 1], ov[0:64, 64:65], r=[ovk], w=[rk])
                            P.ts('dve', o_g[:, r, hh * 64:(hh + 1) * 64], ov[0:64, 0:64], rc[:, iov_ % 2:iov_ % 2 + 1], ALU.mult,
                                 r=[ovk, rk], w=['o_g%d' % r])
                            cnt['iov'] += 1

                        st_ = s_part(0)
                        for r in range(32):
                            nxt = s_part(r + 1) if r + 1 < 32 else None
                            pv_part(r, st_)
                            st_ = nxt
                        isl, ipl, iov = cnt['isl'], cnt['ipl'], cnt['iov']
                    ptb_full = self._ptb_bank
                    ptv = ptb_full[:, :].rearrange("p (c r q) -> p c r q", c=2, r=8)
                    for r8 in range(4):
                        for cc in range(2):
                            for rr in range(8):
                                r = r8 * 8 + rr
                                P.tr(ptv[:, cc, rr, :], o_g[:, r, cc * 128:(cc + 1) * 128], self.ident_b[0:64, 0:64],
                                     r=['o_g%d' % r, 'ident_b'], w=['ptb'], inc=(cc == 1 and rr == 7))
                        P.copy('act', oT[:, 2 * g:2 * g + 2, r8 * 512:(r8 + 1) * 512],
                               ptb_full[:, :].rearrange("p (c n) -> p c n", c=2), r=['ptb'], w=['oT1'])
                    P.end_phase()
        with ExitStack() as st2:
            wo = P.sb('wo1', [128, 8, D], BF16, st2)
            wv_ = I['w_out_c'].rearrange("(c p) f -> p c f", p=128)
            for c in range(8):
                P.dma('pool', wo[:, c, :], wv_[:, c, :], writes=['wo1_%d' % c])
            G = P.sb('G1b', [128, D], F32, st2)
            self.load_gate(G, 'G1b', 1, b, 1)
            self.proj_residual(st2, 'o1', oT, lambda t: [], 16, wo, ['wo1_%d' % c for c in range(8)],
                               lambda t: self.resB[b, NCTX + t * 128:NCTX + (t + 1) * 128, :],
                               lambda t: (G, 'G1b'),
                               lambda t: self.resC[b, t * 128:(t + 1) * 128, :], 'resC')
            P.end_phase()


def _layer1_moe(self, b):
    P, I = self.P, self.I
    NTL = 16
    with ExitStack() as st0:
        comb = P.sb('comb', [128, NTL, NEXP], F32, st0)
        y_acc = P.sb('y_acc', [128, NTL, D], F32, st0)
        with ExitStack() as stA:
            hT = P.sb('hT4', [128, 8, SEQ], BF16, stA)
            with ExitStack() as stn:
                wr = P.sb('wr', [128, 8, NEXP], F32, stn)
                P.dma('sp', wr[:], I['w_router'][:, :, :], writes=['wr'])
                plog = P.bank('plog', F32, stn)
                rt_ = P.sb('rt_', [128, 8, 8], F32, stn)

                def want32(t, h32, key):
                    for c in range(8):
                        P.mm(plog[:, 0:NEXP], h32[:, c, :], wr[:, c, :], start=(c == 0), stop=(c == 7), r=[key, 'wr'], w=['plog'])
                    lg, top8, dd, ex, mk, num, den = [rt_[:, i, :] for i in range(7)]
                    P.copy('dve', lg, plog[:, 0:NEXP], r=['plog'], w=['r_lg'])
                    P.op('dve', lambda e: e.max(out=top8, in_=lg), ['r_lg'], ['r_top'])
                    P.ts('dve', dd, lg, top8[:, 0:1], ALU.subtract, r=['r_lg', 'r_top'], w=['r_dd'])
                    P.act(ex, dd, AF.Exp, r=['r_dd'], w=['r_ex'])
                    P.ts('dve', mk, lg, top8[:, 1:2], ALU.is_ge, r=['r_lg', 'r_top'], w=['r_mk'])
                    P.tt('dve', num, ex, mk, ALU.mult, r=['r_ex', 'r_mk'], w=['r_num'])
                    P.reduce(den[:, 0:1], num, ALU.add, r=['r_num'], w=['r_den'])
                    P.recip(den[:, 1:2], den[:, 0:1], r=['r_den'], w=['r_rden'])
                    P.ts('dve', comb[:, t, :], num, den[:, 1:2], ALU.mult, r=['r_num', 'r_rden'], w=['comb%d' % t])

                self.norm_mod(stn, 'n4', lambda t: self.resC[b, t * 128:(t + 1) * 128, :], NTL, lambda t: b, 1, 1, hT, 'hT4',
                              want32=want32)
                P.end_phase()
            if self.upto == 'l1_router':
                self.dump('comb', comb[:], [128, NTL, NEXP], F32)
                P.end_phase()
                return
            with ExitStack() as st:
                NQ = 7
                act = P.sb('mact', [128, NQ, SEQ], BF16, st)
                wbufs = [(P.sb('wg%d' % i, [128, 8, 256], BF16, st), P.sb('wu%d' % i, [128, 8, 256], BF16, st)) for i in range(4)]
                wd = [P.sb('mwd%d' % i, [128, D], BF16, st) for i in range(8)]
                pgs = [P.bank('pg%d' % i, F32, st) for i in range(2)]
                pus = [P.bank('pu%d' % i, F32, st) for i in range(2)]
                pys = [P.bank('py%d' % i, F32, st) for i in range(2)]
                sgs = [P.sb('sg%d' % i, [128, 512], F32, st) for i in range(2)]
                for t in range(NTL):
                    P.memset('pool' if t % 2 else 'dve', y_acc[:, t, :], 0.0, w=['y%d_0' % t, 'y%d_1' % t])
                blks = [(i * 512, 512) for i in range(4)]
                ctr = [0, 0]
                iw = 0; iy = 0
                import os
                nexp = int(os.environ.get('MOE_NEXP', NEXP))
                for e in range(nexp):
                    wg_src = I['w_moe_gate'][e].rearrange("(c p) f -> p c f", p=128)
                    wu_src = I['w_moe_up'][e].rearrange("(c p) f -> p c f", p=128)
                    for q in range(NFT // NQ):
                        fts = list(range(q * NQ, (q + 1) * NQ))
                        self._moe_gate_up(P, hT, blks, wg_src, wu_src, fts, act, wbufs, pgs, pus, sgs, ctr)
                        wks = []
                        for fl in range(NQ):
                            k = iw % 8; iw += 1
                            P.dma('pool', wd[k][:], I['w_moe_down'][e, (q * NQ + fl) * 128:(q * NQ + fl + 1) * 128, :], writes=['mwd%d' % k])
                            wks.append(k)
                        for t in range(NTL):
                            for dh in range(2):
                                i2 = iy % 2; iy += 1
                                for fl in range(NQ):
                                    P.mm(pys[i2][:], act[:, fl, t * 128:(t + 1) * 128], wd[wks[fl]][:, dh * 512:(dh + 1) * 512],
                                         start=(fl == 0), stop=(fl == NQ - 1), r=['act%d' % fl, 'mwd%d' % wks[fl]], w=['py%d' % i2])
                                yk = 'y%d_%d' % (t, dh)
                                ys = y_acc[:, t, dh * 512:(dh + 1) * 512]
                                P.stt(ys, pys[i2][:], comb[:, t, e:e + 1], ys, ALU.mult, ALU.add, r=['py%d' % i2, yk], w=[yk])
                P.end_phase()
        with ExitStack() as st:
            G2 = P.sb('G2f', [128, D], F32, st)
            gfin = P.sb('gfin', [128, D], F32, st)
            self.load_gate(G2, 'G2f', 1, b, 2)
            P.dma('sp', gfin[:], I['g_final'][0:1, :].partition_broadcast(128), writes=['gfin'])
            xc = [P.sb('xc%d' % i, [128, D], F32, st) for i in range(2)]
            tmp = P.sb('ftmp', [128, D], F32, st)
            xo = P.sb('fxo', [128, D], F32, st)
            junk = P.sb('fjunk', [128, D], BF16, st)
            ss = P.sb('fss', [128, 2], F32, st)
            ot = [P.sb('fot%d' % i, [128, D], F32, st) for i in range(2)]
            for t in range(NTL):
                i2 = t % 2
                P.dma('sp', xc[i2][:], self.resC[b, t * 128:(t + 1) * 128, :], writes=['xc%d' % i2])
                P.tt('dve', tmp[:], y_acc[:, t, :], G2[:], ALU.mult, r=['G2f'], w=['ftmp'])
                P.tt('pool', xo[:], tmp[:], xc[i2][:], ALU.add, r=['ftmp', 'xc%d' % i2], w=['fxo'])
                P.sumsq(junk[:], xo[:], ss[:, 0:1], r=['fxo'], w=['fjunk', 'fss0'])
                P.act(ss[:, 1:2], ss[:, 0:1], AF.Sqrt, r=['fss0', 'epsb'], w=['fss1'], scale=1.0 / D, bias=self.epsb[:, 0:1])
                P.recip(ss[:, 0:1], ss[:, 1:2], r=['fss1'], w=['fss0'])
                P.stt(ot[i2][:], xo[:], ss[:, 0:1], gfin[:], ALU.mult, ALU.mult, r=['fxo', 'fss0', 'gfin'], w=['fot%d' % i2])
                P.dma('pool', self.out[b, t * 128:(t + 1) * 128, :], ot[i2][:], reads=['fot%d' % i2], writes=['out'])
            P.end_phase()


def _moe_gate_up(self, P, hT, blks, wg_src, wu_src, f_tiles, act, wbufs, pgs, pus, sgs, ctr):
    i = 0
    while i < len(f_tiles):
        nf = min(2, len(f_tiles) - i)
        f0 = f_tiles[i]
        k = ctr[0] % len(wbufs); ctr[0] += 1
        wg, wu = wbufs[k]
        P.dma('pool', wg[:, :, 0:nf * 128], wg_src[:, :, f0 * 128:(f0 + nf) * 128], writes=['wg%d' % k])
        P.dma('pool', wu[:, :, 0:nf * 128], wu_src[:, :, f0 * 128:(f0 + nf) * 128], writes=['wu%d' % k])
        for fi in range(nf):
            for (t0, n) in blks:
                j = ctr[1] % 2; ctr[1] += 1
                for c in range(8):
                    P.mm(pgs[j][:, 0:n], wg[:, c, fi * 128:(fi + 1) * 128], hT[:, c, t0:t0 + n], start=(c == 0), stop=(c == 7),
                         r=['wg%d' % k], w=['pg%d' % j])
                for c in range(8):
                    P.mm(pus[j][:, 0:n], wu[:, c, fi * 128:(fi + 1) * 128], hT[:, c, t0:t0 + n], start=(c == 0), stop=(c == 7),
                         r=['wu%d' % k], w=['pu%d' % j])
                P.act(sgs[j][:, 0:n], pgs[j][:, 0:n], AF.Silu, r=['pg%d' % j], w=['sg%d' % j])
                P.tt('dve', act[:, i + fi, t0:t0 + n], sgs[j][:, 0:n], pus[j][:, 0:n], ALU.mult,
                     r=['sg%d' % j, 'pu%d' % j], w=['act%d' % (i + fi)])
        i += nf


def _layer1_moe_sparse(self):
    P, I = self.P, self.I
    NTT = 32
    nc = self.nc
    with ExitStack() as st0:
        m12 = P.sb('m12', [128, 2, NTT, 8], F32, st0)
        pos = P.sb('pos', [128, NTT, 8], F32, st0)
        g12 = P.sb('g12', [128, 2, NTT], F32, st0)
        run = P.sb('run', [128, 8], F32, st0)
        ek_i = P.sb('ek_i', [128, NKT], I32, st0)
        idxg = P.sb('idxg', [128, NKT, 32], I32, st0)
        idxd = P.sb('idxd', [128, NKT, NFT], I32, st0)
        with ExitStack() as stn:
            wr = P.sb('wr', [128, 8, NEXP], F32, stn)
            P.dma('sp', wr[:], I['w_router'][:, :, :], writes=['wr'])
            stri = P.sb('stri', [128, 128], F32, stn)
            P.dma('sp', stri[:], I['stri'][:, :], writes=['stri'])
            ones128 = P.sb('ones128', [128, 128], F32, stn)
            P.memset('dve', ones128[:], 1.0, w=['ones128'])
            P.memset('dve', run[:], 0.0, w=['run'])
            plog = P.bank('plog', F32, stn)
            ppos = P.bank('ppos', F32, stn)
            lg_all = P.sb('lg_all', [128, NTT, 8], F32, stn)
            Abc = P.sb('Abc', [128, D], F32, stn)
            Bbc = P.sb('Bbc', [128, D], F32, stn)
            gnb = P.sb('gnb', [128, D], F32, stn)
            htm = [P.sb('htm%d' % i, [128, D], F32, stn) for i in range(2)]
            hbf = [P.sb('hbf%d' % i, [128, D], BF16, stn) for i in range(2)]
            P.dma('sp', gnb[:], I['gn2_nat'][0:1, :].partition_broadcast(128), writes=['gnb'])
            for b in range(2):
                P.dma('sp', Abc[:], self.mod_d[1, b, 4:5, :].partition_broadcast(128), writes=['Abc'])
                P.dma('sp', Bbc[:], self.mod_d[1, b, 3:4, :].partition_broadcast(128), writes=['Bbc'])
                P.stt(Abc[:], Abc[:], 1.0, gnb[:], ALU.add, ALU.mult, r=['Abc', 'gnb'], w=['Abc'])

                def tm_cb(t, xn, xk, b=b):
                    tt = b * 16 + t
                    i2 = tt % 2
                    P.tt('dve', htm[i2][:], xn[:], Abc[:], ALU.mult, r=[xk, 'Abc'], w=['htm%d' % i2])
                    P.tt('pool', hbf[i2][:], htm[i2][:], Bbc[:], ALU.add, r=['htm%d' % i2, 'Bbc'], w=['hbf%d' % i2])

                def post_cb(t, b=b):
                    tt = b * 16 + t
                    i2 = tt % 2
                    P.dma('act', self.h_d[tt * 128:(tt + 1) * 128, :], hbf[i2][:], reads=['hbf%d' % i2], writes=['h_d'])

                def want32(t, h32, key, b=b):
                    tt = b * 16 + t
                    for c in range(8):
                        P.mm(plog[:, 0:NEXP], h32[:, c, :], wr[:, c, :], start=(c == 0), stop=(c == 7), r=[key, 'wr'], w=['plog'])
                    P.copy('dve', lg_all[:, tt, :], plog[:, 0:NEXP], r=['plog'], w=['lg_all'])

                self.norm_mod(stn, 'n4_%d' % b, lambda t, b=b: self.resC[b, t * 128:(t + 1) * 128, :], 16, lambda t, b=b: b, 1, 1,
                              None, 'hT4', want32=want32, tm_cb=tm_cb, post_cb=post_cb) if b == 0 else \
                    self.norm_mod(stn, 'n5_%d' % b, lambda t, b=b: self.resC[b, t * 128:(t + 1) * 128, :], 16, lambda t, b=b: b, 1, 1,
                                  None, 'hT4', want32=want32, tm_cb=tm_cb, post_cb=post_cb)
            lg2 = P.sb('lg2', [128, NTT, 8], F32, stn)
            msum = P.sb('msum', [128, NTT, 8], F32, stn)
            tq = P.sb('tq', [128, 5, NTT], F32, stn)
            pp_sb = P.sb('pp_sb', [128, NTT, 16], F32, stn)
            runb = P.sb('runb', [128, NTT + 1, 8], F32, stn)
            pposv = ppos[:, :].rearrange("p (t w) -> p t w", w=16)
            P.reduce(tq[:, 0, :], lg_all[:], ALU.max, r=['lg_all'], w=['tq0'])
            P.tt('dve', m12[:, 0, :, :], lg_all[:], bc(tq[:, 0, :].unsqueeze(2), [128, NTT, 8]), ALU.is_equal, r=['lg_all', 'tq0'], w=['m12a'])
            P.stt(lg2[:], m12[:, 0, :, :], -1.0e30, lg_all[:], ALU.mult, ALU.add, r=['m12a', 'lg_all'], w=['lg2'])
            P.reduce(tq[:, 1, :], lg2[:], ALU.max, r=['lg2'], w=['tq1'])
            P.tt('dve', m12[:, 1, :, :], lg2[:], bc(tq[:, 1, :].unsqueeze(2), [128, NTT, 8]), ALU.is_equal, r=['lg2', 'tq1'], w=['m12b'])
            P.tt('dve', tq[:, 2, :], tq[:, 1, :], tq[:, 0, :], ALU.subtract, r=['tq0', 'tq1'], w=['tq2'])
            P.act(tq[:, 3, :], tq[:, 2, :], AF.Exp, r=['tq2'], w=['tq3'])
            P.ts('dve', tq[:, 4, :], tq[:, 3, :], 1.0, ALU.add, r=['tq3'], w=['tq4'])
            P.recip(g12[:, 0, :], tq[:, 4, :], r=['tq4'], w=['g12a'])
            P.tt('dve', g12[:, 1, :], tq[:, 3, :], g12[:, 0, :], ALU.mult, r=['tq3', 'g12a'], w=['g12b'])
            P.tt('dve', msum[:], m12[:, 0, :, :], m12[:, 1, :, :], ALU.add, r=['m12a', 'm12b'], w=['msum'])
            for tt in range(NTT):
                P.mm(pposv[:, tt, 0:8], stri[:], msum[:, tt, :], r=['stri', 'msum'], w=['ppos'], inc=False)
                P.mm(pposv[:, tt, 8:16], ones128[:], msum[:, tt, :], r=['ones128', 'msum'], w=['ppos'], inc=(tt == NTT - 1))
            P.copy('dve', pp_sb[:], pposv, r=['ppos'], w=['pp_sb'])
            P.memset('dve', runb[:, 0, :], 0.0, w=['runb'])
            for tt in range(NTT):
                P.tt('dve', runb[:, tt + 1, :], runb[:, tt, :], pp_sb[:, tt, 8:16], ALU.add, r=['runb', 'pp_sb'], w=['runb'])
            P.tt('dve', pos[:], pp_sb[:, :, 0:8], runb[:, 0:NTT, :], ALU.add, r=['pp_sb', 'runb'], w=['pos'])
            P.copy('dve', run[:], runb[:, NTT, :], r=['runb'], w=['run'])
            thr = P.sb('thr', [128, 8, 8], F32, stn)
            kv = P.sb('kv', [128, NKT, 8], F32, stn)
            tokid = P.sb('tokid', [128, NTT], F32, stn)
            dflt = P.sb('dflt', [128, NSLOT // 128, 4], F32, stn)
            P.dma('sp', thr[:], I['thr'].rearrange("p (e m) -> p e m", m=8), writes=['thr'])
            P.dma('sp', kv[:], I['kv'].rearrange("p (k e) -> p k e", e=8), writes=['kv'])
            P.dma('sp', tokid[:], I['tokid'][:, :], writes=['tokid'])
            P.dma('sp', dflt[:], I['dflt'].rearrange("p (n w) -> p n w", w=4), writes=['dflt'])
            P.dma('sp', self.tab.rearrange("(p n) w -> p n w", p=128), dflt[:], reads=['dflt'], writes=['tab0'])
            cmp8 = P.sb('cmp8', [128, 8, 8], F32, stn)
            tl = P.sb('tl', [128, 4, 8], F32, stn)
            cmpk = P.sb('cmpk', [128, NKT, 8], F32, stn)
            ekf = P.sb('ekf', [128, NKT], F32, stn)
            big = P.sb('big', [128, NTT, 8], F32, stn)
            slf = P.sb('slf', [128, 2 * NTT], F32, stn)
            sli = P.sb('sli', [128, 2 * NTT], I32, stn)
            rowd = P.sb('rowd', [128, 2, NTT, 4], F32, stn)
            P.tt('dve', cmp8[:], thr[:], bc(run[:].unsqueeze(2), [128, 8, 8]), ALU.is_lt, r=['thr', 'run'], w=['cmp8'])
            P.reduce(tl[:, 0, :], cmp8[:], ALU.add, r=['cmp8'], w=['tl0'])
            P.copy('dve', tl[:, 1, 0:1], tl[:, 0, 0:1], r=['tl0'], w=['tl1'])
            for e_ in range(1, 8):
                P.tt('dve', tl[:, 1, e_:e_ + 1], tl[:, 1, e_ - 1:e_], tl[:, 0, e_:e_ + 1], ALU.add, r=['tl0', 'tl1'], w=['tl1'])
            P.tt('dve', tl[:, 2, :], tl[:, 1, :], tl[:, 0, :], ALU.subtract, r=['tl0', 'tl1'], w=['tl2'])
            P.ts('dve', tl[:, 2, :], tl[:, 2, :], float(SLOT_T), ALU.mult, r=['tl2'], w=['tl2'])
            P.tt('dve', cmpk[:], kv[:], bc(tl[:, 1, :].unsqueeze(1), [128, NKT, 8]), ALU.is_ge, r=['kv', 'tl1'], w=['cmpk'])
            P.reduce(ekf[:], cmpk[:], ALU.add, r=['cmpk'], w=['ekf'])
            P.ts('dve', ekf[:], ekf[:], 7.0, ALU.min, r=['ekf'], w=['ekf'])
            P.copy('dve', ek_i[:], ekf[:], r=['ekf'], w=['ek_i'])
            rowc4 = P.sb('rowc4', [128, 32], F32, stn)
            rowf = P.sb('rowf', [128, NFT], F32, stn)
            P.dma('sp', rowc4[:], I['rowc4'][:, :], writes=['rowc4'])
            P.dma('sp', rowf[:], I['rowf'][:, :], writes=['rowf'])
            idxg_f = P.sb('idxg_f', [128, NKT, 32], F32, stn)
            idxd_f = P.sb('idxd_f', [128, NKT, NFT], F32, stn)
            P.stt(idxg_f[:], bc(ekf[:].unsqueeze(2), [128, NKT, 32]), 4096.0, bc(rowc4[:].unsqueeze(1), [128, NKT, 32]),
                  ALU.mult, ALU.add, r=['ekf', 'rowc4'], w=['idxg_f'])
            P.stt(idxd_f[:], bc(ekf[:].unsqueeze(2), [128, NKT, NFT]), float(DFF), bc(rowf[:].unsqueeze(1), [128, NKT, NFT]),
                  ALU.mult, ALU.add, r=['ekf', 'rowf'], w=['idxd_f'])
            P.copy('dve', idxg[:], idxg_f[:], r=['idxg_f'], w=['idxg'])
            P.copy('dve', idxd[:], idxd_f[:], r=['idxd_f'], w=['idxd'])
            for r_ in range(2):
                P.tt('dve', big[:], pos[:], bc(tl[:, 2, :].unsqueeze(1), [128, NTT, 8]), ALU.add, r=['pos', 'tl2'], w=['big'])
                P.tt('dve', big[:], big[:], m12[:, r_, :, :], ALU.mult, r=['big', 'm12a', 'm12b'], w=['big'])
                P.reduce(slf[:, r_ * NTT:(r_ + 1) * NTT], big[:], ALU.add, r=['big'], w=['slf'])
                P.copy('dve', rowd[:, r_, :, 0], tokid[:], r=['tokid'], w=['rowd'])
                P.ts('dve', rowd[:, r_, :, 1], tokid[:], float(r_ * 2 * SEQ), ALU.add, r=['tokid'], w=['rowd'])
                P.copy('dve', rowd[:, r_, :, 2], g12[:, r_, :], r=['g12a', 'g12b'], w=['rowd'])
                P.memset('dve', rowd[:, r_, :, 3], 0.0, w=['rowd'])
            P.copy('dve', sli[:], slf[:], r=['slf'], w=['sli'])
            tab = self.tab
            for r_ in range(2):
                for tt in range(NTT):
                    j = r_ * NTT + tt

                    def sc(e, j=j, r_=r_, tt=tt):
                        return e.indirect_dma_start(out=tab[:, :], out_offset=bass.IndirectOffsetOnAxis(ap=sli[:, j:j + 1], axis=0),
                                                    in_=rowd[:, r_, tt, :], in_offset=None)
                    P.dma_raw('pool', sc, reads=['tab0', 'sli', 'rowd'], writes=['tab_s%d' % j])
            if self.debug:
                self.dump('ek_i', ek_i[:], [128, NKT], I32, reads=['ek_i'])
                self.dump('sli', sli[:], [128, 2 * NTT], I32, reads=['sli'])
                self.dump('run', run[:], [128, 8], F32, reads=['run'])
            P.end_phase()
        if self.upto == 'l1_router':
            return
        with ExitStack() as st:
            act = P.sb('mact', [128, NFT, SLOT_T], BF16, st)
            wbufs = [(P.sb('wg%d' % i, [128, 8, 896], BF16, st), P.sb('wu%d' % i, [128, 8, 896], BF16, st)) for i in range(2)]
            wd = [P.sb('mwd%d' % i, [128, 14, D], BF16, st) for i in range(2)]
            pgs = [P.bank('pg%d' % i, F32, st) for i in range(2)]
            pus = [P.bank('pu%d' % i, F32, st) for i in range(2)]
            pys = [P.bank('py%d' % i, F32, st) for i in range(2)]
            ptg = P.bank('ptg', BF16, st)
            ptgv = ptg[:, :].rearrange("p (c n) -> p c n", c=8)
            sgs = [P.sb('sg%d' % i, [128, 512], F32, st) for i in range(2)]
            tabt = [P.sb('tabt%d' % i, [128, 4, 4], F32, st) for i in range(2)]
            srci = [P.sb('srci%d' % i, [128, 4], I32, st) for i in range(2)]
            dsti = [P.sb('dsti%d' % i, [128, 4], I32, st) for i in range(2)]
            hg = P.sb('hg', [128, 4, D], BF16, st)
            hTg = [P.sb('hTg%d' % i, [128, 8, SLOT_T], BF16, st) for i in range(2)]
            ysb = P.sb('ysb', [128, 4, D], F32, st)
            h_d, y12 = self.h_d, self.y12
            wg4 = I['w_moe_gate'].rearrange("e r (q f) -> (e r q) f", q=4)
            wu4 = I['w_moe_up'].rearrange("e r (q f) -> (e r q) f", q=4)
            wd2 = I['w_moe_down'].rearrange("e r d -> (e r) d")
            ctr = [0, 0]
            iy = 0
            import os
            nkt = int(os.environ.get('MOE_NKT', NKT))
            pending = []

            def fetch(k):
                i2 = k % 2
                P.dma('sp', tabt[i2][:], self.tab[k * SLOT_T:(k + 1) * SLOT_T, :].rearrange("(j p) w -> p j w", p=128),
                      writes=['tabt%d' % i2])
                P.copy('dve', srci[i2][:], tabt[i2][:, :, 0], r=['tabt%d' % i2], w=['srci%d' % i2])
                P.copy('dve', dsti[i2][:], tabt[i2][:, :, 1], r=['tabt%d' % i2], w=['dsti%d' % i2])
                for j in range(4):
                    def ga(e, i2=i2, j=j):
                        return e.indirect_dma_start(out=hgs[i2][:, j, :], out_offset=None, in_=h_d[:, :],
                                                    in_offset=bass.IndirectOffsetOnAxis(ap=srci[i2][:, j:j + 1], axis=0))
                    P.dma_raw('pool', ga, reads=['srci%d' % i2], writes=['hg%d_%d' % (i2, j)])
                for j in range(4):
                    for c in range(8):
                        P.tr(ptgv[:, c, :], hgs[i2][:, j, c * 128:(c + 1) * 128], self.ident_b[:], r=['hg%d_%d' % (i2, j), 'ident_b'],
                             w=['ptg'], inc=(c == 7))
                    P.copy('act' if j % 2 else 'dve', hTg[i2][:, :, j * 128:(j + 1) * 128], ptgv, r=['ptg'], w=['hTg%d_%d' % (i2, j)])

            hgs = [hg, P.sb('hg_b', [128, 4, D], BF16, st)]
            fetch(0)
            for k in range(nkt):
                i2 = k % 2
                hkeys = ['hTg%d_%d' % (i2, j) for j in range(4)]
                for q in range(4):
                    kb = ctr[0] % 2; ctr[0] += 1
                    wg, wu = wbufs[kb]
                    for (wt, src4, nm) in ((wg, wg4, 'wg'), (wu, wu4, 'wu')):
                        for c in range(8):
                            def gw(e, wt=wt, src4=src4, k=k, c=c, q=q):
                                return e.indirect_dma_start(out=wt[:, c, :], out_offset=None, in_=src4[:, :],
                                                            in_offset=bass.IndirectOffsetOnAxis(ap=idxg[:, k, c * 4 + q:c * 4 + q + 1], axis=0))
                            P.dma_raw('pool', gw, reads=['idxg'], writes=['%s%d_%d' % (nm, kb, c)])
                    gk = ['wg%d_%d' % (kb, c) for c in range(8)]
                    uk = ['wu%d_%d' % (kb, c) for c in range(8)]
                    if q == 1:
                        for fn_, rd_, wr_ in pending:
                            P.dma_raw('pool', fn_, reads=rd_, writes=wr_)
                        pending = []
                    for fi in range(7):
                        f = q * 7 + fi
                        jj = ctr[1] % 2; ctr[1] += 1
                        for c in range(8):
                            P.mm(pgs[jj][:], wg[:, c, fi * 128:(fi + 1) * 128], hTg[i2][:, c, :], start=(c == 0), stop=(c == 7),
                                 r=[gk[c]] + hkeys, w=['pg%d' % jj])
                        for c in range(8):
                            P.mm(pus[jj][:], wu[:, c, fi * 128:(fi + 1) * 128], hTg[i2][:, c, :], start=(c == 0), stop=(c == 7),
                                 r=[uk[c]] + hkeys, w=['pu%d' % jj])
                        P.act(sgs[jj][:], pgs[jj][:], AF.Silu, r=['pg%d' % jj], w=['sg%d' % jj])
                        P.tt('dve', act[:, f, :], sgs[jj][:], pus[jj][:], ALU.mult, r=['sg%d' % jj, 'pu%d' % jj], w=['act%d' % f])
                if k + 1 < nkt:
                    fetch(k + 1)
                for fh in range(2):
                    for fl in range(14):
                        f = fh * 14 + fl
                        def gd(e, k=k, f=f, fh=fh, fl=fl):
                            return e.indirect_dma_start(out=wd[fh][:, fl, :], out_offset=None, in_=wd2[:, :],
                                                        in_offset=bass.IndirectOffsetOnAxis(ap=idxd[:, k, f:f + 1], axis=0))
                        P.dma_raw('pool', gd, reads=['idxd'], writes=['mwd%d_%d' % (fh, fl)])
                    for j in range(4):
                        for dh in range(2):
                            ip = iy % 2; iy += 1
                            for fl in range(14):
                                P.mm(pys[ip][:], act[:, fh * 14 + fl, j * 128:(j + 1) * 128], wd[fh][:, fl, dh * 512:(dh + 1) * 512],
                                     start=(fl == 0), stop=(fl == 13), r=['act%d' % (fh * 14 + fl), 'mwd%d_%d' % (fh, fl)], w=['py%d' % ip])
                            ys = ysb[:, j, dh * 512:(dh + 1) * 512]
                            yk = 'ysb%d_%d' % (j, dh)
                            if fh == 0:
                                P.ts('dve', ys, pys[ip][:], tabt[i2][:, j, 2:3], ALU.mult, r=['py%d' % ip, 'tabt%d' % i2], w=[yk])
                            else:
                                P.stt(ys, pys[ip][:], tabt[i2][:, j, 2:3], ys, ALU.mult, ALU.add, r=['py%d' % ip, 'tabt%d' % i2, yk], w=[yk])
                for j in range(4):
                    def scy(e, i2=i2, j=j):
                        return e.indirect_dma_start(out=y12[:, :], out_offset=bass.IndirectOffsetOnAxis(ap=dsti[i2][:, j:j + 1], axis=0),
                                                    in_=ysb[:, j, :], in_offset=None)
                    pending.append((scy, ['dsti%d' % i2, 'ysb%d_0' % j, 'ysb%d_1' % j], ['y12_%d_%d' % (k, j)]))
            for fn_, rd_, wr_ in pending:
                P.dma_raw('pool', fn_, reads=rd_, writes=wr_)
            P.end_phase()
        if self.upto == 'l1_experts':
            return
        with ExitStack() as st:
            G2 = [P.sb('G2f%d' % b, [128, D], F32, st) for b in range(2)]
            gfin = P.sb('gfin', [128, D], F32, st)
            for b in range(2):
                self.load_gate(G2[b], 'G2f%d' % b, 1, b, 2)
            P.dma('sp', gfin[:], I['g_final'][0:1, :].partition_broadcast(128), writes=['gfin'])
            xc = [P.sb('xc%d' % i, [128, D], F32, st) for i in range(2)]
            y1 = [P.sb('y1_%d' % i, [128, D], F32, st) for i in range(2)]
            y2 = [P.sb('y2_%d' % i, [128, D], F32, st) for i in range(2)]
            tmp = [P.sb('ftmp%d' % i, [128, D], F32, st) for i in range(2)]
            xo = [P.sb('fxo%d' % i, [128, D], F32, st) for i in range(2)]
            junk = P.sb('fjunk', [128, D], BF16, st)
            ss = P.sb('fss', [128, 2, 2], F32, st)
            ot = [P.sb('fot%d' % i, [128, D], F32, st) for i in range(2)]
            for tt in range(NTT):
                b, t = tt // 16, tt % 16
                i2 = tt % 2
                P.dma('sp', xc[i2][:], self.resC[b, t * 128:(t + 1) * 128, :], writes=['xc%d' % i2])
                P.dma('sp', y1[i2][:], self.y12[tt * 128:(tt + 1) * 128, :], writes=['y1_%d' % i2])
                P.dma('sp', y2[i2][:], self.y12[2 * SEQ + tt * 128:2 * SEQ + (tt + 1) * 128, :], writes=['y2_%d' % i2])
                P.tt('dve', y1[i2][:], y1[i2][:], y2[i2][:], ALU.add, r=['y1_%d' % i2, 'y2_%d' % i2], w=['y1_%d' % i2])
                P.tt('dve', tmp[i2][:], y1[i2][:], G2[b][:], ALU.mult, r=['y1_%d' % i2, 'G2f%d' % b], w=['ftmp%d' % i2])
                P.tt('pool', xo[i2][:], tmp[i2][:], xc[i2][:], ALU.add, r=['ftmp%d' % i2, 'xc%d' % i2], w=['fxo%d' % i2])
                P.sumsq(junk[:], xo[i2][:], ss[:, i2, 0:1], r=['fxo%d' % i2], w=['fjunk', 'fss0_%d' % i2])
                P.act(ss[:, i2, 1:2], ss[:, i2, 0:1], AF.Sqrt, r=['fss0_%d' % i2, 'epsb'], w=['fss1_%d' % i2], scale=1.0 / D, bias=self.epsb[:, 0:1])
                P.recip(ss[:, i2, 0:1], ss[:, i2, 1:2], r=['fss1_%d' % i2], w=['fss0_%d' % i2])
                P.stt(ot[i2][:], xo[i2][:], ss[:, i2, 0:1], gfin[:], ALU.mult, ALU.mult, r=['fxo%d' % i2, 'fss0_%d' % i2, 'gfin'], w=['fot%d' % i2])
                P.dma('act', self.out[b, t * 128:(t + 1) * 128, :], ot[i2][:], reads=['fot%d' % i2], writes=['out'])
            P.end_phase()


Builder.layer1_moe_sparse = _layer1_moe_sparse
Builder.l0_ffn = _l0_ffn
Builder.layer1_mixer = _layer1_mixer
Builder.layer1_moe = _layer1_moe
Builder._moe_gate_up = _moe_gate_up


def build(upto='all', debug=False):
    import os
    phases = {'all': ('l0a', 'l0f', 'l1a', 'l1f'), 'l0f_only': ('l0f',), 'l1a_only': ('l1a',), 'l1f_only': ('l1f',),
              'l1_router': ('l1f',), 'l1_experts': ('l1f',), 'l0': ('l0a', 'l0f'), 'mod': ()}.get(upto, ('l0a',))
    preload = {'l0f_only': ['resA'], 'l1a_only': ['resB'], 'l1f_only': ['resC'], 'l1_router': ['resC'], 'l1_experts': ['resC']}.get(upto, [])
    B = Builder(upto, debug, preload)
    B.declare()
    B.consts()
    B._ptb_bank = B.P.bank('ptb', BF16)
    B._ptb = B._ptb_bank[:, 0:512].rearrange("p (h d) -> p h d", d=128)
    B.phase_mod()
    nb = int(os.environ.get('NB', 2))
    if 'l0a' in phases:
        for b in range(nb):
            B.layer0_mixer(b)
            if upto in ('l0a_b0', 'l0_norm', 'l0_inproj', 'l0_gqa', 'l0_gla'):
                break
    if 'l0f' in phases:
        for b in range(nb):
            for half in range(2):
                B.l0_ffn(b, half)
    if 'l1a' in phases:
        for b in range(nb):
            B.layer1_mixer(b)
    if 'l1f' in phases:
        if os.environ.get('MOE_DENSE'):
            for b in range(nb):
                B.layer1_moe(b)
        else:
            B.layer1_moe_sparse()
    B.P.end_phase()
    return B


def _rope_tables():
    t = np.arange(SEQ, dtype=np.int32)
    row = (t // 64).astype(np.float32)
    col = (t % 64).astype(np.float32)
    n_axis = 16
    inv = np.power(np.float32(10000.0), -np.arange(n_axis, dtype=np.float32) / np.float32(n_axis)).astype(np.float32)
    ang = np.concatenate([row[:, None] * inv, col[:, None] * inv], axis=-1).astype(np.float32)
    cos = np.cos(ang).astype(np.float32)
    sin = np.sin(ang).astype(np.float32)
    C = np.repeat(cos, 2, axis=1)
    S = np.stack([-sin, sin], axis=-1).reshape(SEQ, 64)
    C = C.reshape(16, 128, 64).transpose(1, 0, 2)
    S = S.reshape(16, 128, 64).transpose(1, 0, 2)
    return np.ascontiguousarray(C), np.ascontiguousarray(S)


def _na_bias(rpb):
    NEG = np.float32(-30000.0)
    cols = np.arange(64)
    col_start = np.clip(cols - 8, 0, 48)
    in_win = (cols[None, :] >= col_start[:, None]) & (cols[None, :] < col_start[:, None] + 16)
    col_idx = np.clip(cols[None, :] - cols[:, None] + 15, 0, 30)
    dlist = list(range(-7, 7, 2)) + list(range(-6, 8, 2))
    out = np.empty((16, 128, 14, 64), np.float32)
    for di, d in enumerate(dlist):
        for i in range(2):
            dr = d + i + 7
            dr_c = min(max(dr, 0), 14)
            blk = rpb[:, dr_c][:, col_idx]
            blk = np.where(in_win[None], blk, NEG)
            out[:, 64 * i:64 * (i + 1), di, :] = blk.transpose(0, 2, 1)
    return np.ascontiguousarray(out.reshape(16, 128, 14 * 64))


def prep_inputs(inp, ncores=NCORES):
    f = lambda a: np.ascontiguousarray(np.asarray(a, dtype=np.float32))
    shared = {}
    shared['w_mod'] = f(inp['w_mod']); shared['b_mod'] = f(inp['b_mod'])
    gn = np.stack([f(inp['g_norm1']), f(inp['g_norm2'])], axis=1)
    shared['gnT'] = np.ascontiguousarray(gn.reshape(2, 2, 8, 128).transpose(3, 0, 1, 2))
    shared['w_in_ab'] = f(inp['w_in_ab'][0]); shared['g_q'] = f(inp['g_q']); shared['g_k'] = f(inp['g_k'])
    shared['w2f'] = f(np.concatenate([inp['w_a2_f'][0], inp['b_a_f'][0][None]], 0))
    shared['w2b'] = f(np.concatenate([inp['w_a2_b'][0], inp['b_a_b'][0][None]], 0))
    shared['g_gla'] = f(inp['g_gla'])
    shared['w_out_ab'] = f(inp['w_out_ab'][0])
    shared['w_ff_gate'] = f(inp['w_ff_gate'][0]); shared['w_ff_up'] = f(inp['w_ff_up'][0]); shared['w_ff_down'] = f(inp['w_ff_down'][0])
    shared['w_in_c'] = f(inp['w_in_c'][0]); shared['nabias'] = _na_bias(f(inp['rpb_c'][0])); shared['w_out_c'] = f(inp['w_out_c'][0])
    shared['w_router'] = np.ascontiguousarray(f(inp['w_router'][0]).reshape(8, 128, NEXP).transpose(1, 0, 2))
    shared['w_moe_gate'] = f(inp['w_moe_gate'][0]); shared['w_moe_up'] = f(inp['w_moe_up'][0]); shared['w_moe_down'] = f(inp['w_moe_down'][0])
    shared['g_final'] = f(inp['g_final']).reshape(1, D)
    shared['ident'] = np.eye(128, dtype=np.float32)
    idx = np.arange(128)
    shared['tri_f'] = (idx[:, None] <= idx[None, :]).astype(np.float32)
    shared['tri_b'] = (idx[:, None] >= idx[None, :]).astype(np.float32)
    shared['ropeC'], shared['ropeS'] = _rope_tables()
    shared['stri'] = (idx[:, None] < idx[None, :]).astype(np.float32)
    shared['thr'] = np.ascontiguousarray(np.broadcast_to((np.arange(8, dtype=np.float32) * SLOT_T)[None, None, :], (128, 8, 8)).reshape(128, 64))
    shared['kv'] = np.ascontiguousarray(np.broadcast_to(np.arange(NKT, dtype=np.float32)[None, :, None], (128, NKT, 8)).reshape(128, NKT * 8))
    shared['tokid'] = np.ascontiguousarray((np.arange(32, dtype=np.float32)[None, :] * 128 + np.arange(128, dtype=np.float32)[:, None]))
    dfl = np.zeros((NSLOT, 4), np.float32)
    dfl[:, 1] = 4 * SEQ + np.arange(NSLOT, dtype=np.float32)
    shared['dflt'] = np.ascontiguousarray(dfl.reshape(128, -1))
    shared['gn2_nat'] = f(inp['g_norm2'][1]).reshape(1, D)
    pp = np.arange(128, dtype=np.float32)[:, None, None]
    shared['rowc4'] = np.ascontiguousarray((4.0 * (np.arange(8, dtype=np.float32)[None, :, None] * 128 + pp)
                                            + np.arange(4, dtype=np.float32)[None, None, :]).reshape(128, 32))
    shared['rowf'] = np.ascontiguousarray(np.arange(NFT, dtype=np.float32)[None, :] * 128 + np.arange(128, dtype=np.float32)[:, None])
    x = f(inp['x']); ctx = f(inp['ctx']); c = f(inp['c']); cc = f(inp['c_ctx'])
    maps = []
    for k in range(ncores):
        m = dict(shared)
        m['x'] = x[2 * k:2 * k + 2]
        m['ctx'] = ctx[2 * k:2 * k + 2]
        cond = np.stack([c[2 * k], c[2 * k + 1], cc], axis=1)
        m['condT'] = np.ascontiguousarray(cond.reshape(8, 128, 3).transpose(1, 0, 2))
        maps.append(m)
    return maps


_CACHE = {}


def kernel(**inputs):
    if 'B' not in _CACHE:
        _CACHE['B'] = build()
    B = _CACHE['B']
    maps = prep_inputs(inputs)
    maps = [{k: v for k, v in m.items() if k in B.I} for m in maps]
    res = run_bass_kernel_spmd(B.nc, maps, core_ids=list(range(NCORES)))
    out = np.concatenate([np.asarray(r['out']) for r in res.results], axis=0)
    return out.astype(np.float32)
```
